# Optimizing a Trainium2 kernel written in Bass

```python
import jax, jax.numpy as jnp
from jax import lax
import numpy as np

D_MODEL = 2048
BATCH = 4
SEQ = 4096
DEPTH = 4
DEC_BATCH = 8
DEC_SEQ = 16
PAST_LEN = 2048

CHUNK = 64
N_MIXERS = 3
N_SGU_LAYERS = (DEPTH + 2) // 3
N_CONV_LAYERS = (DEPTH + 1) // 3
N_ATTN_LAYERS = DEPTH // 3
EPS = 1e-6
SGU_CHUNK = 128
SGU_DIM = 3 * D_MODEL
SGU_GROUPS = 8
SGU_GROUP_DIM = SGU_DIM // SGU_GROUPS
CONV_W = 31
WINDOW = 128
HEAD_DIM = 64
N_HEADS = D_MODEL // HEAD_DIM
KV_HEADS = N_HEADS // 8
Q_PER_KV = N_HEADS // KV_HEADS
Q_DIM = N_HEADS * HEAD_DIM
KV_DIM = KV_HEADS * HEAD_DIM
BAND_PREV = WINDOW // CHUNK
BAND = (BAND_PREV + 1) * CHUNK
NEG_INF = -1e30
PEER_HEADS = 8
N_KEYS = 128
N_EXPERTS = N_KEYS * N_KEYS
PEER_TOPK = 16
PEER_KEY_DIM = 256
PEER_HALF = PEER_KEY_DIM // 2
PEER_BLOCK = 128

kernel_name = "hybrid_streaming_encoder_step"


def rms_norm(x, g):
    xf = x.astype(jnp.float32)
    y = xf * lax.rsqrt(jnp.mean(xf * xf, axis=-1, keepdims=True) + EPS)
    return (y * g.astype(jnp.float32)).astype(x.dtype)


def layer_norm(x, g, b):
    xf = x.astype(jnp.float32)
    mu = jnp.mean(xf, axis=-1, keepdims=True)
    var = jnp.mean(jnp.square(xf - mu), axis=-1, keepdims=True)
    y = (xf - mu) * lax.rsqrt(var + EPS)
    return (y * g.astype(jnp.float32) + b.astype(jnp.float32)).astype(x.dtype)


def ada_params(c, w, b):
    m = jax.nn.silu(c) @ w + b
    return [t[:, None, :] for t in jnp.split(m, 6, axis=-1)]


def modulate(x, g, shift, scale):
    return rms_norm(x, g) * (1 + scale) + shift


def sgu_mask():
    t = jnp.arange(SGU_CHUNK)
    return (t[None, :] // CHUNK) <= (t[:, None] // CHUNK)


def sgu_mix(v, w_s, b_s):
    n_pos = v.shape[1]
    ws = (w_s * sgu_mask())[:, :n_pos, :n_pos].astype(v.dtype)
    return jnp.einsum('gts,nsge->ntge', ws, v) + b_s[:, :n_pos].T[None, :, :, None]


def chunk_mlp(h, w_in, b_in, ln_g, ln_b, w_s, b_s, w_out):
    b, s, _ = h.shape
    z = jax.nn.gelu(h @ w_in + b_in)
    u, v = z[..., :SGU_DIM], z[..., SGU_DIM:]
    v = layer_norm(v, ln_g, ln_b)
    n_pos = min(s, SGU_CHUNK)
    vc = v.reshape(b * (s // n_pos), n_pos, SGU_GROUPS, SGU_GROUP_DIM)
    mixed = sgu_mix(vc, w_s, b_s).reshape(b, s, SGU_DIM)
    return (u * mixed) @ w_out, v


def conv_module(h, hist, w_in, b_in, dw, dw_b, ln_g, ln_b, w_out, b_out):
    a = h @ w_in + b_in
    glu = a[..., :D_MODEL] * jax.nn.sigmoid(a[..., D_MODEL:])
    xp = jnp.concatenate([hist, glu], axis=1)
    y = lax.conv_general_dilated(xp, dw[:, None, :].astype(xp.dtype), window_strides=(1,), padding='VALID',
                                 dimension_numbers=('NWC', 'WIO', 'NWC'), feature_group_count=D_MODEL)
    y = jax.nn.silu(layer_norm(y + dw_b, ln_g, ln_b))
    return y @ w_out + b_out, xp[:, -(CONV_W - 1):]


def split_qkv(a):
    lead = a.shape[:-1]
    q = a[..., :Q_DIM].reshape(*lead, KV_HEADS, Q_PER_KV, HEAD_DIM)
    k = a[..., Q_DIM:Q_DIM + KV_DIM].reshape(*lead, KV_HEADS, HEAD_DIM)
    v = a[..., Q_DIM + KV_DIM:].reshape(*lead, KV_HEADS, HEAD_DIM)
    return q, k, v


def band_attention(q, k, v, key_valid, sinks):
    n_q, n_k = q.shape[-4], k.shape[-3]
    s = jnp.einsum('...qgrd,...kgd->...grqk', q, k).astype(jnp.float32) * (HEAD_DIM ** -0.5)
    slopes = jnp.exp2(-8.0 * jnp.arange(1, N_HEADS + 1, dtype=jnp.float32) / N_HEADS).reshape(KV_HEADS, Q_PER_KV)
    dist = jnp.abs(WINDOW + jnp.arange(n_q)[:, None] - jnp.arange(n_k)[None, :]).astype(jnp.float32)
    s = s - slopes[:, :, None, None] * dist
    s = jnp.where(key_valid[..., None, None, None, :], s, NEG_INF)
    sink = sinks.astype(jnp.float32).reshape(KV_HEADS, Q_PER_KV)[:, :, None, None]
    m = jnp.maximum(jnp.max(s, axis=-1, keepdims=True), sink)
    p = jnp.exp(s - m)
    p = p / (jnp.sum(p, axis=-1, keepdims=True) + jnp.exp(sink - m))
    return jnp.einsum('...grqk,...kgd->...qgrd', p.astype(v.dtype), v)


def swa_prompt(h, w_qkv, b_qkv, sinks, w_o):
    b, s, _ = h.shape
    n_c = s // CHUNK
    q, k, v = split_qkv(h @ w_qkv + b_qkv)
    qc = q.reshape(b, n_c, CHUNK, KV_HEADS, Q_PER_KV, HEAD_DIM)
    pad = ((0, 0), (WINDOW, 0), (0, 0), (0, 0))
    kp = jnp.pad(k, pad).reshape(b, n_c + BAND_PREV, CHUNK, KV_HEADS, HEAD_DIM)
    vp = jnp.pad(v, pad).reshape(b, n_c + BAND_PREV, CHUNK, KV_HEADS, HEAD_DIM)
    kb = jnp.concatenate([kp[:, j:j + n_c] for j in range(BAND_PREV + 1)], axis=2)
    vb = jnp.concatenate([vp[:, j:j + n_c] for j in range(BAND_PREV + 1)], axis=2)
    k_pos = jnp.arange(n_c)[:, None] * CHUNK - WINDOW + jnp.arange(BAND)[None, :]
    o = band_attention(qc, kb, vb, (k_pos >= 0)[None], sinks).reshape(b, s, Q_DIM)
    return o @ w_o, k[:, -WINDOW:], v[:, -WINDOW:]


def swa_sample(h, cache_k, cache_v, w_qkv, b_qkv, sinks, w_o):
    b, t, _ = h.shape
    q, k, v = split_qkv(h @ w_qkv + b_qkv)
    kk = jnp.concatenate([cache_k.astype(k.dtype), k], axis=1)
    vv = jnp.concatenate([cache_v.astype(v.dtype), v], axis=1)
    valid = jnp.ones((1, kk.shape[1]), dtype=bool)
    o = band_attention(q, kk, vv, valid, sinks).reshape(b, t, Q_DIM)
    return o @ w_o, kk[:, -WINDOW:], vv[:, -WINDOW:]


def peer(x, w_q, subkeys, u_tab, v_tab):
    lead = x.shape[:-1]
    xf = x.reshape(-1, D_MODEL)
    n = xf.shape[0]
    n_blocks = -(-n // PEER_BLOCK)
    xb = jnp.pad(xf, ((0, n_blocks * PEER_BLOCK - n), (0, 0))).reshape(n_blocks, PEER_BLOCK, D_MODEL)

    def block(xt):
        q = (xt @ w_q).reshape(PEER_BLOCK, PEER_HEADS, 2, PEER_HALF)
        s = jnp.einsum('thcd,hckd->thck', q, subkeys).astype(jnp.float32)
        sv, si = lax.top_k(s, PEER_TOPK)
        cand = (sv[:, :, 0, :, None] + sv[:, :, 1, None, :]).reshape(PEER_BLOCK, PEER_HEADS, PEER_TOPK * PEER_TOPK)
        best, bi = lax.top_k(cand, PEER_TOPK)
        i1 = jnp.take_along_axis(si[:, :, 0], bi // PEER_TOPK, axis=-1)
        i2 = jnp.take_along_axis(si[:, :, 1], bi % PEER_TOPK, axis=-1)
        e = i1 * N_KEYS + i2
        g = jax.nn.softmax(best, axis=-1)
        a = jnp.einsum('thkd,td->thk', u_tab[e], xt)
        w = (g * jax.nn.gelu(a.astype(jnp.float32))).astype(xt.dtype)
        return jnp.einsum('thk,thkd->td', w, v_tab[e])

    out = lax.map(block, xb).reshape(n_blocks * PEER_BLOCK, D_MODEL)[:n]
    return out.reshape(*lead, D_MODEL)


def setup_inputs(seed: int = 0) -> dict:
    key = jax.random.key(seed)
    ks = iter(jax.random.split(key, 48))

    def nrm(shape, std):
        return std * jax.random.normal(next(ks), shape, jnp.float32)

    def gain(shape):
        return 1.0 + nrm(shape, 0.05)

    d = D_MODEL
    return {
        "x_prompt": nrm((BATCH, SEQ, d), 1.0),
        "x_sample": nrm((DEC_BATCH, DEC_SEQ, d), 1.0),
        "cache_k_win": nrm((N_ATTN_LAYERS, DEC_BATCH, WINDOW, KV_HEADS, HEAD_DIM), 1.0),
        "cache_v_win": nrm((N_ATTN_LAYERS, DEC_BATCH, WINDOW, KV_HEADS, HEAD_DIM), 1.0),
        "state_conv": nrm((N_CONV_LAYERS, DEC_BATCH, CONV_W - 1, d), 0.5),
        "c_prompt": nrm((BATCH, d), 1.0),
        "c_sample": nrm((DEC_BATCH, d), 1.0),
        "norm_mix_g": gain((DEPTH, d)),
        "norm_ch_g": gain((DEPTH, d)),
        "norm_final_g": gain((d,)),
        "ada_w": nrm((DEPTH, d, 6 * d), 0.5 * d ** -0.5),
        "ada_b": nrm((DEPTH, 6 * d), 0.02),
        "sgu_w_in": nrm((N_SGU_LAYERS, d, 2 * SGU_DIM), d ** -0.5),
        "sgu_b_in": nrm((N_SGU_LAYERS, 2 * SGU_DIM), 0.02),
        "sgu_ln_g": gain((N_SGU_LAYERS, SGU_DIM)),
        "sgu_ln_b": nrm((N_SGU_LAYERS, SGU_DIM), 0.02),
        "sgu_w_s": nrm((N_SGU_LAYERS, SGU_GROUPS, SGU_CHUNK, SGU_CHUNK), SGU_CHUNK ** -0.5),
        "sgu_b_s": 1.0 + nrm((N_SGU_LAYERS, SGU_GROUPS, SGU_CHUNK), 0.1),
        "sgu_w_out": nrm((N_SGU_LAYERS, SGU_DIM, d), SGU_DIM ** -0.5),
        "conv_w_in": nrm((N_CONV_LAYERS, d, 2 * d), d ** -0.5),
        "conv_b_in": nrm((N_CONV_LAYERS, 2 * d), 0.02),
        "conv_dw": nrm((N_CONV_LAYERS, CONV_W, d), CONV_W ** -0.5),
        "conv_dw_b": nrm((N_CONV_LAYERS, d), 0.02),
        "conv_ln_g": gain((N_CONV_LAYERS, d)),
        "conv_ln_b": nrm((N_CONV_LAYERS, d), 0.02),
        "conv_w_out": nrm((N_CONV_LAYERS, d, d), d ** -0.5),
        "conv_b_out": nrm((N_CONV_LAYERS, d), 0.02),
        "attn_w_qkv": nrm((N_ATTN_LAYERS, d, Q_DIM + 2 * KV_DIM), d ** -0.5),
        "attn_b_qkv": nrm((N_ATTN_LAYERS, Q_DIM + 2 * KV_DIM), 0.02),
        "attn_sinks": nrm((N_ATTN_LAYERS, N_HEADS), 0.5),
        "attn_w_o": nrm((N_ATTN_LAYERS, Q_DIM, d), Q_DIM ** -0.5),
        "peer_w_q": nrm((DEPTH, d, PEER_HEADS * PEER_KEY_DIM), d ** -0.5),
        "peer_subkeys": nrm((DEPTH, PEER_HEADS, 2, N_KEYS, PEER_HALF), PEER_HALF ** -0.5),
        "peer_u": nrm((DEPTH, N_EXPERTS, d), d ** -0.5),
        "peer_v": nrm((DEPTH, N_EXPERTS, d), PEER_HEADS ** -0.5),
    }


def reference(x_prompt, x_sample, cache_k_win, cache_v_win, state_conv, c_prompt, c_sample,
              norm_mix_g, norm_ch_g, norm_final_g, ada_w, ada_b,
              sgu_w_in, sgu_b_in, sgu_ln_g, sgu_ln_b, sgu_w_s, sgu_b_s, sgu_w_out,
              conv_w_in, conv_b_in, conv_dw, conv_dw_b, conv_ln_g, conv_ln_b, conv_w_out, conv_b_out,
              attn_w_qkv, attn_b_qkv, attn_sinks, attn_w_o,
              peer_w_q, peer_subkeys, peer_u, peer_v):
    xp, xs = x_prompt, x_sample
    ia = ib = ic = 0
    conv_p, conv_s, kwin_p, vwin_p, kwin_s, vwin_s, sgu_s = [], [], [], [], [], [], []
    for layer in range(DEPTH):
        sh1p, sc1p, g1p, sh2p, sc2p, g2p = ada_params(c_prompt, ada_w[layer], ada_b[layer])
        sh1s, sc1s, g1s, sh2s, sc2s, g2s = ada_params(c_sample, ada_w[layer], ada_b[layer])
        hp = modulate(xp, norm_mix_g[layer], sh1p, sc1p)
        hs = modulate(xs, norm_mix_g[layer], sh1s, sc1s)
        kind = layer % N_MIXERS
        if kind == 0:
            args = (sgu_w_in[ia], sgu_b_in[ia], sgu_ln_g[ia], sgu_ln_b[ia], sgu_w_s[ia], sgu_b_s[ia], sgu_w_out[ia])
            op, _ = chunk_mlp(hp, *args)
            os_, v_rows = chunk_mlp(hs, *args)
            sgu_s.append(v_rows)
            ia += 1
        elif kind == 1:
            args = (conv_w_in[ib], conv_b_in[ib], conv_dw[ib], conv_dw_b[ib], conv_ln_g[ib], conv_ln_b[ib],
                    conv_w_out[ib], conv_b_out[ib])
            op, st_p = conv_module(hp, jnp.zeros((hp.shape[0], CONV_W - 1, D_MODEL), hp.dtype), *args)
            os_, st_s = conv_module(hs, state_conv[ib].astype(hs.dtype), *args)
            conv_p.append(st_p)
            conv_s.append(st_s)
            ib += 1
        else:
            args = (attn_w_qkv[ic], attn_b_qkv[ic], attn_sinks[ic], attn_w_o[ic])
            op, kp, vp = swa_prompt(hp, *args)
            os_, ks, vs = swa_sample(hs, cache_k_win[ic], cache_v_win[ic], *args)
            kwin_p.append(kp)
            vwin_p.append(vp)
            kwin_s.append(ks)
            vwin_s.append(vs)
            ic += 1
        xp = xp + g1p * op
        xs = xs + g1s * os_
        hp = modulate(xp, norm_ch_g[layer], sh2p, sc2p)
        hs = modulate(xs, norm_ch_g[layer], sh2s, sc2s)
        peer_args = (peer_w_q[layer], peer_subkeys[layer], peer_u[layer], peer_v[layer])
        xp = xp + g2p * peer(hp, *peer_args)
        xs = xs + g2s * peer(hs, *peer_args)
    y_prompt = rms_norm(xp, norm_final_g)
    y_sample = rms_norm(xs, norm_final_g)
    return (y_prompt, y_sample, jnp.stack(conv_p), jnp.stack(kwin_p), jnp.stack(vwin_p),
            jnp.stack(conv_s), jnp.stack(kwin_s), jnp.stack(vwin_s), jnp.stack(sgu_s))
```

```python
import numpy as np
from contextlib import ExitStack
import concourse.bass as bass
import concourse.mybir as mybir
from concourse.bass_utils import run_bass_kernel_spmd

F32 = mybir.dt.float32
BF16 = mybir.dt.bfloat16
I32 = mybir.dt.int32
U32 = mybir.dt.uint32
ALU = mybir.AluOpType
AF = mybir.ActivationFunctionType
AX = mybir.AxisListType


class Ctr:
    LIM = 30000

    def __init__(self, K, name, step):
        self.K, self.name, self.step = K, name, step
        self.sems = []
        self._new()

    def _new(self):
        s = self.K.stack.enter_context(self.K.nc.semaphore(f"{self.name}_{len(self.sems)}"))
        self.sems.append(s)
        self.val = 0

    def next(self):
        if self.val + self.step > self.LIM:
            self._new()
        self.val += self.step
        return (self.sems[-1], self.val)


class Buf:
    def __init__(self, K, ap, name):
        self.K, self.ap, self.name = K, ap, name
        self.w = None
        self.r = {}
        self._d = None

    @property
    def dctr(self):
        if self._d is None:
            self._d = Ctr(self.K, "d_" + self.name, 16)
        return self._d

    def __getitem__(self, k):
        return self.ap[k]


class Eng:
    def __init__(self, K, name, obj, counted):
        self.name, self.obj = name, obj
        self.ctr = Ctr(K, "e_" + name, 1) if counted else None
        self.known = {}
        self.prog = []

    def filt(self, deps):
        best = {}
        for (s, v) in deps:
            if self.name == "pe" and self.ctr is not None and s in self.ctr.sems:
                continue
            if self.known.get(s, 0) >= v:
                continue
            if best.get(s, 0) < v:
                best[s] = v
        for s, v in best.items():
            self.known[s] = v
        return list(best.items())


class K:
    def __init__(self, nc, stack):
        self.nc, self.stack = nc, stack
        self.eng = {
            "pe": Eng(self, "pe", nc.tensor, True),
            "act": Eng(self, "act", nc.scalar, True),
            "dve": Eng(self, "dve", nc.vector, True),
            "pool": Eng(self, "pool", nc.gpsimd, True),
            "sp": Eng(self, "sp", nc.sync, False),
        }
        self.bufs = []
        self.n = 0

    def sb(self, name, shape, dt):
        t = self.stack.enter_context(self.nc.sbuf_tensor(name, list(shape), dt))
        b = Buf(self, t, name)
        self.bufs.append(b)
        return b

    def sb_once(self, name, shape, dt):
        if not hasattr(self, "_once"):
            self._once = {}
        if name not in self._once:
            self._once[name] = self.sb(name, shape, dt)
        return self._once[name]

    def ps(self, name, shape, dt):
        t = self.stack.enter_context(self.nc.psum_tensor(name, list(shape), dt))
        b = Buf(self, t, name)
        self.bufs.append(b)
        return b

    def view(self, ap, name):
        b = Buf(self, ap, name)
        self.bufs.append(b)
        return b

    def _deps(self, reads, writes):
        deps = []
        for b in reads:
            if b.w:
                deps.append(b.w)
        for b in writes:
            if b.w:
                deps.append(b.w)
            deps.extend(b.r.items())
        return deps

    def _commit(self, tok, reads, writes):
        for b in reads:
            if b.r.get(tok[0], 0) < tok[1]:
                b.r[tok[0]] = tok[1]
        for b in writes:
            b.w = tok
            b.r = {}

    def op(self, eng, fn, reads=(), writes=()):
        E = self.eng[eng]
        waits = E.filt(self._deps(reads, writes))
        tok = E.ctr.next()
        E.prog.append((waits, fn, tok, 1))
        self._commit(tok, reads, writes)
        self.n += 1

    def dma(self, q, fn, cbuf, reads=(), writes=()):
        E = self.eng[q]
        waits = E.filt(self._deps(reads, writes))
        tok = cbuf.dctr.next()
        E.prog.append((waits, fn, tok, 16))
        self._commit(tok, reads, writes)
        self.n += 1

    def barrier(self):
        deps = []
        for b in self.bufs:
            if b.w:
                deps.append(b.w)
            deps.extend(b.r.items())
        for E in self.eng.values():
            waits = E.filt(deps)
            if waits:
                E.prog.append((waits, None, None, 0))
        for b in self.bufs:
            b.w = None
            b.r = {}

    def finish(self):
        E = self.eng["sp"]
        deps = []
        for b in self.bufs:
            if b.w:
                deps.append(b.w)
            deps.extend(b.r.items())
        waits = E.filt(deps)
        E.prog.append((waits, None, None, 0))
        nc = self.nc

        def replay(name, e):
            for (waits, fn, tok, inc) in self.eng[name].prog:
                for (s, v) in waits:
                    e.wait_ge(s, v)
                if fn is not None:
                    ins = fn(e)
                    ins.then_inc(tok[0], inc)

        with nc.Block() as block:
            @block.sync
            def _(e):
                replay("sp", e)

            @block.tensor
            def _(e):
                replay("pe", e)

            @block.scalar
            def _(e):
                replay("act", e)

            @block.vector
            def _(e):
                replay("dve", e)

            @block.gpsimd
            def _(e):
                replay("pool", e)


D = 2048
NCH = 16
PH, PK, PTOP = 8, 128, 16
NHC = 16
NEG = -1e30


class Common:
    def __init__(self, k):
        self.k = k
        identi = k.sb("identi", [128, 128], I32)
        self.ident = k.sb("ident", [128, 128], F32)
        k.op("pool", lambda e: e.iota(identi[:], pattern=[[1, 128]], base=0, channel_multiplier=-1), writes=[identi])
        k.op("dve", lambda e: e.tensor_single_scalar(out=self.ident[:], in_=identi[:], scalar=0, op=ALU.is_equal),
             reads=[identi], writes=[self.ident])
        self.ones_bf = k.sb("ones_bf", [128, 128], BF16)
        k.op("dve", lambda e: e.memset(self.ones_bf[:], 1.0), writes=[self.ones_bf])
        io16 = k.sb("io16i", [128, 16], I32)
        self.iota16 = k.sb("iota16", [128, 16], F32)
        k.op("pool", lambda e: e.iota(io16[:], pattern=[[1, 16]], base=0, channel_multiplier=0), writes=[io16])
        k.op("dve", lambda e: e.tensor_copy(out=self.iota16[:], in_=io16[:]), reads=[io16], writes=[self.iota16])
        self.bank = [k.ps(f"bank{i}", [128, 512], F32) for i in range(8)]


def rms_modulate(k, C, xT, gm, sh, hT32, hTb, T=128):
    sq = k.sb_once("rm_sq", [128, NCH, 128], BF16)
    rs = k.sb_once("rm_rstd", [128, 128], F32)
    pb = C.bank[0]
    k.op("act", lambda e: e.activation(out=sq[:, :, 0:T], in_=xT[:, :, 0:T], func=AF.Square), reads=[xT], writes=[sq])
    for c in range(NCH):
        k.op("pe", lambda e, c=c: e.matmul(pb[:, 0:T], lhsT=C.ones_bf[:], rhs=sq[:, c, 0:T], start=(c == 0), stop=(c == NCH - 1)),
             reads=[sq, C.ones_bf], writes=[pb])
    k.op("dve", lambda e: e.tensor_scalar(out=rs[:, 0:T], in0=pb[:, 0:T], scalar1=1.0 / D, scalar2=1e-6, op0=ALU.mult, op1=ALU.add),
         reads=[pb], writes=[rs])
    k.op("act", lambda e: e.activation(out=rs[:, 0:T], in_=rs[:, 0:T], func=AF.Sqrt), reads=[rs], writes=[rs])
    k.op("dve", lambda e: e.reciprocal(out=rs[:, 0:T], in_=rs[:, 0:T]), reads=[rs], writes=[rs])
    k.op("dve", lambda e: e.tensor_tensor(out=hT32[:, :, 0:T], in0=xT[:, :, 0:T],
                                          in1=rs[:, 0:T].unsqueeze(1).to_broadcast([128, NCH, T]), op=ALU.mult),
         reads=[xT, rs], writes=[hT32])
    k.op("dve", lambda e: e.tensor_tensor(out=hT32[:, :, 0:T], in0=hT32[:, :, 0:T],
                                          in1=gm[:, :].unsqueeze(2).to_broadcast([128, NCH, T]), op=ALU.mult),
         reads=[hT32, gm], writes=[hT32])
    k.op("dve", lambda e: e.tensor_tensor(out=hT32[:, :, 0:T], in0=hT32[:, :, 0:T],
                                          in1=sh[:, :].unsqueeze(2).to_broadcast([128, NCH, T]), op=ALU.add),
         reads=[hT32, sh], writes=[hT32])
    if hTb is not None:
        k.op("act", lambda e: e.copy(out=hTb[:, :, 0:T], in_=hT32[:, :, 0:T]), reads=[hT32], writes=[hTb])


def to_token_major(k, C, srcT, dst, T=128):
    for b4 in range(4):
        pb = C.bank[1 + (b4 % 2)]
        for j in range(4):
            c = b4 * 4 + j
            k.op("pe", lambda e, c=c, j=j, pb=pb: e.transpose(pb[0:T, j * 128:(j + 1) * 128], srcT[:, c, 0:T], C.ident[:]),
                 reads=[srcT, C.ident], writes=[pb])
        k.op("act", lambda e, b4=b4, pb=pb: e.copy(out=dst[0:T, b4 * 512:(b4 + 1) * 512], in_=pb[0:T, :]), reads=[pb], writes=[dst])


def gated_residual_add(k, C, xT, out_tm, gate, T=128):
    for b4 in range(4):
        pb = C.bank[1 + (b4 % 2)]
        for j in range(4):
            c = b4 * 4 + j
            k.op("pe", lambda e, c=c, j=j, pb=pb: e.transpose(pb[:, j * 128:j * 128 + T], out_tm[0:T, c * 128:(c + 1) * 128], C.ident[0:T, 0:T]),
                 reads=[out_tm, C.ident], writes=[pb])
        for j in range(4):
            c = b4 * 4 + j
            k.op("dve", lambda e, c=c, j=j, pb=pb: e.scalar_tensor_tensor(out=xT[:, c, 0:T], in0=pb[:, j * 128:j * 128 + T], scalar=gate[:, c:c + 1],
                                                                          in1=xT[:, c, 0:T], op0=ALU.mult, op1=ALU.add),
                 reads=[pb, gate, xT], writes=[xT])


class PeerW:
    def __init__(self, k, wq_b, skT_b, u_tab, v_tab):
        self.wq, self.skT, self.u, self.v = wq_b, skT_b, u_tab, v_tab


def peer(k, C, xT, gm, sh, gate, W, S, T=128, ws=None, mid_barrier=False):
    rms_modulate(k, C, xT, gm, sh, S.hT32, S.hTb, T)
    to_token_major(k, C, S.hT32, S.h_tm, T)
    if ws is not None:
        peer_q_streamed(k, C, ws, W.wq, S, T)
    else:
        for hc in range(NHC):
            pb = C.bank[3 + (hc % 2)]
            for c in range(NCH):
                k.op("pe", lambda e, hc=hc, c=c, pb=pb: e.matmul(pb[:, 0:T], lhsT=W.wq[:, c, hc * 128:(hc + 1) * 128], rhs=S.hTb[:, c, 0:T],
                                                                start=(c == 0), stop=(c == NCH - 1)), reads=[W.wq, S.hTb], writes=[pb])
            k.op("act", lambda e, hc=hc, pb=pb: e.copy(out=S.qT[:, hc, 0:T], in_=pb[:, 0:T]), reads=[pb], writes=[S.qT])
    for hc in range(NHC):
        pb = C.bank[5 + (hc // 4) % 2]
        k.op("pe", lambda e, hc=hc, pb=pb: e.matmul(pb[0:T, (hc % 4) * 128:(hc % 4 + 1) * 128], lhsT=S.qT[:, hc, 0:T], rhs=W.skT[:, hc, :], start=True, stop=True),
             reads=[S.qT, W.skT], writes=[pb])
        if hc % 4 == 3:
            k.op("act", lambda e, hc=hc, pb=pb: e.copy(out=S.s[0:T, hc - 3:hc + 1, :], in_=pb[0:T, :].rearrange("p (a b) -> p a b", a=4)), reads=[pb], writes=[S.s])
    for hc in range(NHC):
        k.op("dve", lambda e, hc=hc: e.max(out=S.sv[0:T, hc, 0:8], in_=S.s[0:T, hc, :]), reads=[S.s], writes=[S.sv])
        k.op("dve", lambda e, hc=hc: e.max_index(out=S.si[0:T, hc, 0:8], in_max=S.sv[0:T, hc, 0:8], in_values=S.s[0:T, hc, :]), reads=[S.s, S.sv], writes=[S.si])
        k.op("dve", lambda e, hc=hc: e.match_replace(out=S.s2[0:T, hc, :], in_to_replace=S.sv[0:T, hc, 0:8], in_values=S.s[0:T, hc, :], imm_value=NEG), reads=[S.s, S.sv], writes=[S.s2])
        k.op("dve", lambda e, hc=hc: e.max(out=S.sv[0:T, hc, 8:16], in_=S.s2[0:T, hc, :]), reads=[S.s2], writes=[S.sv])
        k.op("dve", lambda e, hc=hc: e.max_index(out=S.si[0:T, hc, 8:16], in_max=S.sv[0:T, hc, 8:16], in_values=S.s2[0:T, hc, :]), reads=[S.s2, S.sv], writes=[S.si])
    k.op("dve", lambda e: e.tensor_copy(out=S.sif[0:T], in_=S.si[0:T]), reads=[S.si], writes=[S.sif])
    sv4 = S.sv.ap.rearrange("p (h c) j -> p h c j", c=2)
    sif4 = S.sif.ap.rearrange("p (h c) j -> p h c j", c=2)
    for h in range(PH):
        k.op("dve", lambda e, h=h: e.tensor_tensor(out=S.cand[0:T, h, :, :], in0=sv4[0:T, h, 0, :].unsqueeze(2).to_broadcast([T, 16, 16]),
                                                   in1=sv4[0:T, h, 1, :].unsqueeze(1).to_broadcast([T, 16, 16]), op=ALU.add), reads=[S.sv], writes=[S.cand])
    candf = S.cand.ap.rearrange("p h a b -> p h (a b)")
    cand2f = S.cand2.ap.rearrange("p h a b -> p h (a b)")
    for h in range(PH):
        k.op("dve", lambda e, h=h: e.max(out=S.best[0:T, h, 0:8], in_=candf[0:T, h, :]), reads=[S.cand], writes=[S.best])
        k.op("dve", lambda e, h=h: e.max_index(out=S.bi[0:T, h, 0:8], in_max=S.best[0:T, h, 0:8], in_values=candf[0:T, h, :]), reads=[S.cand, S.best], writes=[S.bi])
        k.op("dve", lambda e, h=h: e.match_replace(out=cand2f[0:T, h, :], in_to_replace=S.best[0:T, h, 0:8], in_values=candf[0:T, h, :], imm_value=NEG), reads=[S.cand, S.best], writes=[S.cand2])
        k.op("dve", lambda e, h=h: e.max(out=S.best[0:T, h, 8:16], in_=cand2f[0:T, h, :]), reads=[S.cand2], writes=[S.best])
        k.op("dve", lambda e, h=h: e.max_index(out=S.bi[0:T, h, 8:16], in_max=S.best[0:T, h, 8:16], in_values=cand2f[0:T, h, :]), reads=[S.cand2, S.best], writes=[S.bi])
    k.op("dve", lambda e: e.tensor_single_scalar(out=S.b1[0:T], in_=S.bi[0:T], scalar=4, op=ALU.logical_shift_right), reads=[S.bi], writes=[S.b1])
    k.op("dve", lambda e: e.tensor_single_scalar(out=S.b2[0:T], in_=S.bi[0:T], scalar=15, op=ALU.bitwise_and), reads=[S.bi], writes=[S.b2])
    k.op("dve", lambda e: e.tensor_copy(out=S.b1f[0:T], in_=S.b1[0:T]), reads=[S.b1], writes=[S.b1f])
    k.op("dve", lambda e: e.tensor_copy(out=S.b2f[0:T], in_=S.b2[0:T]), reads=[S.b2], writes=[S.b2f])
    for (bf, half, dst) in ((S.b1f, 0, S.I1), (S.b2f, 1, S.I2)):
        for h in range(PH):
            k.op("dve", lambda e, bf=bf, h=h: e.tensor_tensor(out=S.oh[0:T], in0=C.iota16[0:T, :].unsqueeze(1).to_broadcast([T, 16, 16]),
                                                              in1=bf[0:T, h, :].unsqueeze(2).to_broadcast([T, 16, 16]), op=ALU.is_equal), reads=[C.iota16, bf], writes=[S.oh])
            k.op("dve", lambda e, h=h, half=half: e.tensor_tensor(out=S.oh[0:T], in0=S.oh[0:T], in1=sif4[0:T, h, half, :].unsqueeze(1).to_broadcast([T, 16, 16]), op=ALU.mult),
                 reads=[S.oh, S.sif], writes=[S.oh])
            k.op("dve", lambda e, h=h, dst=dst: e.tensor_reduce(out=dst[0:T, h, :], in_=S.oh[0:T], axis=AX.X, op=ALU.add), reads=[S.oh], writes=[dst])
    k.op("dve", lambda e: e.scalar_tensor_tensor(out=S.ef[0:T], in0=S.I1[0:T], scalar=float(PK), in1=S.I2[0:T], op0=ALU.mult, op1=ALU.add), reads=[S.I1, S.I2], writes=[S.ef])
    k.op("dve", lambda e: e.tensor_copy(out=S.ei[0:T], in_=S.ef[0:T]), reads=[S.ef], writes=[S.ei])
    k.op("dve", lambda e: e.tensor_tensor(out=S.g[0:T], in0=S.best[0:T], in1=S.best[0:T, :, 0:1].to_broadcast([T, PH, 16]), op=ALU.subtract), reads=[S.best], writes=[S.g])
    k.op("act", lambda e: e.activation(out=S.g[0:T], in_=S.g[0:T], func=AF.Exp), reads=[S.g], writes=[S.g])
    k.op("dve", lambda e: e.tensor_reduce(out=S.gs[0:T], in_=S.g[0:T], axis=AX.X, op=ALU.add), reads=[S.g], writes=[S.gs])
    k.op("dve", lambda e: e.reciprocal(out=S.gs[0:T], in_=S.gs[0:T]), reads=[S.gs], writes=[S.gs])
    k.op("dve", lambda e: e.tensor_tensor(out=S.g[0:T], in0=S.g[0:T], in1=S.gs[0:T].unsqueeze(2).to_broadcast([T, PH, 16]), op=ALU.mult), reads=[S.g, S.gs], writes=[S.g])
    if mid_barrier:
        k.barrier()
    eif = S.ei.ap.rearrange("p h k -> p (h k)")
    af = S.a.ap.rearrange("p h k -> p (h k)")
    nb = len(S.gbuf)
    for hk in range(PH * PTOP):
        gb = S.gbuf[hk % nb]
        k.dma("pool", lambda e, hk=hk, gb=gb: e.indirect_dma_start(out=gb[0:T, :], out_offset=None, in_=W.u,
              in_offset=bass.IndirectOffsetOnAxis(ap=eif[0:T, hk:hk + 1], axis=0)), gb, reads=[S.ei], writes=[gb])
        k.op("dve", lambda e, hk=hk, gb=gb: e.scalar_tensor_tensor(out=S.junk[0:T], in0=gb[0:T, :], scalar=1.0, in1=S.h_tm[0:T, :], op0=ALU.mult, op1=ALU.mult,
                                                                   accum_out=af[0:T, hk:hk + 1]), reads=[gb, S.h_tm], writes=[S.junk, S.a])
    k.op("act", lambda e: e.activation(out=S.w[0:T], in_=S.a[0:T], func=AF.Gelu_apprx_tanh), reads=[S.a], writes=[S.w])
    k.op("dve", lambda e: e.tensor_tensor(out=S.w[0:T], in0=S.w[0:T], in1=S.g[0:T], op=ALU.mult), reads=[S.w, S.g], writes=[S.w])
    wf = S.w.ap.rearrange("p h k -> p (h k)")
    for hk in range(PH * PTOP):
        gb = S.gbuf[hk % nb]
        k.dma("pool", lambda e, hk=hk, gb=gb: e.indirect_dma_start(out=gb[0:T, :], out_offset=None, in_=W.v,
              in_offset=bass.IndirectOffsetOnAxis(ap=eif[0:T, hk:hk + 1], axis=0)), gb, reads=[S.ei], writes=[gb])
        if hk == 0:
            k.op("dve", lambda e, gb=gb: e.tensor_scalar(out=S.acc[0:T, :], in0=gb[0:T, :], scalar1=wf[0:T, 0:1], scalar2=None, op0=ALU.mult), reads=[gb, S.w], writes=[S.acc])
        else:
            k.op("dve", lambda e, hk=hk, gb=gb: e.scalar_tensor_tensor(out=S.acc[0:T, :], in0=gb[0:T, :], scalar=wf[0:T, hk:hk + 1], in1=S.acc[0:T, :], op0=ALU.mult, op1=ALU.add),
                 reads=[gb, S.w, S.acc], writes=[S.acc])
    gated_residual_add(k, C, xT, S.acc, gate, T)


class NS:
    pass


def peer_scratch(k, ngb=4):
    S = NS()
    S.hT32 = k.sb_once("p_hT32", [128, NCH, 128], F32)
    S.hTb = k.sb_once("p_hTb", [128, NCH, 128], BF16)
    S.h_tm = k.sb("p_h_tm", [128, D], F32)
    S.qT = k.sb("p_qT", [128, NHC, 128], BF16)
    S.s = k.sb("p_s", [128, NHC, PK], F32)
    S.s2 = k.sb("p_s2", [128, NHC, PK], F32)
    S.sv = k.sb("p_sv", [128, NHC, 16], F32)
    S.si = k.sb("p_si", [128, NHC, 16], U32)
    S.sif = k.sb("p_sif", [128, NHC, 16], F32)
    S.cand = k.sb("p_cand", [128, PH, 16, 16], F32)
    S.cand2 = k.sb("p_cand2", [128, PH, 16, 16], F32)
    S.best = k.sb("p_best", [128, PH, 16], F32)
    S.bi = k.sb("p_bi", [128, PH, 16], U32)
    S.b1 = k.sb("p_b1", [128, PH, 16], U32)
    S.b2 = k.sb("p_b2", [128, PH, 16], U32)
    S.b1f = k.sb("p_b1f", [128, PH, 16], F32)
    S.b2f = k.sb("p_b2f", [128, PH, 16], F32)
    S.oh = k.sb("p_oh", [128, 16, 16], F32)
    S.I1 = k.sb("p_I1", [128, PH, 16], F32)
    S.I2 = k.sb("p_I2", [128, PH, 16], F32)
    S.ef = k.sb("p_ef", [128, PH, 16], F32)
    S.ei = k.sb("p_ei", [128, PH, 16], I32)
    S.g = k.sb("p_g", [128, PH, 16], F32)
    S.gs = k.sb("p_gs", [128, PH], F32)
    S.a = k.sb("p_a", [128, PH, 16], F32)
    S.w = k.sb("p_w", [128, PH, 16], F32)
    S.junk = k.sb("p_junk", [128, D], F32)
    S.acc = k.sb_once("p_acc", [128, D], F32)
    S.gbuf = [k.sb(f"p_gb{i}", [128, D], F32) for i in range(ngb)]
    return S


class WS:
    def __init__(self, k, nslots=4, q="sp"):
        self.k, self.q, self.i = k, q, 0
        self.slots = [k.sb(f"wslot{i}", [128, 16, 512], BF16) for i in range(nslots)]
        self.bslots = [k.sb(f"bslot{i}", [128, 512], F32) for i in range(4)]
        self.bi = 0

    def block(self, w2d, kc0, col0, nkc=16, ncols=512):
        s = self.slots[self.i % len(self.slots)]
        self.i += 1
        src = w2d[kc0 * 128:(kc0 + nkc) * 128, col0:col0 + ncols].rearrange("(c p) n -> p c n", p=128)
        self.k.dma(self.q, lambda e: e.dma_start(out=s[:, 0:nkc, 0:ncols], in_=src), s, writes=[s])
        return s

    def row_bcast(self, row1d, col0, ncols=512):
        s = self.bslots[self.bi % len(self.bslots)]
        self.bi += 1
        src = row1d[col0:col0 + ncols].unsqueeze(0).partition_broadcast(128)
        self.k.dma(self.q, lambda e: e.dma_start(out=s[:, 0:ncols], in_=src), s, writes=[s])
        return s


SGD = 6144


def sgu(k, C, ws, xT, gm, sh, gate, Wd, S, T=128, v_out=None):
    rms_modulate(k, C, xT, gm, sh, S.hT32, S.hTb, T)
    for nb in range(24):
        slot = ws.block(Wd["w_in"], 0, nb * 512)
        brow = ws.row_bcast(Wd["b_in"], nb * 512)
        pb = C.bank[3 + nb % 2]
        for c in range(NCH):
            k.op("pe", lambda e, c=c, pb=pb, slot=slot: e.matmul(pb[0:T, :], lhsT=S.hTb[:, c, 0:T], rhs=slot[:, c, :], start=(c == 0), stop=(c == NCH - 1)),
                 reads=[S.hTb, slot], writes=[pb])
        off = (nb % 12) * 512
        if nb < 12:
            k.op("dve", lambda e, pb=pb, brow=brow: e.tensor_tensor(out=S.tmp[0:T, :], in0=pb[0:T, :], in1=brow[0:T, :], op=ALU.add), reads=[pb, brow], writes=[S.tmp])
            k.op("act", lambda e, off=off: e.activation(out=S.u[0:T, off:off + 512], in_=S.tmp[0:T, :], func=AF.Gelu_apprx_tanh), reads=[S.tmp], writes=[S.u])
        else:
            k.op("dve", lambda e, pb=pb, brow=brow, off=off: e.tensor_tensor(out=S.v[0:T, off:off + 512], in0=pb[0:T, :], in1=brow[0:T, :], op=ALU.add), reads=[pb, brow], writes=[S.v])
            k.op("act", lambda e, off=off: e.activation(out=S.v[0:T, off:off + 512], in_=S.v[0:T, off:off + 512], func=AF.Gelu_apprx_tanh), reads=[S.v], writes=[S.v])
    k.op("dve", lambda e: e.tensor_reduce(out=S.st[0:T, 0:1], in_=S.v[0:T, :], axis=AX.X, op=ALU.add), reads=[S.v], writes=[S.st])
    k.op("act", lambda e: e.activation(out=S.vb[0:T, :], in_=S.v[0:T, :], func=AF.Square, accum_out=S.st[0:T, 1:2]), reads=[S.v, S.st], writes=[S.vb, S.st])
    k.op("dve", lambda e: e.tensor_scalar(out=S.st[0:T, 0:2], in0=S.st[0:T, 0:2], scalar1=1.0 / SGD, scalar2=None, op0=ALU.mult), reads=[S.st], writes=[S.st])
    k.op("dve", lambda e: e.tensor_tensor(out=S.st[0:T, 2:3], in0=S.st[0:T, 0:1], in1=S.st[0:T, 0:1], op=ALU.mult), reads=[S.st], writes=[S.st])
    k.op("dve", lambda e: e.tensor_tensor(out=S.st[0:T, 2:3], in0=S.st[0:T, 1:2], in1=S.st[0:T, 2:3], op=ALU.subtract), reads=[S.st], writes=[S.st])
    k.op("dve", lambda e: e.tensor_scalar(out=S.st[0:T, 2:3], in0=S.st[0:T, 2:3], scalar1=1e-6, scalar2=None, op0=ALU.add), reads=[S.st], writes=[S.st])
    k.op("act", lambda e: e.activation(out=S.st[0:T, 2:3], in_=S.st[0:T, 2:3], func=AF.Sqrt), reads=[S.st], writes=[S.st])
    k.op("dve", lambda e: e.reciprocal(out=S.st[0:T, 2:3], in_=S.st[0:T, 2:3]), reads=[S.st], writes=[S.st])
    k.op("dve", lambda e: e.tensor_scalar(out=S.v[0:T, :], in0=S.v[0:T, :], scalar1=S.st[0:T, 0:1], scalar2=S.st[0:T, 2:3], op0=ALU.subtract, op1=ALU.mult),
         reads=[S.v, S.st], writes=[S.v])
    for nb in range(12):
        grow = ws.row_bcast(Wd["ln_g"], nb * 512)
        brow = ws.row_bcast(Wd["ln_b"], nb * 512)
        k.op("dve", lambda e, nb=nb, grow=grow: e.tensor_tensor(out=S.v[0:T, nb * 512:(nb + 1) * 512], in0=S.v[0:T, nb * 512:(nb + 1) * 512], in1=grow[0:T, :], op=ALU.mult),
             reads=[S.v, grow], writes=[S.v])
        k.op("dve", lambda e, nb=nb, brow=brow: e.tensor_tensor(out=S.v[0:T, nb * 512:(nb + 1) * 512], in0=S.v[0:T, nb * 512:(nb + 1) * 512], in1=brow[0:T, :], op=ALU.add),
             reads=[S.v, brow], writes=[S.v])
    if v_out is not None:
        k.dma("sp", lambda e: e.dma_start(out=v_out, in_=S.v[0:T, :]), S.v, reads=[S.v])
    k.op("act", lambda e: e.copy(out=S.vb[0:T, :], in_=S.v[0:T, :]), reads=[S.v], writes=[S.vb])
    for g in range(8):
        for (o, n) in ((0, 512), (512, 256)):
            pb = C.bank[5 + (2 * g + (o > 0)) % 2]
            c0 = g * 768 + o
            k.op("pe", lambda e, g=g, pb=pb, c0=c0, n=n: e.matmul(pb[0:T, 0:n], lhsT=Wd["wsT"][0:T, g, 0:T], rhs=S.vb[0:T, c0:c0 + n], start=True, stop=True),
                 reads=[Wd["wsT"], S.vb], writes=[pb])
            k.op("dve", lambda e, g=g, pb=pb, c0=c0, n=n: e.scalar_tensor_tensor(out=S.prod[0:T, c0:c0 + n], in0=pb[0:T, 0:n], scalar=Wd["bs"][0:T, g:g + 1],
                                                                                in1=S.u[0:T, c0:c0 + n], op0=ALU.add, op1=ALU.mult), reads=[pb, Wd["bs"], S.u], writes=[S.prod])
    for b4 in range(12):
        pb = C.bank[1 + b4 % 2]
        for j in range(4):
            c = b4 * 4 + j
            k.op("pe", lambda e, c=c, j=j, pb=pb: e.transpose(pb[:, j * 128:j * 128 + T], S.prod[0:T, c * 128:(c + 1) * 128], C.ident[0:T, 0:T]), reads=[S.prod, C.ident], writes=[pb])
        k.op("act", lambda e, b4=b4, pb=pb: e.copy(out=S.prodT[:, b4 * 4:(b4 + 1) * 4, 0:T], in_=pb[:, :].rearrange("p (a b) -> p a b", a=4)[:, :, 0:T]), reads=[pb], writes=[S.prodT])
    for nb in range(4):
        pb = C.bank[3 + nb % 2]
        for ku in range(3):
            slot = ws.block(Wd["w_out"], ku * 16, nb * 512)
            for c in range(16):
                kc = ku * 16 + c
                k.op("pe", lambda e, kc=kc, c=c, pb=pb, slot=slot: e.matmul(pb[0:T, :], lhsT=S.prodT[:, kc, 0:T], rhs=slot[:, c, :], start=(kc == 0), stop=(kc == 47)),
                     reads=[S.prodT, slot], writes=[pb])
        k.op("act", lambda e, nb=nb, pb=pb: e.copy(out=S.out_tm[0:T, nb * 512:(nb + 1) * 512], in_=pb[0:T, :]), reads=[pb], writes=[S.out_tm])
    gated_residual_add(k, C, xT, S.out_tm, gate, T)


def sgu_scratch(k):
    S = NS()
    S.hT32 = k.sb_once("p_hT32", [128, NCH, 128], F32)
    S.hTb = k.sb_once("p_hTb", [128, NCH, 128], BF16)
    S.u = k.sb("g_u", [128, SGD], BF16)
    S.v = k.sb("g_v", [128, SGD], F32)
    S.vb = k.sb("g_vb", [128, SGD], BF16)
    S.prod = S.v
    S.tmp = k.sb("g_tmp", [128, 512], F32)
    S.prodT = k.sb("g_prodT", [128, 48, 128], BF16)
    S.st = k.sb("g_st", [128, 4], F32)
    S.out_tm = k.sb_once("p_acc", [128, D], F32)
    return S


CW = 31


def conv_load_hist_zero(k, S):
    k.op("dve", lambda e: e.memset(S.xp[:, :, 0:CW - 1], 0.0), writes=[S.xp])


def conv_load_hist_state(k, C, S, state_dram):
    k.dma("sp", lambda e: e.dma_start(out=S.tm30[0:CW - 1, :], in_=state_dram), S.tm30, writes=[S.tm30])
    for b4 in range(4):
        pb = C.bank[1 + b4 % 2]
        for j in range(4):
            c = b4 * 4 + j
            k.op("pe", lambda e, c=c, j=j, pb=pb: e.transpose(pb[:, j * 128:j * 128 + CW - 1], S.tm30[0:CW - 1, c * 128:(c + 1) * 128], C.ident[0:CW - 1, 0:CW - 1]),
                 reads=[S.tm30, C.ident], writes=[pb])
        k.op("act", lambda e, b4=b4, pb=pb: e.copy(out=S.xp[:, b4 * 4:(b4 + 1) * 4, 0:CW - 1], in_=pb[:, :].rearrange("p (a b) -> p a b", a=4)[:, :, 0:CW - 1]),
             reads=[pb], writes=[S.xp])


def conv(k, C, ws, xT, gm, sh, gate, Wd, S, T=128, state_out=None, carry=True):
    H = CW - 1
    rms_modulate(k, C, xT, gm, sh, S.hT32, S.hTb, T)
    for b in range(4):
        sl = ws.block(Wd["w_in"], 0, b * 512)
        sg = ws.block(Wd["w_in"], 0, (b + 4) * 512)
        pl, pg = C.bank[3 + b % 2], C.bank[5 + b % 2]
        for j in range(4):
            for c in range(NCH):
                k.op("pe", lambda e, j=j, c=c, pl=pl, sl=sl: e.matmul(pl[:, j * 128:j * 128 + T], lhsT=sl[:, c, j * 128:(j + 1) * 128], rhs=S.hTb[:, c, 0:T], start=(c == 0), stop=(c == NCH - 1)),
                     reads=[sl, S.hTb], writes=[pl])
            for c in range(NCH):
                k.op("pe", lambda e, j=j, c=c, pg=pg, sg=sg: e.matmul(pg[:, j * 128:j * 128 + T], lhsT=sg[:, c, j * 128:(j + 1) * 128], rhs=S.hTb[:, c, 0:T], start=(c == 0), stop=(c == NCH - 1)),
                     reads=[sg, S.hTb], writes=[pg])
        for j in range(4):
            ch = b * 4 + j
            k.op("act", lambda e, j=j, ch=ch, pg=pg: e.activation(out=S.sg[:, 0:T], in_=pg[:, j * 128:j * 128 + T], func=AF.Sigmoid, bias=Wd["b_in"][:, 16 + ch:17 + ch]),
                 reads=[pg, Wd["b_in"]], writes=[S.sg])
            k.op("dve", lambda e, j=j, ch=ch, pl=pl: e.scalar_tensor_tensor(out=S.xp[:, ch, H:H + T], in0=pl[:, j * 128:j * 128 + T], scalar=Wd["b_in"][:, ch:ch + 1], in1=S.sg[:, 0:T],
                                                                            op0=ALU.add, op1=ALU.mult), reads=[pl, Wd["b_in"], S.sg], writes=[S.xp])
    if state_out is not None:
        for b4 in range(4):
            pb = C.bank[1 + b4 % 2]
            for j in range(4):
                c = b4 * 4 + j
                k.op("pe", lambda e, c=c, j=j, pb=pb: e.transpose(pb[0:H, j * 128:(j + 1) * 128], S.xp[:, c, T:T + H], C.ident[:]), reads=[S.xp, C.ident], writes=[pb])
            k.op("act", lambda e, b4=b4, pb=pb: e.copy(out=S.tm30[0:H, b4 * 512:(b4 + 1) * 512], in_=pb[0:H, :]), reads=[pb], writes=[S.tm30])
        k.dma("sp", lambda e: e.dma_start(out=state_out, in_=S.tm30[0:H, :]), S.tm30, reads=[S.tm30])
    for c in range(NCH):
        k.op("act", lambda e, c=c: e.activation(out=S.y[:, c, 0:T], in_=S.xp[:, c, 0:T], func=AF.Identity, scale=Wd["dw"][:, c, 0:1], bias=Wd["dw_b"][:, c:c + 1]),
             reads=[S.xp, Wd["dw"], Wd["dw_b"]], writes=[S.y])
        for kk in range(1, CW):
            k.op("dve", lambda e, c=c, kk=kk: e.scalar_tensor_tensor(out=S.y[:, c, 0:T], in0=S.xp[:, c, kk:kk + T], scalar=Wd["dw"][:, c, kk:kk + 1], in1=S.y[:, c, 0:T],
                                                                     op0=ALU.mult, op1=ALU.add), reads=[S.xp, Wd["dw"], S.y], writes=[S.y])
    if carry:
        k.op("act", lambda e: e.copy(out=S.hist_tmp[:, :, :], in_=S.xp[:, :, T:T + H]), reads=[S.xp], writes=[S.hist_tmp])
        k.op("act", lambda e: e.copy(out=S.xp[:, :, 0:H], in_=S.hist_tmp[:, :, :]), reads=[S.hist_tmp], writes=[S.xp])
    k.op("act", lambda e: e.copy(out=S.yb[:, :, 0:T], in_=S.y[:, :, 0:T]), reads=[S.y], writes=[S.yb])
    k.op("act", lambda e: e.activation(out=S.y2b[:, :, 0:T], in_=S.y[:, :, 0:T], func=AF.Square), reads=[S.y], writes=[S.y2b])
    pb = C.bank[0]
    for (src, off) in ((S.yb, 0), (S.y2b, 128)):
        for c in range(NCH):
            k.op("pe", lambda e, src=src, off=off, c=c, pb=pb: e.matmul(pb[:, off:off + T], lhsT=C.ones_bf[:], rhs=src[:, c, 0:T], start=(c == 0), stop=(c == NCH - 1)),
                 reads=[src, C.ones_bf], writes=[pb])
    k.op("dve", lambda e, pb=pb: e.tensor_scalar(out=S.mv[:, 0:256], in0=pb[:, 0:256], scalar1=1.0 / D, scalar2=None, op0=ALU.mult), reads=[pb], writes=[S.mv])
    k.op("dve", lambda e: e.tensor_tensor(out=S.mv[:, 256:256 + T], in0=S.mv[:, 0:T], in1=S.mv[:, 0:T], op=ALU.mult), reads=[S.mv], writes=[S.mv])
    k.op("dve", lambda e: e.tensor_tensor(out=S.mv[:, 256:256 + T], in0=S.mv[:, 128:128 + T], in1=S.mv[:, 256:256 + T], op=ALU.subtract), reads=[S.mv], writes=[S.mv])
    k.op("dve", lambda e: e.tensor_scalar(out=S.mv[:, 256:256 + T], in0=S.mv[:, 256:256 + T], scalar1=1e-6, scalar2=None, op0=ALU.add), reads=[S.mv], writes=[S.mv])
    k.op("act", lambda e: e.activation(out=S.mv[:, 256:256 + T], in_=S.mv[:, 256:256 + T], func=AF.Sqrt), reads=[S.mv], writes=[S.mv])
    k.op("dve", lambda e: e.reciprocal(out=S.mv[:, 256:256 + T], in_=S.mv[:, 256:256 + T]), reads=[S.mv], writes=[S.mv])
    k.op("dve", lambda e: e.tensor_tensor(out=S.y[:, :, 0:T], in0=S.y[:, :, 0:T], in1=S.mv[:, 0:T].unsqueeze(1).to_broadcast([128, NCH, T]), op=ALU.subtract), reads=[S.y, S.mv], writes=[S.y])
    k.op("dve", lambda e: e.tensor_tensor(out=S.y[:, :, 0:T], in0=S.y[:, :, 0:T], in1=S.mv[:, 256:256 + T].unsqueeze(1).to_broadcast([128, NCH, T]), op=ALU.mult), reads=[S.y, S.mv], writes=[S.y])
    for c in range(NCH):
        k.op("act", lambda e, c=c: e.activation(out=S.yb[:, c, 0:T], in_=S.y[:, c, 0:T], func=AF.Silu, scale=Wd["ln_g"][:, c:c + 1], bias=Wd["ln_b"][:, c:c + 1]),
             reads=[S.y, Wd["ln_g"], Wd["ln_b"]], writes=[S.yb])
    for nb in range(4):
        slot = ws.block(Wd["w_out"], 0, nb * 512)
        brow = ws.row_bcast(Wd["b_out"], nb * 512)
        pb = C.bank[3 + nb % 2]
        for c in range(NCH):
            k.op("pe", lambda e, c=c, pb=pb, slot=slot: e.matmul(pb[0:T, :], lhsT=S.yb[:, c, 0:T], rhs=slot[:, c, :], start=(c == 0), stop=(c == NCH - 1)), reads=[S.yb, slot], writes=[pb])
        k.op("dve", lambda e, nb=nb, pb=pb, brow=brow: e.tensor_tensor(out=S.out_tm[0:T, nb * 512:(nb + 1) * 512], in0=pb[0:T, :], in1=brow[0:T, :], op=ALU.add), reads=[pb, brow], writes=[S.out_tm])
    gated_residual_add(k, C, xT, S.out_tm, gate, T)


def conv_scratch(k):
    S = NS()
    S.hT32 = k.sb_once("p_hT32", [128, NCH, 128], F32)
    S.hTb = k.sb_once("p_hTb", [128, NCH, 128], BF16)
    S.out_tm = k.sb_once("p_acc", [128, D], F32)
    S.xp = k.sb("c_xp", [128, NCH, CW - 1 + 128], F32)
    S.hist_tmp = k.sb("c_hist", [128, NCH, CW - 1], F32)
    S.y = k.sb("c_y", [128, NCH, 128], F32)
    S.yb = k.sb("c_yb", [128, NCH, 128], BF16)
    S.y2b = k.sb("c_y2b", [128, NCH, 128], BF16)
    S.sg = k.sb("c_sg", [128, 128], F32)
    S.mv = k.sb("c_mv", [128, 384], F32)
    S.tm30 = k.sb("c_tm30", [32, D], F32)
    return S


NH, NKV, HD = 32, 4, 64
BIGD = 1e32


def attn_consts(k):
    A = NS()
    di = k.sb("a_di", [128, 256], I32)
    A.Dmid = k.sb("a_Dmid", [128, 256], F32)
    A.Dfirst = k.sb("a_Dfirst", [128, 256], F32)
    A.Dsamp = k.sb("a_Dsamp", [128, 256], F32)
    k.op("pool", lambda e: e.iota(di[:], pattern=[[-1, 256]], base=128, channel_multiplier=1), writes=[di])
    k.op("dve", lambda e: e.tensor_copy(out=A.Dsamp[:], in_=di[:]), reads=[di], writes=[A.Dsamp])
    k.op("dve", lambda e: e.tensor_scalar(out=A.Dmid[:], in0=A.Dsamp[:], scalar1=-1.0, scalar2=None, op0=ALU.mult), reads=[A.Dsamp], writes=[A.Dmid])
    k.op("dve", lambda e: e.tensor_max(out=A.Dsamp[:], in0=A.Dsamp[:], in1=A.Dmid[:]), reads=[A.Dsamp, A.Dmid], writes=[A.Dsamp])
    for Dm in (A.Dmid, A.Dfirst):
        k.op("dve", lambda e, Dm=Dm: e.tensor_copy(out=Dm[:], in_=A.Dsamp[:]), reads=[A.Dsamp], writes=[Dm])
        k.op("dve", lambda e, Dm=Dm: e.memset(Dm[0:64, 192:256], BIGD), writes=[Dm])
        k.op("dve", lambda e, Dm=Dm: e.memset(Dm[64:128, 0:64], BIGD), writes=[Dm])
    k.op("dve", lambda e: e.memset(A.Dfirst[:, 0:128], BIGD), writes=[A.Dfirst])
    return A


def attn_load_prev(k, C, S, ck_dram, cv_dram):
    k.dma("sp", lambda e: e.dma_start(out=S.kv_tm[:, 0:256], in_=ck_dram), S.kv_tm, writes=[S.kv_tm])
    k.dma("sp", lambda e: e.dma_start(out=S.kv_tm[:, 256:512], in_=cv_dram), S.kv_tm, writes=[S.kv_tm])
    pb = C.bank[1]
    for g in range(NKV):
        k.op("pe", lambda e, g=g: e.transpose(pb[0:HD, g * 128:(g + 1) * 128], S.kv_tm[:, g * HD:(g + 1) * HD], C.ident[:]), reads=[S.kv_tm, C.ident], writes=[pb])
    k.op("act", lambda e: e.copy(out=S.kTprev[:, :, :], in_=pb[0:HD, :].rearrange("p (a b) -> p a b", a=NKV)), reads=[pb], writes=[S.kTprev])
    k.op("act", lambda e: e.copy(out=S.vprev[:, :], in_=S.kv_tm[:, 256:512]), reads=[S.kv_tm], writes=[S.vprev])


def attn(k, C, ws, xT, gm, sh, gate, Wd, S, Dm, T=128, kwin_out=None, vwin_out=None, win_rows=None, carry=True):
    slopes = [2.0 ** (-8.0 * (h + 1) / NH) for h in range(NH)]
    rms_modulate(k, C, xT, gm, sh, S.hT32, S.hTb, T)
    for nb in range(4):
        slot = ws.block(Wd["w_qkv"], 0, nb * 512)
        for half in range(2):
            pb = C.bank[1 + half]
            for j in range(4):
                hl = half * 4 + j
                for c in range(NCH):
                    k.op("pe", lambda e, hl=hl, j=j, c=c, pb=pb, slot=slot: e.matmul(pb[0:HD, j * 128:j * 128 + T], lhsT=slot[:, c, hl * HD:(hl + 1) * HD], rhs=S.hTb[:, c, 0:T],
                                                                                start=(c == 0), stop=(c == NCH - 1)), reads=[slot, S.hTb], writes=[pb])
            for j in range(4):
                h = nb * 8 + half * 4 + j
                k.op("dve", lambda e, h=h, j=j, pb=pb: e.tensor_scalar(out=S.qT[:, h, 0:T], in0=pb[0:HD, j * 128:j * 128 + T], scalar1=Wd["bq"][:, h:h + 1], scalar2=HD ** -0.5,
                                                                       op0=ALU.add, op1=ALU.mult), reads=[pb, Wd["bq"]], writes=[S.qT])
    slot = ws.block(Wd["w_qkv"], 0, 2048)
    brow = ws.row_bcast(Wd["b_qkv"], 2048)
    pb = C.bank[3]
    for c in range(NCH):
        k.op("pe", lambda e, c=c, pb=pb, slot=slot: e.matmul(pb[0:T, :], lhsT=S.hTb[:, c, 0:T], rhs=slot[:, c, :], start=(c == 0), stop=(c == NCH - 1)), reads=[S.hTb, slot], writes=[pb])
    k.op("dve", lambda e, pb=pb, brow=brow: e.tensor_tensor(out=S.kv_tm[0:T, :], in0=pb[0:T, :], in1=brow[0:T, :], op=ALU.add), reads=[pb, brow], writes=[S.kv_tm])
    if kwin_out is not None:
        r0, n = win_rows
        k.dma("sp", lambda e: e.dma_start(out=kwin_out[r0:r0 + n, :], in_=S.kv_tm[T - n:T, 0:256]), S.kv_tm, reads=[S.kv_tm])
        k.dma("sp", lambda e: e.dma_start(out=vwin_out[r0:r0 + n, :], in_=S.kv_tm[T - n:T, 256:512]), S.kv_tm, reads=[S.kv_tm])
    k.op("act", lambda e: e.copy(out=S.vcur[0:T, :], in_=S.kv_tm[0:T, 256:512]), reads=[S.kv_tm], writes=[S.vcur])
    pb = C.bank[4]
    for g in range(NKV):
        k.op("pe", lambda e, g=g, pb=pb: e.transpose(pb[0:HD, g * 128:g * 128 + T], S.kv_tm[0:T, g * HD:(g + 1) * HD], C.ident[0:T, 0:T]), reads=[S.kv_tm, C.ident], writes=[pb])
    k.op("act", lambda e, pb=pb: e.copy(out=S.kTcur[:, :, 0:T], in_=pb[0:HD, :].rearrange("p (a b) -> p a b", a=NKV)[:, :, 0:T]), reads=[pb], writes=[S.kTcur])
    NKEY = 128 + T
    for h in range(NH):
        g = h // (NH // NKV)
        ps_, pt_, po_ = C.bank[5 + h % 2], C.bank[1 + h % 2], C.bank[7]
        k.op("pe", lambda e, h=h, g=g, ps_=ps_: e.matmul(ps_[0:T, 0:128], lhsT=S.qT[:, h, 0:T], rhs=S.kTprev[:, g, :], start=True, stop=True), reads=[S.qT, S.kTprev], writes=[ps_])
        k.op("pe", lambda e, h=h, g=g, ps_=ps_: e.matmul(ps_[0:T, 128:NKEY], lhsT=S.qT[:, h, 0:T], rhs=S.kTcur[:, g, 0:T], start=True, stop=True), reads=[S.qT, S.kTcur], writes=[ps_])
        k.op("dve", lambda e, h=h, ps_=ps_: e.scalar_tensor_tensor(out=S.sc[0:T, 0:NKEY], in0=Dm[0:T, 0:NKEY], scalar=-slopes[h], in1=ps_[0:T, 0:NKEY], op0=ALU.mult, op1=ALU.add),
             reads=[Dm, ps_], writes=[S.sc])
        k.op("dve", lambda e: e.tensor_reduce(out=S.m[0:T, 0:1], in_=S.sc[0:T, 0:NKEY], axis=AX.X, op=ALU.max), reads=[S.sc], writes=[S.m])
        k.op("dve", lambda e, h=h: e.tensor_tensor(out=S.m[0:T, 0:1], in0=S.m[0:T, 0:1], in1=Wd["sink"][0:T, h:h + 1], op=ALU.max), reads=[S.m, Wd["sink"]], writes=[S.m])
        k.op("dve", lambda e, h=h: e.tensor_tensor(out=S.m[0:T, 2:3], in0=Wd["sink"][0:T, h:h + 1], in1=S.m[0:T, 0:1], op=ALU.subtract), reads=[S.m, Wd["sink"]], writes=[S.m])
        k.op("dve", lambda e: e.tensor_scalar(out=S.m[0:T, 1:2], in0=S.m[0:T, 0:1], scalar1=-1.0, scalar2=None, op0=ALU.mult), reads=[S.m], writes=[S.m])
        k.op("act", lambda e: e.activation(out=S.p[0:T, 0:NKEY], in_=S.sc[0:T, 0:NKEY], func=AF.Exp, bias=S.m[0:T, 1:2], accum_out=S.m[0:T, 3:4]), reads=[S.sc, S.m], writes=[S.p, S.m])
        k.op("act", lambda e: e.activation(out=S.m[0:T, 2:3], in_=S.m[0:T, 2:3], func=AF.Exp), reads=[S.m], writes=[S.m])
        k.op("dve", lambda e: e.tensor_tensor(out=S.m[0:T, 3:4], in0=S.m[0:T, 3:4], in1=S.m[0:T, 2:3], op=ALU.add), reads=[S.m], writes=[S.m])
        k.op("dve", lambda e: e.reciprocal(out=S.m[0:T, 3:4], in_=S.m[0:T, 3:4]), reads=[S.m], writes=[S.m])
        k.op("pe", lambda e, pt_=pt_: e.transpose(pt_[:, 0:T], S.p[0:T, 0:128], C.ident[0:T, 0:T]), reads=[S.p, C.ident], writes=[pt_])
        k.op("pe", lambda e, pt_=pt_: e.transpose(pt_[0:T, 128:128 + T], S.p[0:T, 128:NKEY], C.ident[0:T, 0:T]), reads=[S.p, C.ident], writes=[pt_])
        k.op("act", lambda e, pt_=pt_: e.copy(out=S.pT[:, 0, 0:T], in_=pt_[:, 0:T]), reads=[pt_], writes=[S.pT])
        k.op("act", lambda e, pt_=pt_: e.copy(out=S.pT[0:T, 1, 0:T], in_=pt_[0:T, 128:128 + T]), reads=[pt_], writes=[S.pT])
        oc = (h % 8) * HD
        k.op("pe", lambda e, g=g, oc=oc, po_=po_: e.matmul(po_[0:T, oc:oc + HD], lhsT=S.pT[:, 0, 0:T], rhs=S.vprev[:, g * HD:(g + 1) * HD], start=True, stop=False), reads=[S.pT, S.vprev], writes=[po_])
        k.op("pe", lambda e, g=g, oc=oc, po_=po_: e.matmul(po_[0:T, oc:oc + HD], lhsT=S.pT[0:T, 1, 0:T], rhs=S.vcur[0:T, g * HD:(g + 1) * HD], start=False, stop=True), reads=[S.pT, S.vcur], writes=[po_])
        k.op("dve", lambda e, h=h, oc=oc, po_=po_: e.tensor_scalar(out=S.o_tm[0:T, h * HD:(h + 1) * HD], in0=po_[0:T, oc:oc + HD], scalar1=S.m[0:T, 3:4], scalar2=None, op0=ALU.mult),
             reads=[po_, S.m], writes=[S.o_tm])
    if carry:
        k.op("act", lambda e: e.copy(out=S.kTprev[:, :, :], in_=S.kTcur[:, :, :]), reads=[S.kTcur], writes=[S.kTprev])
        k.op("act", lambda e: e.copy(out=S.vprev[:, :], in_=S.vcur[:, :]), reads=[S.vcur], writes=[S.vprev])
    for b4 in range(4):
        pb = C.bank[1 + b4 % 2]
        for j in range(4):
            c = b4 * 4 + j
            k.op("pe", lambda e, c=c, j=j, pb=pb: e.transpose(pb[:, j * 128:j * 128 + T], S.o_tm[0:T, c * 128:(c + 1) * 128], C.ident[0:T, 0:T]), reads=[S.o_tm, C.ident], writes=[pb])
        k.op("act", lambda e, b4=b4, pb=pb: e.copy(out=S.oT[:, b4 * 4:(b4 + 1) * 4, 0:T], in_=pb[:, :].rearrange("p (a b) -> p a b", a=4)[:, :, 0:T]), reads=[pb], writes=[S.oT])
    for nb in range(4):
        slot = ws.block(Wd["w_o"], 0, nb * 512)
        pb = C.bank[3 + nb % 2]
        for c in range(NCH):
            k.op("pe", lambda e, c=c, pb=pb, slot=slot: e.matmul(pb[0:T, :], lhsT=S.oT[:, c, 0:T], rhs=slot[:, c, :], start=(c == 0), stop=(c == NCH - 1)), reads=[S.oT, slot], writes=[pb])
        k.op("act", lambda e, nb=nb, pb=pb: e.copy(out=S.out_tm[0:T, nb * 512:(nb + 1) * 512], in_=pb[0:T, :]), reads=[pb], writes=[S.out_tm])
    gated_residual_add(k, C, xT, S.out_tm, gate, T)


def attn_scratch(k):
    S = NS()
    S.hT32 = k.sb_once("p_hT32", [128, NCH, 128], F32)
    S.hTb = k.sb_once("p_hTb", [128, NCH, 128], BF16)
    S.out_tm = k.sb_once("p_acc", [128, D], F32)
    S.qT = k.sb("t_qT", [HD, NH, 128], BF16)
    S.kv_tm = k.sb("t_kv", [128, 512], F32)
    S.kTcur = k.sb("t_kTc", [HD, NKV, 128], BF16)
    S.kTprev = k.sb("t_kTp", [HD, NKV, 128], BF16)
    S.vcur = k.sb("t_vc", [128, 256], BF16)
    S.vprev = k.sb("t_vp", [128, 256], BF16)
    S.sc = k.sb("t_sc", [128, 256], F32)
    S.p = k.sb("t_p", [128, 256], F32)
    S.pT = k.sb("t_pT", [128, 2, 128], BF16)
    S.m = k.sb("t_m", [128, 4], F32)
    S.o_tm = k.sb("t_o", [128, D], F32)
    S.oT = k.sb("t_oT", [128, NCH, 128], BF16)
    return S


def ada_layer(k, C, ws, scT, adaw, adab_fm, modv, l, ng):
    for nb in range(24):
        slot = ws.block(adaw, 0, nb * 512)
        pb = C.bank[3 + nb % 2]
        for j in range(4):
            for c in range(NCH):
                k.op("pe", lambda e, j=j, c=c, pb=pb, slot=slot: e.matmul(pb[:, j * 8:j * 8 + ng], lhsT=slot[:, c, j * 128:(j + 1) * 128], rhs=scT[:, c, 0:ng],
                                                                        start=(c == 0), stop=(c == NCH - 1)), reads=[slot, scT], writes=[pb])
        for j in range(4):
            ch = nb * 4 + j
            k.op("dve", lambda e, j=j, ch=ch, pb=pb: e.tensor_scalar(out=modv[:, l, 0:ng, ch], in0=pb[:, j * 8:j * 8 + ng], scalar1=adab_fm[:, ch:ch + 1], scalar2=None, op0=ALU.add),
                 reads=[pb, adab_fm], writes=[modv])


def final_norm_out(k, C, xT, gfin, zero16, S, out_rows, T=128):
    rms_modulate(k, C, xT, gfin, zero16, S.hT32, None, T)
    to_token_major(k, C, S.hT32, S.out_tm, T)
    k.dma("sp", lambda e: e.dma_start(out=out_rows, in_=S.out_tm[0:T, :]), S.out_tm, reads=[S.out_tm])


def peer_q_streamed(k, C, ws, wq_dram, S, T=128):
    for nb in range(4):
        slot = ws.block(wq_dram, 0, nb * 512)
        for j in range(4):
            hc = nb * 4 + j
            pb = C.bank[3 + (hc % 2)]
            for c in range(NCH):
                k.op("pe", lambda e, j=j, c=c, pb=pb, slot=slot: e.matmul(pb[:, 0:T], lhsT=slot[:, c, j * 128:(j + 1) * 128], rhs=S.hTb[:, c, 0:T],
                                                                        start=(c == 0), stop=(c == NCH - 1)), reads=[slot, S.hTb], writes=[pb])
            k.op("act", lambda e, hc=hc, pb=pb: e.copy(out=S.qT[:, hc, 0:T], in_=pb[:, 0:T]), reads=[pb], writes=[S.qT])


NCORES = 4
NTILE = 32
DEPTH = 4
KIND = [0, 1, 2, 0]


def _view(k, arena, p0, p1, a, b, name, shape3=None):
    ap = arena.ap[p0:p1, a:b]
    if shape3 is not None:
        ap = ap.rearrange("p (a b) -> p a b", a=shape3[0])
    return k.view(ap, name)


def build_nc(ntile=NTILE, nsamp=2):
    nc = bass.Bass("TRN2", target_bir_lowering=False)

    def din(n, s, dt=F32):
        return nc.dram_tensor(n, list(s), dt, kind="ExternalInput").ap()

    def dout(n, s, dt=F32):
        return nc.dram_tensor(n, list(s), dt, kind="ExternalOutput").ap()

    NG = 1 + nsamp
    xp_d = din("xp_fm", [ntile, 128, 16, 128]); xs_d = din("xs_fm", [nsamp, 128, 16, 128]); cT_d = din("cT", [128, 16, NG])
    ck_d = din("cache_k", [nsamp, 128, 256]); cv_d = din("cache_v", [nsamp, 128, 256]); st_d = din("state_conv", [nsamp, 30, 2048])
    gmix_d = din("g_mix_fm", [128, DEPTH, 16]); gch_d = din("g_ch_fm", [128, DEPTH, 16]); gfin_d = din("g_fin_fm", [128, 16])
    adaw_d = [din(f"ada_w{l}", [2048, 12288]) for l in range(DEPTH)]; adab_d = din("ada_b_fm", [128, DEPTH, 96])
    sgu_win_d = [din(f"sgu_w_in{i}", [2048, 12288]) for i in range(2)]; sgu_wout_d = [din(f"sgu_w_out{i}", [6144, 2048]) for i in range(2)]
    sgu_bin_d = din("sgu_b_in", [2, 12288]); sgu_lng_d = din("sgu_ln_g", [2, 6144]); sgu_lnb_d = din("sgu_ln_b", [2, 6144])
    wsT_d = din("sgu_wsT", [128, 2, 8, 128]); bs_d = din("sgu_bs_tm", [128, 2, 8])
    cwin_d = din("conv_w_in", [2048, 4096]); cwout_d = din("conv_w_out", [2048, 2048]); cbout_d = din("conv_b_out", [2048])
    cbin_d = din("conv_b_in_fm", [128, 32]); cdw_d = din("conv_dwT", [128, 16, 31]); cdwb_d = din("conv_dw_b_fm", [128, 16])
    clng_d = din("conv_ln_g_fm", [128, 16]); clnb_d = din("conv_ln_b_fm", [128, 16])
    wqkv_d = din("attn_w_qkv", [2048, 2560]); bqkv_d = din("attn_b_qkv", [2560]); bq_d = din("attn_bq_fm", [64, 32]); sink_d = din("attn_sinks", [32]); wo_d = din("attn_w_o", [2048, 2048])
    pwq_d = [din(f"peer_w_q{l}", [2048, 2048]) for l in range(DEPTH)]; skT_d = din("peer_skT", [128, DEPTH, 16, 128])
    pu_d = [din(f"peer_u{l}", [16384, 2048]) for l in range(DEPTH)]; pv_d = [din(f"peer_v{l}", [16384, 2048]) for l in range(DEPTH)]
    y_p = dout("y_p", [ntile * 128, 2048]); y_s = dout("y_s", [nsamp, 16, 2048]); cs_p = dout("cs_p", [30, 2048]); kw_p = dout("kw_p", [128, 256]); vw_p = dout("vw_p", [128, 256])
    cs_s = dout("cs_s", [nsamp, 30, 2048]); kw_s = dout("kw_s", [nsamp, 128, 256]); vw_s = dout("vw_s", [nsamp, 128, 256]); sv_s = dout("sv_s", [2, nsamp, 16, 6144])

    with ExitStack() as st:
        k = K(nc, st)
        C = Common(k)
        A = attn_consts(k)
        def load(name, shape, src, dt=F32, q="sp"):
            b = k.sb(name, shape, dt)
            k.dma(q, lambda e, b=b, src=src: e.dma_start(out=b[:], in_=src), b, writes=[b])
            return b
        cT = load("cT_sb", [128, 16, NG], cT_d)
        gmix = load("gmix_sb", [128, DEPTH, 16], gmix_d); gch = load("gch_sb", [128, DEPTH, 16], gch_d); gfin = load("gfin_sb", [128, 16], gfin_d)
        adab = load("adab_sb", [128, DEPTH, 96], adab_d)
        wsT = load("wsT_sb", [128, 2, 8, 128], wsT_d, BF16, q="pool")
        bs = load("bs_sb", [128, 2, 8], bs_d)
        cbin = load("cbin_sb", [128, 32], cbin_d); cdw = load("cdw_sb", [128, 16, 31], cdw_d); cdwb = load("cdwb_sb", [128, 16], cdwb_d)
        clng = load("clng_sb", [128, 16], clng_d); clnb = load("clnb_sb", [128, 16], clnb_d)
        bq = load("bq_sb", [64, 32], bq_d)
        sink = k.sb("sink_sb", [128, 32], F32)
        k.dma("sp", lambda e: e.dma_start(out=sink[:], in_=sink_d.unsqueeze(0).partition_broadcast(128)), sink, writes=[sink])
        zero16 = k.sb("zero16", [128, 16], F32)
        k.op("dve", lambda e: e.memset(zero16[:], 0.0), writes=[zero16])
        k.op("dve", lambda e: e.memset(wsT[64:128, :, :, 0:64], 0.0), writes=[wsT])
        xT = k.sb("xT", [128, 16, 128], F32)
        hT32 = k.sb_once("p_hT32", [128, NCH, 128], F32); hTb = k.sb_once("p_hTb", [128, NCH, 128], BF16); acc = k.sb_once("p_acc", [128, D], F32)
        NF, NB = 10240, 18432
        AFa = k.sb("arena_f32", [128, NF], F32); ABa = k.sb("arena_bf16", [128, NB], BF16)
        skslot = k.sb("skslot", [128, 16, 128], BF16)
        SG = NS(); SG.hT32, SG.hTb, SG.out_tm = hT32, hTb, acc
        SG.v = _view(k, AFa, 0, 128, 0, 6144, "g_v"); SG.prod = SG.v
        SG.u = _view(k, ABa, 0, 128, 0, 6144, "g_u"); SG.vb = _view(k, ABa, 0, 128, 6144, 12288, "g_vb")
        SG.prodT = _view(k, ABa, 0, 128, 12288, 18432, "g_prodT", (48, 128)); SG.st = k.sb("g_st", [128, 4], F32)
        SG.tmp = _view(k, AFa, 0, 128, 6144, 6656, "g_tmp")
        CV = NS(); CV.hT32, CV.hTb, CV.out_tm = hT32, hTb, acc
        CV.xp = k.sb("c_xp", [128, NCH, CW - 1 + 128], F32); CV.hist_tmp = k.sb("c_hist", [128, NCH, CW - 1], F32)
        CV.y = _view(k, AFa, 0, 128, 0, 2048, "c_y", (16, 128)); CV.tm30 = _view(k, AFa, 0, 32, 2048, 4096, "c_tm30")
        CV.sg = _view(k, AFa, 0, 128, 4096, 4224, "c_sg"); CV.mv = _view(k, AFa, 0, 128, 4224, 4608, "c_mv")
        CV.yb = _view(k, ABa, 0, 128, 0, 2048, "c_yb", (16, 128)); CV.y2b = _view(k, ABa, 0, 128, 2048, 4096, "c_y2b", (16, 128))
        AT = NS(); AT.hT32, AT.hTb, AT.out_tm = hT32, hTb, acc
        AT.kTprev = k.sb("t_kTp", [HD, NKV, 128], BF16); AT.vprev = k.sb("t_vp", [128, 256], BF16); AT.m = k.sb("t_m", [128, 4], F32)
        AT.o_tm = _view(k, AFa, 0, 128, 0, 2048, "t_o"); AT.kv_tm = _view(k, AFa, 0, 128, 2048, 2560, "t_kv")
        AT.sc = _view(k, AFa, 0, 128, 2560, 2816, "t_sc"); AT.p = _view(k, AFa, 0, 128, 2816, 3072, "t_p")
        AT.qT = _view(k, ABa, 0, HD, 0, 4096, "t_qT", (NH, 128)); AT.kTcur = _view(k, ABa, 0, HD, 4096, 4608, "t_kTc", (NKV, 128))
        AT.vcur = _view(k, ABa, 0, 128, 4608, 4864, "t_vc"); AT.pT = _view(k, ABa, 0, 128, 4864, 5120, "t_pT", (2, 128))
        AT.oT = _view(k, ABa, 0, 128, 5120, 7168, "t_oT", (16, 128))
        PE = NS(); PE.hT32, PE.hTb, PE.acc = hT32, hTb, acc
        PE.h_tm = _view(k, AFa, 0, 128, 0, 2048, "p_h_tm")
        PE.s = _view(k, AFa, 0, 128, 2048, 4096, "p_s", (NHC, PK)); PE.s2 = _view(k, AFa, 0, 128, 4096, 6144, "p_s2", (NHC, PK))
        PE.cand = k.view(AFa.ap[:, 6144:8192].rearrange("p (h a b) -> p h a b", h=PH, a=16), "p_cand")
        PE.cand2 = k.view(AFa.ap[:, 8192:10240].rearrange("p (h a b) -> p h a b", h=PH, a=16), "p_cand2")
        PE.gbuf = [_view(k, AFa, 0, 128, 2048 * (i + 1), 2048 * (i + 2), f"p_gb{i}") for i in range(3)]
        PE.junk = _view(k, AFa, 0, 128, 8192, 10240, "p_junk")
        PE.qT = _view(k, ABa, 0, 128, 0, 2048, "p_qT", (NHC, 128))
        for n_, shp, dt_ in (("sv", [128, NHC, 16], F32), ("si", [128, NHC, 16], U32), ("sif", [128, NHC, 16], F32), ("best", [128, PH, 16], F32), ("bi", [128, PH, 16], U32),
                             ("b1", [128, PH, 16], U32), ("b2", [128, PH, 16], U32), ("b1f", [128, PH, 16], F32), ("b2f", [128, PH, 16], F32), ("oh", [128, 16, 16], F32),
                             ("I1", [128, PH, 16], F32), ("I2", [128, PH, 16], F32), ("ef", [128, PH, 16], F32), ("ei", [128, PH, 16], I32), ("g", [128, PH, 16], F32),
                             ("gs", [128, PH], F32), ("a", [128, PH, 16], F32), ("w", [128, PH, 16], F32)):
            setattr(PE, n_, k.sb("p_" + n_, shp, dt_))
        scT = k.sb("scT", [128, 16, NG], BF16)
        modv = k.sb("modv", [128, DEPTH, NG, 96], F32)
        der = k.sb("der", [128, DEPTH, NG, 2, 16], F32)
        k.sb_once("rm_sq", [128, NCH, 128], BF16); k.sb_once("rm_rstd", [128, 128], F32)
        left = nc.sbuf_bytes_remaining - 4 * 2048 - 1024
        nsl = max(1, min(3, left // 16384))
        print("[kernel] sbuf left before weight slots:", nc.sbuf_bytes_remaining, "-> slots:", nsl, flush=True)
        assert left >= 16384, "no room for a weight slot"
        ws = WS(k, nslots=nsl, q="pool")
        k.op("act", lambda e: e.activation(out=scT[:], in_=cT[:], func=AF.Silu), reads=[cT], writes=[scT])
        for l in range(DEPTH):
            adab_l = k.view(adab.ap[:, l, :], f"adab{l}")
            k.barrier()
            ada_layer(k, C, ws, scT, adaw_d[l], adab_l, modv, l, NG)
        for l in range(DEPTH):
            for g in range(NG):
                for (j, off, gn) in ((0, 16, gmix), (1, 64, gch)):
                    k.op("dve", lambda e, l=l, g=g, j=j, off=off: e.tensor_scalar(out=der[:, l, g, j, :], in0=modv[:, l, g, off:off + 16], scalar1=1.0, scalar2=None, op0=ALU.add),
                         reads=[modv], writes=[der])
                    k.op("dve", lambda e, l=l, g=g, j=j, gn=gn: e.tensor_tensor(out=der[:, l, g, j, :], in0=der[:, l, g, j, :], in1=gn[:, l, :], op=ALU.mult), reads=[der, gn], writes=[der])
        k.barrier()

        def vec(ap, name):
            return k.view(ap, name)

        def mods(l, g):
            return dict(gm1=vec(der.ap[:, l, g, 0, :], "gm1"), sh1=vec(modv.ap[:, l, g, 0:16], "sh1"), g1=vec(modv.ap[:, l, g, 32:48], "g1"),
                        gm2=vec(der.ap[:, l, g, 1, :], "gm2"), sh2=vec(modv.ap[:, l, g, 48:64], "sh2"), g2=vec(modv.ap[:, l, g, 80:96], "g2"))
        MODS = [[mods(l, g) for g in range(NG)] for l in range(DEPTH)]
        SGW = [dict(w_in=sgu_win_d[i], b_in=sgu_bin_d[i], ln_g=sgu_lng_d[i], ln_b=sgu_lnb_d[i], w_out=sgu_wout_d[i],
                    wsT=vec(wsT.ap[:, i], f"wsT{i}"), bs=vec(bs.ap[:, i], f"bs{i}")) for i in range(2)]
        CVW = dict(w_in=cwin_d, w_out=cwout_d, b_out=cbout_d, b_in=cbin, dw=cdw, dw_b=cdwb, ln_g=clng, ln_b=clnb)
        ATW = dict(w_qkv=wqkv_d, b_qkv=bqkv_d, w_o=wo_d, bq=bq, sink=sink)

        def run_tile(grp, T, x_src, first, last, si):
            k.dma("sp", lambda e: e.dma_start(out=xT[:], in_=x_src), xT, writes=[xT])
            isgu = 0
            for l in range(DEPTH):
                M = MODS[l][grp]
                k.barrier()
                if KIND[l] == 0:
                    vout = sv_s[isgu, si] if si is not None else None
                    sgu(k, C, ws, xT, M["gm1"], M["sh1"], M["g1"], SGW[isgu], SG, T=T, v_out=vout)
                    isgu += 1
                elif KIND[l] == 1:
                    if si is not None:
                        conv_load_hist_state(k, C, CV, st_d[si])
                    elif first:
                        conv_load_hist_zero(k, CV)
                    so = cs_s[si] if si is not None else (cs_p if last else None)
                    conv(k, C, ws, xT, M["gm1"], M["sh1"], M["g1"], CVW, CV, T=T, state_out=so, carry=(si is None))
                else:
                    if si is not None:
                        attn_load_prev(k, C, AT, ck_d[si], cv_d[si])
                        cpy = k.view(kw_s[si], f"kws_cpy{si}")
                        k.dma("sp", lambda e, si=si: e.dma_start(out=kw_s[si, 0:112, :], in_=ck_d[si, 16:128, :]), cpy, writes=[cpy])
                        k.dma("sp", lambda e, si=si: e.dma_start(out=vw_s[si, 0:112, :], in_=cv_d[si, 16:128, :]), cpy, writes=[cpy])
                        attn(k, C, ws, xT, M["gm1"], M["sh1"], M["g1"], ATW, AT, A.Dsamp, T=T, kwin_out=kw_s[si], vwin_out=vw_s[si], win_rows=(112, 16), carry=False)
                    else:
                        if first:
                            k.op("dve", lambda e: e.memset(AT.kTprev[:, :, :], 0.0), writes=[AT.kTprev])
                            k.op("dve", lambda e: e.memset(AT.vprev[:, :], 0.0), writes=[AT.vprev])
                        attn(k, C, ws, xT, M["gm1"], M["sh1"], M["g1"], ATW, AT, A.Dfirst if first else A.Dmid, T=T,
                             kwin_out=kw_p if last else None, vwin_out=vw_p if last else None, win_rows=(0, 128) if last else None)
                k.barrier()
                k.dma("pool", lambda e, l=l: e.dma_start(out=skslot[:], in_=skT_d[:, l]), skslot, writes=[skslot])
                peer(k, C, xT, M["gm2"], M["sh2"], M["g2"], PeerW(k, pwq_d[l], skslot, pu_d[l], pv_d[l]), PE, T=128, ws=ws, mid_barrier=True)
            k.barrier()
            return

        FN = NS(); FN.hT32, FN.out_tm = hT32, acc
        for t in range(ntile):
            run_tile(0, 128, xp_d[t], t == 0, t == ntile - 1, None)
            final_norm_out(k, C, xT, gfin, zero16, FN, y_p[t * 128:(t + 1) * 128, :], T=128)
        for si in range(nsamp):
            run_tile(1 + si, 16, xs_d[si], False, False, si)
            final_norm_out(k, C, xT, gfin, zero16, FN, y_s[si], T=16)
        k.finish()
        print("[kernel] instructions:", k.n, "sbuf bytes left:", nc.sbuf_bytes_remaining, "weight slots:", nsl, flush=True)
    return nc


def _fm(v):
    v = np.asarray(v)
    lead = v.shape[:-1]
    return np.ascontiguousarray(np.moveaxis(v.reshape(*lead, -1, 128), -1, 0))


def make_in_maps(I, ncores, ntile, nsamp):
    f32 = lambda a: np.ascontiguousarray(np.asarray(a), dtype=np.float32)
    shared = {}
    shared["g_mix_fm"] = _fm(I["norm_mix_g"]); shared["g_ch_fm"] = _fm(I["norm_ch_g"]); shared["g_fin_fm"] = _fm(I["norm_final_g"])
    shared["ada_b_fm"] = _fm(I["ada_b"])
    for l in range(DEPTH):
        shared[f"ada_w{l}"] = f32(I["ada_w"][l]); shared[f"peer_w_q{l}"] = f32(I["peer_w_q"][l])
        shared[f"peer_u{l}"] = f32(I["peer_u"][l]); shared[f"peer_v{l}"] = f32(I["peer_v"][l])
    for i in range(2):
        shared[f"sgu_w_in{i}"] = f32(I["sgu_w_in"][i]); shared[f"sgu_w_out{i}"] = f32(I["sgu_w_out"][i])
    shared["sgu_b_in"] = f32(I["sgu_b_in"]); shared["sgu_ln_g"] = f32(I["sgu_ln_g"]); shared["sgu_ln_b"] = f32(I["sgu_ln_b"])
    shared["sgu_wsT"] = np.ascontiguousarray(np.asarray(I["sgu_w_s"]).transpose(3, 0, 1, 2))
    shared["sgu_bs_tm"] = np.ascontiguousarray(np.asarray(I["sgu_b_s"]).transpose(2, 0, 1))
    shared["conv_w_in"] = f32(I["conv_w_in"][0]); shared["conv_w_out"] = f32(I["conv_w_out"][0]); shared["conv_b_out"] = f32(I["conv_b_out"][0])
    shared["conv_b_in_fm"] = _fm(I["conv_b_in"][0]); shared["conv_dw_b_fm"] = _fm(I["conv_dw_b"][0])
    shared["conv_ln_g_fm"] = _fm(I["conv_ln_g"][0]); shared["conv_ln_b_fm"] = _fm(I["conv_ln_b"][0])
    shared["conv_dwT"] = np.ascontiguousarray(np.asarray(I["conv_dw"][0]).T.reshape(16, 128, 31).transpose(1, 0, 2))
    shared["attn_w_qkv"] = f32(I["attn_w_qkv"][0]); shared["attn_b_qkv"] = f32(I["attn_b_qkv"][0]); shared["attn_w_o"] = f32(I["attn_w_o"][0])
    shared["attn_bq_fm"] = np.ascontiguousarray(np.asarray(I["attn_b_qkv"][0][:2048]).reshape(32, 64).T); shared["attn_sinks"] = f32(I["attn_sinks"][0])
    shared["peer_skT"] = np.ascontiguousarray(np.asarray(I["peer_subkeys"]).reshape(DEPTH, 16, 128, 128).transpose(3, 0, 1, 2))
    xp = np.asarray(I["x_prompt"]); xs = np.asarray(I["x_sample"])
    in_maps = []
    for c in range(ncores):
        m = dict(shared)
        m["xp_fm"] = np.ascontiguousarray(xp[c].reshape(ntile, 128, 16, 128).transpose(0, 3, 2, 1))
        xsp = np.zeros((nsamp, 128, 2048), np.float32); xsp[:, :16] = xs[nsamp * c:nsamp * c + nsamp]
        m["xs_fm"] = np.ascontiguousarray(xsp.reshape(nsamp, 128, 16, 128).transpose(0, 3, 2, 1))
        cc = np.stack([np.asarray(I["c_prompt"])[c]] + [np.asarray(I["c_sample"])[nsamp * c + i] for i in range(nsamp)])
        m["cT"] = np.ascontiguousarray(cc.reshape(1 + nsamp, 16, 128).transpose(2, 1, 0))
        m["cache_k"] = f32(np.asarray(I["cache_k_win"])[0, nsamp * c:nsamp * c + nsamp].reshape(nsamp, 128, 256))
        m["cache_v"] = f32(np.asarray(I["cache_v_win"])[0, nsamp * c:nsamp * c + nsamp].reshape(nsamp, 128, 256))
        m["state_conv"] = f32(np.asarray(I["state_conv"])[0, nsamp * c:nsamp * c + nsamp])
        in_maps.append(m)
    return in_maps


def kernel(**I):
    in_maps = make_in_maps(I, NCORES, NTILE, 2)
    nc = build_nc()
    res = run_bass_kernel_spmd(nc, in_maps, core_ids=list(range(NCORES))).results
    R = lambda key: np.stack([np.asarray(r[key]) for r in res])
    y_prompt = R("y_p").reshape(4, 4096, 2048)
    y_sample = R("y_s").reshape(8, 16, 2048)
    conv_state_prompt = R("cs_p").reshape(1, 4, 30, 2048)
    k_win_prompt = R("kw_p").reshape(1, 4, 128, 4, 64); v_win_prompt = R("vw_p").reshape(1, 4, 128, 4, 64)
    conv_state_sample = R("cs_s").reshape(1, 8, 30, 2048)
    k_win_sample = R("kw_s").reshape(1, 8, 128, 4, 64); v_win_sample = R("vw_s").reshape(1, 8, 128, 4, 64)
    sgu_v_sample = np.ascontiguousarray(R("sv_s").transpose(1, 0, 2, 3, 4)).reshape(2, 8, 16, 6144)
    return tuple(np.ascontiguousarray(a, dtype=np.float32) for a in (y_prompt, y_sample, conv_state_prompt, k_win_prompt, v_win_prompt,
                                                                     conv_state_sample, k_win_sample, v_win_sample, sgu_v_sample))
```

```python
import numpy as np
from contextlib import ExitStack
import concourse.bass as bass
import concourse.mybir as mybir
from concourse.bass_utils import run_bass_kernel_spmd

F32 = mybir.dt.float32
BF16 = mybir.dt.bfloat16
I32 = mybir.dt.int32
U32 = mybir.dt.uint32
ALU = mybir.AluOpType
AF = mybir.ActivationFunctionType
AX = mybir.AxisListType


class Ctr:
    LIM = 30000

    def __init__(self, K, name, step):
        self.K, self.name, self.step = K, name, step
        self.sems = []
        self._new()

    def _new(self):
        s = self.K.stack.enter_context(self.K.nc.semaphore(f"{self.name}_{len(self.sems)}"))
        self.sems.append(s)
        self.val = 0

    def next(self):
        if self.val + self.step > self.LIM:
            self._new()
        self.val += self.step
        return (self.sems[-1], self.val)


class Buf:
    def __init__(self, K, ap, name):
        self.K, self.ap, self.name = K, ap, name
        self.w = None
        self.r = {}
        self._d = None

    @property
    def dctr(self):
        if self._d is None:
            self._d = Ctr(self.K, "d_" + self.name, 16)
        return self._d

    def __getitem__(self, k):
        return self.ap[k]


class Eng:
    def __init__(self, K, name, obj, counted):
        self.name, self.obj = name, obj
        self.ctr = Ctr(K, "e_" + name, 1) if counted else None
        self.known = {}
        self.prog = []

    def filt(self, deps):
        best = {}
        for (s, v) in deps:
            if self.name == "pe" and self.ctr is not None and s in self.ctr.sems:
                continue
            if self.known.get(s, 0) >= v:
                continue
            if best.get(s, 0) < v:
                best[s] = v
        for s, v in best.items():
            self.known[s] = v
        return list(best.items())


class K:
    def __init__(self, nc, stack):
        self.nc, self.stack = nc, stack
        self.eng = {
            "pe": Eng(self, "pe", nc.tensor, True),
            "act": Eng(self, "act", nc.scalar, True),
            "dve": Eng(self, "dve", nc.vector, True),
            "pool": Eng(self, "pool", nc.gpsimd, True),
            "sp": Eng(self, "sp", nc.sync, False),
        }
        self.bufs = []
        self.n = 0

    def sb(self, name, shape, dt):
        t = self.stack.enter_context(self.nc.sbuf_tensor(name, list(shape), dt))
        b = Buf(self, t, name)
        self.bufs.append(b)
        return b

    def sb_once(self, name, shape, dt):
        if not hasattr(self, "_once"):
            self._once = {}
        if name not in self._once:
            self._once[name] = self.sb(name, shape, dt)
        return self._once[name]

    def ps(self, name, shape, dt):
        t = self.stack.enter_context(self.nc.psum_tensor(name, list(shape), dt))
        b = Buf(self, t, name)
        self.bufs.append(b)
        return b

    def view(self, ap, name):
        b = Buf(self, ap, name)
        self.bufs.append(b)
        return b

    def _deps(self, reads, writes):
        deps = []
        for b in reads:
            if b.w:
                deps.append(b.w)
        for b in writes:
            if b.w:
                deps.append(b.w)
            deps.extend(b.r.items())
        return deps

    def _commit(self, tok, reads, writes):
        for b in reads:
            if b.r.get(tok[0], 0) < tok[1]:
                b.r[tok[0]] = tok[1]
        for b in writes:
            b.w = tok
            b.r = {}

    def op(self, eng, fn, reads=(), writes=()):
        E = self.eng[eng]
        waits = E.filt(self._deps(reads, writes))
        tok = E.ctr.next()
        E.prog.append((waits, fn, tok, 1))
        self._commit(tok, reads, writes)
        self.n += 1

    def dma(self, q, fn, cbuf, reads=(), writes=()):
        E = self.eng[q]
        waits = E.filt(self._deps(reads, writes))
        tok = cbuf.dctr.next()
        E.prog.append((waits, fn, tok, 16))
        self._commit(tok, reads, writes)
        self.n += 1

    def barrier(self):
        deps = []
        for b in self.bufs:
            if b.w:
                deps.append(b.w)
            deps.extend(b.r.items())
        for E in self.eng.values():
            waits = E.filt(deps)
            if waits:
                E.prog.append((waits, None, None, 0))
        for b in self.bufs:
            b.w = None
            b.r = {}

    def finish(self):
        E = self.eng["sp"]
        deps = []
        for b in self.bufs:
            if b.w:
                deps.append(b.w)
            deps.extend(b.r.items())
        waits = E.filt(deps)
        E.prog.append((waits, None, None, 0))
        nc = self.nc

        def replay(name, e):
            for (waits, fn, tok, inc) in self.eng[name].prog:
                for (s, v) in waits:
                    e.wait_ge(s, v)
                if fn is not None:
                    ins = fn(e)
                    ins.then_inc(tok[0], inc)

        with nc.Block() as block:
            @block.sync
            def _(e):
                replay("sp", e)

            @block.tensor
            def _(e):
                replay("pe", e)

            @block.scalar
            def _(e):
                replay("act", e)

            @block.vector
            def _(e):
                replay("dve", e)

            @block.gpsimd
            def _(e):
                replay("pool", e)


D = 2048
NCH = 16
PH, PK, PTOP = 8, 128, 16
NHC = 16
NEG = -1e30


class Common:
    def __init__(self, k):
        self.k = k
        identi = k.sb("identi", [128, 128], I32)
        self.ident = k.sb("ident", [128, 128], F32)
        k.op("pool", lambda e: e.iota(identi[:], pattern=[[1, 128]], base=0, channel_multiplier=-1), writes=[identi])
        k.op("dve", lambda e: e.tensor_single_scalar(out=self.ident[:], in_=identi[:], scalar=0, op=ALU.is_equal),
             reads=[identi], writes=[self.ident])
        self.ones_bf = k.sb("ones_bf", [128, 128], BF16)
        k.op("dve", lambda e: e.memset(self.ones_bf[:], 1.0), writes=[self.ones_bf])
        io16 = k.sb("io16i", [128, 16], I32)
        self.iota16 = k.sb("iota16", [128, 16], F32)
        k.op("pool", lambda e: e.iota(io16[:], pattern=[[1, 16]], base=0, channel_multiplier=0), writes=[io16])
        k.op("dve", lambda e: e.tensor_copy(out=self.iota16[:], in_=io16[:]), reads=[io16], writes=[self.iota16])
        self.bank = [k.ps(f"bank{i}", [128, 512], F32) for i in range(8)]


def rms_modulate(k, C, xT, gm, sh, hT32, hTb, T=128):
    sq = k.sb_once("rm_sq", [128, NCH, 128], BF16)
    rs = k.sb_once("rm_rstd", [128, 128], F32)
    pb = C.bank[0]
    k.op("act", lambda e: e.activation(out=sq[:, :, 0:T], in_=xT[:, :, 0:T], func=AF.Square), reads=[xT], writes=[sq])
    for c in range(NCH):
        k.op("pe", lambda e, c=c: e.matmul(pb[:, 0:T], lhsT=C.ones_bf[:], rhs=sq[:, c, 0:T], start=(c == 0), stop=(c == NCH - 1)),
             reads=[sq, C.ones_bf], writes=[pb])
    k.op("dve", lambda e: e.tensor_scalar(out=rs[:, 0:T], in0=pb[:, 0:T], scalar1=1.0 / D, scalar2=1e-6, op0=ALU.mult, op1=ALU.add),
         reads=[pb], writes=[rs])
    k.op("act", lambda e: e.activation(out=rs[:, 0:T], in_=rs[:, 0:T], func=AF.Sqrt), reads=[rs], writes=[rs])
    k.op("dve", lambda e: e.reciprocal(out=rs[:, 0:T], in_=rs[:, 0:T]), reads=[rs], writes=[rs])
    k.op("dve", lambda e: e.tensor_tensor(out=hT32[:, :, 0:T], in0=xT[:, :, 0:T],
                                          in1=rs[:, 0:T].unsqueeze(1).to_broadcast([128, NCH, T]), op=ALU.mult),
         reads=[xT, rs], writes=[hT32])
    k.op("dve", lambda e: e.tensor_tensor(out=hT32[:, :, 0:T], in0=hT32[:, :, 0:T],
                                          in1=gm[:, :].unsqueeze(2).to_broadcast([128, NCH, T]), op=ALU.mult),
         reads=[hT32, gm], writes=[hT32])
    k.op("dve", lambda e: e.tensor_tensor(out=hT32[:, :, 0:T], in0=hT32[:, :, 0:T],
                                          in1=sh[:, :].unsqueeze(2).to_broadcast([128, NCH, T]), op=ALU.add),
         reads=[hT32, sh], writes=[hT32])
    if hTb is not None:
        k.op("act", lambda e: e.copy(out=hTb[:, :, 0:T], in_=hT32[:, :, 0:T]), reads=[hT32], writes=[hTb])


def to_token_major(k, C, srcT, dst, T=128):
    for b4 in range(4):
        pb = C.bank[1 + (b4 % 2)]
        for j in range(4):
            c = b4 * 4 + j
            k.op("pe", lambda e, c=c, j=j, pb=pb: e.transpose(pb[0:T, j * 128:(j + 1) * 128], srcT[:, c, 0:T], C.ident[:]),
                 reads=[srcT, C.ident], writes=[pb])
        k.op("act", lambda e, b4=b4, pb=pb: e.copy(out=dst[0:T, b4 * 512:(b4 + 1) * 512], in_=pb[0:T, :]), reads=[pb], writes=[dst])


def gated_residual_add(k, C, xT, out_tm, gate, T=128):
    for b4 in range(4):
        pb = C.bank[1 + (b4 % 2)]
        for j in range(4):
            c = b4 * 4 + j
            k.op("pe", lambda e, c=c, j=j, pb=pb: e.transpose(pb[:, j * 128:j * 128 + T], out_tm[0:T, c * 128:(c + 1) * 128], C.ident[0:T, 0:T]),
                 reads=[out_tm, C.ident], writes=[pb])
        for j in range(4):
            c = b4 * 4 + j
            k.op("dve", lambda e, c=c, j=j, pb=pb: e.scalar_tensor_tensor(out=xT[:, c, 0:T], in0=pb[:, j * 128:j * 128 + T], scalar=gate[:, c:c + 1],
                                                                          in1=xT[:, c, 0:T], op0=ALU.mult, op1=ALU.add),
                 reads=[pb, gate, xT], writes=[xT])


class PeerW:
    def __init__(self, k, wq_b, skT_b, u_tab, v_tab):
        self.wq, self.skT, self.u, self.v = wq_b, skT_b, u_tab, v_tab


def peer(k, C, xT, gm, sh, gate, W, S, T=128, ws=None, mid_barrier=False):
    rms_modulate(k, C, xT, gm, sh, S.hT32, S.hTb, T)
    to_token_major(k, C, S.hT32, S.h_tm, T)
    if ws is not None:
        peer_q_streamed(k, C, ws, W.wq, S, T)
    else:
        for hc in range(NHC):
            pb = C.bank[3 + (hc % 2)]
            for c in range(NCH):
                k.op("pe", lambda e, hc=hc, c=c, pb=pb: e.matmul(pb[:, 0:T], lhsT=W.wq[:, c, hc * 128:(hc + 1) * 128], rhs=S.hTb[:, c, 0:T],
                                                                start=(c == 0), stop=(c == NCH - 1)), reads=[W.wq, S.hTb], writes=[pb])
            k.op("act", lambda e, hc=hc, pb=pb: e.copy(out=S.qT[:, hc, 0:T], in_=pb[:, 0:T]), reads=[pb], writes=[S.qT])
    for hc in range(NHC):
        pb = C.bank[5 + (hc // 4) % 2]
        k.op("pe", lambda e, hc=hc, pb=pb: e.matmul(pb[0:T, (hc % 4) * 128:(hc % 4 + 1) * 128], lhsT=S.qT[:, hc, 0:T], rhs=W.skT[:, hc, :], start=True, stop=True),
             reads=[S.qT, W.skT], writes=[pb])
        if hc % 4 == 3:
            k.op("act", lambda e, hc=hc, pb=pb: e.copy(out=S.s[0:T, hc - 3:hc + 1, :], in_=pb[0:T, :].rearrange("p (a b) -> p a b", a=4)), reads=[pb], writes=[S.s])
    for hc in range(NHC):
        k.op("dve", lambda e, hc=hc: e.max(out=S.sv[0:T, hc, 0:8], in_=S.s[0:T, hc, :]), reads=[S.s], writes=[S.sv])
        k.op("dve", lambda e, hc=hc: e.max_index(out=S.si[0:T, hc, 0:8], in_max=S.sv[0:T, hc, 0:8], in_values=S.s[0:T, hc, :]), reads=[S.s, S.sv], writes=[S.si])
        k.op("dve", lambda e, hc=hc: e.match_replace(out=S.s2[0:T, hc, :], in_to_replace=S.sv[0:T, hc, 0:8], in_values=S.s[0:T, hc, :], imm_value=NEG), reads=[S.s, S.sv], writes=[S.s2])
        k.op("dve", lambda e, hc=hc: e.max(out=S.sv[0:T, hc, 8:16], in_=S.s2[0:T, hc, :]), reads=[S.s2], writes=[S.sv])
        k.op("dve", lambda e, hc=hc: e.max_index(out=S.si[0:T, hc, 8:16], in_max=S.sv[0:T, hc, 8:16], in_values=S.s2[0:T, hc, :]), reads=[S.s2, S.sv], writes=[S.si])
    k.op("dve", lambda e: e.tensor_copy(out=S.sif[0:T], in_=S.si[0:T]), reads=[S.si], writes=[S.sif])
    sv4 = S.sv.ap.rearrange("p (h c) j -> p h c j", c=2)
    sif4 = S.sif.ap.rearrange("p (h c) j -> p h c j", c=2)
    for h in range(PH):
        k.op("dve", lambda e, h=h: e.tensor_tensor(out=S.cand[0:T, h, :, :], in0=sv4[0:T, h, 0, :].unsqueeze(2).to_broadcast([T, 16, 16]),
                                                   in1=sv4[0:T, h, 1, :].unsqueeze(1).to_broadcast([T, 16, 16]), op=ALU.add), reads=[S.sv], writes=[S.cand])
    candf = S.cand.ap.rearrange("p h a b -> p h (a b)")
    cand2f = S.cand2.ap.rearrange("p h a b -> p h (a b)")
    for h in range(PH):
        k.op("dve", lambda e, h=h: e.max(out=S.best[0:T, h, 0:8], in_=candf[0:T, h, :]), reads=[S.cand], writes=[S.best])
        k.op("dve", lambda e, h=h: e.max_index(out=S.bi[0:T, h, 0:8], in_max=S.best[0:T, h, 0:8], in_values=candf[0:T, h, :]), reads=[S.cand, S.best], writes=[S.bi])
        k.op("dve", lambda e, h=h: e.match_replace(out=cand2f[0:T, h, :], in_to_replace=S.best[0:T, h, 0:8], in_values=candf[0:T, h, :], imm_value=NEG), reads=[S.cand, S.best], writes=[S.cand2])
        k.op("dve", lambda e, h=h: e.max(out=S.best[0:T, h, 8:16], in_=cand2f[0:T, h, :]), reads=[S.cand2], writes=[S.best])
        k.op("dve", lambda e, h=h: e.max_index(out=S.bi[0:T, h, 8:16], in_max=S.best[0:T, h, 8:16], in_values=cand2f[0:T, h, :]), reads=[S.cand2, S.best], writes=[S.bi])
    k.op("dve", lambda e: e.tensor_single_scalar(out=S.b1[0:T], in_=S.bi[0:T], scalar=4, op=ALU.logical_shift_right), reads=[S.bi], writes=[S.b1])
    k.op("dve", lambda e: e.tensor_single_scalar(out=S.b2[0:T], in_=S.bi[0:T], scalar=15, op=ALU.bitwise_and), reads=[S.bi], writes=[S.b2])
    k.op("dve", lambda e: e.tensor_copy(out=S.b1f[0:T], in_=S.b1[0:T]), reads=[S.b1], writes=[S.b1f])
    k.op("dve", lambda e: e.tensor_copy(out=S.b2f[0:T], in_=S.b2[0:T]), reads=[S.b2], writes=[S.b2f])
    for (bf, half, dst) in ((S.b1f, 0, S.I1), (S.b2f, 1, S.I2)):
        for h in range(PH):
            k.op("dve", lambda e, bf=bf, h=h: e.tensor_tensor(out=S.oh[0:T], in0=C.iota16[0:T, :].unsqueeze(1).to_broadcast([T, 16, 16]),
                                                              in1=bf[0:T, h, :].unsqueeze(2).to_broadcast([T, 16, 16]), op=ALU.is_equal), reads=[C.iota16, bf], writes=[S.oh])
            k.op("dve", lambda e, h=h, half=half: e.tensor_tensor(out=S.oh[0:T], in0=S.oh[0:T], in1=sif4[0:T, h, half, :].unsqueeze(1).to_broadcast([T, 16, 16]), op=ALU.mult),
                 reads=[S.oh, S.sif], writes=[S.oh])
            k.op("dve", lambda e, h=h, dst=dst: e.tensor_reduce(out=dst[0:T, h, :], in_=S.oh[0:T], axis=AX.X, op=ALU.add), reads=[S.oh], writes=[dst])
    k.op("dve", lambda e: e.scalar_tensor_tensor(out=S.ef[0:T], in0=S.I1[0:T], scalar=float(PK), in1=S.I2[0:T], op0=ALU.mult, op1=ALU.add), reads=[S.I1, S.I2], writes=[S.ef])
    k.op("dve", lambda e: e.tensor_copy(out=S.ei[0:T], in_=S.ef[0:T]), reads=[S.ef], writes=[S.ei])
    k.op("dve", lambda e: e.tensor_tensor(out=S.g[0:T], in0=S.best[0:T], in1=S.best[0:T, :, 0:1].to_broadcast([T, PH, 16]), op=ALU.subtract), reads=[S.best], writes=[S.g])
    k.op("act", lambda e: e.activation(out=S.g[0:T], in_=S.g[0:T], func=AF.Exp), reads=[S.g], writes=[S.g])
    k.op("dve", lambda e: e.tensor_reduce(out=S.gs[0:T], in_=S.g[0:T], axis=AX.X, op=ALU.add), reads=[S.g], writes=[S.gs])
    k.op("dve", lambda e: e.reciprocal(out=S.gs[0:T], in_=S.gs[0:T]), reads=[S.gs], writes=[S.gs])
    k.op("dve", lambda e: e.tensor_tensor(out=S.g[0:T], in0=S.g[0:T], in1=S.gs[0:T].unsqueeze(2).to_broadcast([T, PH, 16]), op=ALU.mult), reads=[S.g, S.gs], writes=[S.g])
    if mid_barrier:
        k.barrier()
    eif = S.ei.ap.rearrange("p h k -> p (h k)")
    af = S.a.ap.rearrange("p h k -> p (h k)")
    nb = len(S.gbuf)
    for hk in range(PH * PTOP):
        gb = S.gbuf[hk % nb]
        k.dma("pool", lambda e, hk=hk, gb=gb: e.indirect_dma_start(out=gb[0:T, :], out_offset=None, in_=W.u,
              in_offset=bass.IndirectOffsetOnAxis(ap=eif[0:T, hk:hk + 1], axis=0)), gb, reads=[S.ei], writes=[gb])
        k.op("dve", lambda e, hk=hk, gb=gb: e.scalar_tensor_tensor(out=S.junk[0:T], in0=gb[0:T, :], scalar=1.0, in1=S.h_tm[0:T, :], op0=ALU.mult, op1=ALU.mult,
                                                                   accum_out=af[0:T, hk:hk + 1]), reads=[gb, S.h_tm], writes=[S.junk, S.a])
    k.op("act", lambda e: e.activation(out=S.w[0:T], in_=S.a[0:T], func=AF.Gelu_apprx_tanh), reads=[S.a], writes=[S.w])
    k.op("dve", lambda e: e.tensor_tensor(out=S.w[0:T], in0=S.w[0:T], in1=S.g[0:T], op=ALU.mult), reads=[S.w, S.g], writes=[S.w])
    wf = S.w.ap.rearrange("p h k -> p (h k)")
    for hk in range(PH * PTOP):
        gb = S.gbuf[hk % nb]
        k.dma("pool", lambda e, hk=hk, gb=gb: e.indirect_dma_start(out=gb[0:T, :], out_offset=None, in_=W.v,
              in_offset=bass.IndirectOffsetOnAxis(ap=eif[0:T, hk:hk + 1], axis=0)), gb, reads=[S.ei], writes=[gb])
        if hk == 0:
            k.op("dve", lambda e, gb=gb: e.tensor_scalar(out=S.acc[0:T, :], in0=gb[0:T, :], scalar1=wf[0:T, 0:1], scalar2=None, op0=ALU.mult), reads=[gb, S.w], writes=[S.acc])
        else:
            k.op("dve", lambda e, hk=hk, gb=gb: e.scalar_tensor_tensor(out=S.acc[0:T, :], in0=gb[0:T, :], scalar=wf[0:T, hk:hk + 1], in1=S.acc[0:T, :], op0=ALU.mult, op1=ALU.add),
                 reads=[gb, S.w, S.acc], writes=[S.acc])
    gated_residual_add(k, C, xT, S.acc, gate, T)


class NS:
    pass


def peer_scratch(k, ngb=4):
    S = NS()
    S.hT32 = k.sb_once("p_hT32", [128, NCH, 128], F32)
    S.hTb = k.sb_once("p_hTb", [128, NCH, 128], BF16)
    S.h_tm = k.sb("p_h_tm", [128, D], F32)
    S.qT = k.sb("p_qT", [128, NHC, 128], BF16)
    S.s = k.sb("p_s", [128, NHC, PK], F32)
    S.s2 = k.sb("p_s2", [128, NHC, PK], F32)
    S.sv = k.sb("p_sv", [128, NHC, 16], F32)
    S.si = k.sb("p_si", [128, NHC, 16], U32)
    S.sif = k.sb("p_sif", [128, NHC, 16], F32)
    S.cand = k.sb("p_cand", [128, PH, 16, 16], F32)
    S.cand2 = k.sb("p_cand2", [128, PH, 16, 16], F32)
    S.best = k.sb("p_best", [128, PH, 16], F32)
    S.bi = k.sb("p_bi", [128, PH, 16], U32)
    S.b1 = k.sb("p_b1", [128, PH, 16], U32)
    S.b2 = k.sb("p_b2", [128, PH, 16], U32)
    S.b1f = k.sb("p_b1f", [128, PH, 16], F32)
    S.b2f = k.sb("p_b2f", [128, PH, 16], F32)
    S.oh = k.sb("p_oh", [128, 16, 16], F32)
    S.I1 = k.sb("p_I1", [128, PH, 16], F32)
    S.I2 = k.sb("p_I2", [128, PH, 16], F32)
    S.ef = k.sb("p_ef", [128, PH, 16], F32)
    S.ei = k.sb("p_ei", [128, PH, 16], I32)
    S.g = k.sb("p_g", [128, PH, 16], F32)
    S.gs = k.sb("p_gs", [128, PH], F32)
    S.a = k.sb("p_a", [128, PH, 16], F32)
    S.w = k.sb("p_w", [128, PH, 16], F32)
    S.junk = k.sb("p_junk", [128, D], F32)
    S.acc = k.sb_once("p_acc", [128, D], F32)
    S.gbuf = [k.sb(f"p_gb{i}", [128, D], F32) for i in range(ngb)]
    return S


class WS:
    def __init__(self, k, nslots=4, q="sp"):
        self.k, self.q, self.i = k, q, 0
        self.slots = [k.sb(f"wslot{i}", [128, 16, 512], BF16) for i in range(nslots)]
        self.bslots = [k.sb(f"bslot{i}", [128, 512], F32) for i in range(4)]
        self.bi = 0

    def block(self, w2d, kc0, col0, nkc=16, ncols=512):
        s = self.slots[self.i % len(self.slots)]
        self.i += 1
        src = w2d[kc0 * 128:(kc0 + nkc) * 128, col0:col0 + ncols].rearrange("(c p) n -> p c n", p=128)
        self.k.dma(self.q, lambda e: e.dma_start(out=s[:, 0:nkc, 0:ncols], in_=src), s, writes=[s])
        return s

    def row_bcast(self, row1d, col0, ncols=512):
        s = self.bslots[self.bi % len(self.bslots)]
        self.bi += 1
        src = row1d[col0:col0 + ncols].unsqueeze(0).partition_broadcast(128)
        self.k.dma(self.q, lambda e: e.dma_start(out=s[:, 0:ncols], in_=src), s, writes=[s])
        return s


SGD = 6144


def sgu(k, C, ws, xT, gm, sh, gate, Wd, S, T=128, v_out=None):
    rms_modulate(k, C, xT, gm, sh, S.hT32, S.hTb, T)
    for nb in range(24):
        slot = ws.block(Wd["w_in"], 0, nb * 512)
        brow = ws.row_bcast(Wd["b_in"], nb * 512)
        pb = C.bank[3 + nb % 2]
        for c in range(NCH):
            k.op("pe", lambda e, c=c, pb=pb, slot=slot: e.matmul(pb[0:T, :], lhsT=S.hTb[:, c, 0:T], rhs=slot[:, c, :], start=(c == 0), stop=(c == NCH - 1)),
                 reads=[S.hTb, slot], writes=[pb])
        off = (nb % 12) * 512
        if nb < 12:
            k.op("dve", lambda e, pb=pb, brow=brow: e.tensor_tensor(out=S.tmp[0:T, :], in0=pb[0:T, :], in1=brow[0:T, :], op=ALU.add), reads=[pb, brow], writes=[S.tmp])
            k.op("act", lambda e, off=off: e.activation(out=S.u[0:T, off:off + 512], in_=S.tmp[0:T, :], func=AF.Gelu_apprx_tanh), reads=[S.tmp], writes=[S.u])
        else:
            k.op("dve", lambda e, pb=pb, brow=brow, off=off: e.tensor_tensor(out=S.v[0:T, off:off + 512], in0=pb[0:T, :], in1=brow[0:T, :], op=ALU.add), reads=[pb, brow], writes=[S.v])
            k.op("act", lambda e, off=off: e.activation(out=S.v[0:T, off:off + 512], in_=S.v[0:T, off:off + 512], func=AF.Gelu_apprx_tanh), reads=[S.v], writes=[S.v])
    k.op("dve", lambda e: e.tensor_reduce(out=S.st[0:T, 0:1], in_=S.v[0:T, :], axis=AX.X, op=ALU.add), reads=[S.v], writes=[S.st])
    k.op("act", lambda e: e.activation(out=S.vb[0:T, :], in_=S.v[0:T, :], func=AF.Square, accum_out=S.st[0:T, 1:2]), reads=[S.v, S.st], writes=[S.vb, S.st])
    k.op("dve", lambda e: e.tensor_scalar(out=S.st[0:T, 0:2], in0=S.st[0:T, 0:2], scalar1=1.0 / SGD, scalar2=None, op0=ALU.mult), reads=[S.st], writes=[S.st])
    k.op("dve", lambda e: e.tensor_tensor(out=S.st[0:T, 2:3], in0=S.st[0:T, 0:1], in1=S.st[0:T, 0:1], op=ALU.mult), reads=[S.st], writes=[S.st])
    k.op("dve", lambda e: e.tensor_tensor(out=S.st[0:T, 2:3], in0=S.st[0:T, 1:2], in1=S.st[0:T, 2:3], op=ALU.subtract), reads=[S.st], writes=[S.st])
    k.op("dve", lambda e: e.tensor_scalar(out=S.st[0:T, 2:3], in0=S.st[0:T, 2:3], scalar1=1e-6, scalar2=None, op0=ALU.add), reads=[S.st], writes=[S.st])
    k.op("act", lambda e: e.activation(out=S.st[0:T, 2:3], in_=S.st[0:T, 2:3], func=AF.Sqrt), reads=[S.st], writes=[S.st])
    k.op("dve", lambda e: e.reciprocal(out=S.st[0:T, 2:3], in_=S.st[0:T, 2:3]), reads=[S.st], writes=[S.st])
    k.op("dve", lambda e: e.tensor_scalar(out=S.v[0:T, :], in0=S.v[0:T, :], scalar1=S.st[0:T, 0:1], scalar2=S.st[0:T, 2:3], op0=ALU.subtract, op1=ALU.mult),
         reads=[S.v, S.st], writes=[S.v])
    for nb in range(12):
        grow = ws.row_bcast(Wd["ln_g"], nb * 512)
        brow = ws.row_bcast(Wd["ln_b"], nb * 512)
        k.op("dve", lambda e, nb=nb, grow=grow: e.tensor_tensor(out=S.v[0:T, nb * 512:(nb + 1) * 512], in0=S.v[0:T, nb * 512:(nb + 1) * 512], in1=grow[0:T, :], op=ALU.mult),
             reads=[S.v, grow], writes=[S.v])
        k.op("dve", lambda e, nb=nb, brow=brow: e.tensor_tensor(out=S.v[0:T, nb * 512:(nb + 1) * 512], in0=S.v[0:T, nb * 512:(nb + 1) * 512], in1=brow[0:T, :], op=ALU.add),
             reads=[S.v, brow], writes=[S.v])
    if v_out is not None:
        k.dma("sp", lambda e: e.dma_start(out=v_out, in_=S.v[0:T, :]), S.v, reads=[S.v])
    k.op("act", lambda e: e.copy(out=S.vb[0:T, :], in_=S.v[0:T, :]), reads=[S.v], writes=[S.vb])
    for g in range(8):
        for (o, n) in ((0, 512), (512, 256)):
            pb = C.bank[5 + (2 * g + (o > 0)) % 2]
            c0 = g * 768 + o
            k.op("pe", lambda e, g=g, pb=pb, c0=c0, n=n: e.matmul(pb[0:T, 0:n], lhsT=Wd["wsT"][0:T, g, 0:T], rhs=S.vb[0:T, c0:c0 + n], start=True, stop=True),
                 reads=[Wd["wsT"], S.vb], writes=[pb])
            k.op("dve", lambda e, g=g, pb=pb, c0=c0, n=n: e.scalar_tensor_tensor(out=S.prod[0:T, c0:c0 + n], in0=pb[0:T, 0:n], scalar=Wd["bs"][0:T, g:g + 1],
                                                                                in1=S.u[0:T, c0:c0 + n], op0=ALU.add, op1=ALU.mult), reads=[pb, Wd["bs"], S.u], writes=[S.prod])
    for b4 in range(12):
        pb = C.bank[1 + b4 % 2]
        for j in range(4):
            c = b4 * 4 + j
            k.op("pe", lambda e, c=c, j=j, pb=pb: e.transpose(pb[:, j * 128:j * 128 + T], S.prod[0:T, c * 128:(c + 1) * 128], C.ident[0:T, 0:T]), reads=[S.prod, C.ident], writes=[pb])
        k.op("act", lambda e, b4=b4, pb=pb: e.copy(out=S.prodT[:, b4 * 4:(b4 + 1) * 4, 0:T], in_=pb[:, :].rearrange("p (a b) -> p a b", a=4)[:, :, 0:T]), reads=[pb], writes=[S.prodT])
    for nb in range(4):
        pb = C.bank[3 + nb % 2]
        for ku in range(3):
            slot = ws.block(Wd["w_out"], ku * 16, nb * 512)
            for c in range(16):
                kc = ku * 16 + c
                k.op("pe", lambda e, kc=kc, c=c, pb=pb, slot=slot: e.matmul(pb[0:T, :], lhsT=S.prodT[:, kc, 0:T], rhs=slot[:, c, :], start=(kc == 0), stop=(kc == 47)),
                     reads=[S.prodT, slot], writes=[pb])
        k.op("act", lambda e, nb=nb, pb=pb: e.copy(out=S.out_tm[0:T, nb * 512:(nb + 1) * 512], in_=pb[0:T, :]), reads=[pb], writes=[S.out_tm])
    gated_residual_add(k, C, xT, S.out_tm, gate, T)


def sgu_scratch(k):
    S = NS()
    S.hT32 = k.sb_once("p_hT32", [128, NCH, 128], F32)
    S.hTb = k.sb_once("p_hTb", [128, NCH, 128], BF16)
    S.u = k.sb("g_u", [128, SGD], BF16)
    S.v = k.sb("g_v", [128, SGD], F32)
    S.vb = k.sb("g_vb", [128, SGD], BF16)
    S.prod = S.v
    S.tmp = k.sb("g_tmp", [128, 512], F32)
    S.prodT = k.sb("g_prodT", [128, 48, 128], BF16)
    S.st = k.sb("g_st", [128, 4], F32)
    S.out_tm = k.sb_once("p_acc", [128, D], F32)
    return S


CW = 31


def conv_load_hist_zero(k, S):
    k.op("dve", lambda e: e.memset(S.xp[:, :, 0:CW - 1], 0.0), writes=[S.xp])


def conv_load_hist_state(k, C, S, state_dram):
    k.dma("sp", lambda e: e.dma_start(out=S.tm30[0:CW - 1, :], in_=state_dram), S.tm30, writes=[S.tm30])
    for b4 in range(4):
        pb = C.bank[1 + b4 % 2]
        for j in range(4):
            c = b4 * 4 + j
            k.op("pe", lambda e, c=c, j=j, pb=pb: e.transpose(pb[:, j * 128:j * 128 + CW - 1], S.tm30[0:CW - 1, c * 128:(c + 1) * 128], C.ident[0:CW - 1, 0:CW - 1]),
                 reads=[S.tm30, C.ident], writes=[pb])
        k.op("act", lambda e, b4=b4, pb=pb: e.copy(out=S.xp[:, b4 * 4:(b4 + 1) * 4, 0:CW - 1], in_=pb[:, :].rearrange("p (a b) -> p a b", a=4)[:, :, 0:CW - 1]),
             reads=[pb], writes=[S.xp])


def conv(k, C, ws, xT, gm, sh, gate, Wd, S, T=128, state_out=None, carry=True):
    H = CW - 1
    rms_modulate(k, C, xT, gm, sh, S.hT32, S.hTb, T)
    for b in range(4):
        sl = ws.block(Wd["w_in"], 0, b * 512)
        sg = ws.block(Wd["w_in"], 0, (b + 4) * 512)
        pl, pg = C.bank[3 + b % 2], C.bank[5 + b % 2]
        for j in range(4):
            for c in range(NCH):
                k.op("pe", lambda e, j=j, c=c, pl=pl, sl=sl: e.matmul(pl[:, j * 128:j * 128 + T], lhsT=sl[:, c, j * 128:(j + 1) * 128], rhs=S.hTb[:, c, 0:T], start=(c == 0), stop=(c == NCH - 1)),
                     reads=[sl, S.hTb], writes=[pl])
            for c in range(NCH):
                k.op("pe", lambda e, j=j, c=c, pg=pg, sg=sg: e.matmul(pg[:, j * 128:j * 128 + T], lhsT=sg[:, c, j * 128:(j + 1) * 128], rhs=S.hTb[:, c, 0:T], start=(c == 0), stop=(c == NCH - 1)),
                     reads=[sg, S.hTb], writes=[pg])
        for j in range(4):
            ch = b * 4 + j
            k.op("act", lambda e, j=j, ch=ch, pg=pg: e.activation(out=S.sg[:, 0:T], in_=pg[:, j * 128:j * 128 + T], func=AF.Sigmoid, bias=Wd["b_in"][:, 16 + ch:17 + ch]),
                 reads=[pg, Wd["b_in"]], writes=[S.sg])
            k.op("dve", lambda e, j=j, ch=ch, pl=pl: e.scalar_tensor_tensor(out=S.xp[:, ch, H:H + T], in0=pl[:, j * 128:j * 128 + T], scalar=Wd["b_in"][:, ch:ch + 1], in1=S.sg[:, 0:T],
                                                                            op0=ALU.add, op1=ALU.mult), reads=[pl, Wd["b_in"], S.sg], writes=[S.xp])
    if state_out is not None:
        for b4 in range(4):
            pb = C.bank[1 + b4 % 2]
            for j in range(4):
                c = b4 * 4 + j
                k.op("pe", lambda e, c=c, j=j, pb=pb: e.transpose(pb[0:H, j * 128:(j + 1) * 128], S.xp[:, c, T:T + H], C.ident[:]), reads=[S.xp, C.ident], writes=[pb])
            k.op("act", lambda e, b4=b4, pb=pb: e.copy(out=S.tm30[0:H, b4 * 512:(b4 + 1) * 512], in_=pb[0:H, :]), reads=[pb], writes=[S.tm30])
        k.dma("sp", lambda e: e.dma_start(out=state_out, in_=S.tm30[0:H, :]), S.tm30, reads=[S.tm30])
    for c in range(NCH):
        k.op("act", lambda e, c=c: e.activation(out=S.y[:, c, 0:T], in_=S.xp[:, c, 0:T], func=AF.Identity, scale=Wd["dw"][:, c, 0:1], bias=Wd["dw_b"][:, c:c + 1]),
             reads=[S.xp, Wd["dw"], Wd["dw_b"]], writes=[S.y])
        for kk in range(1, CW):
            k.op("dve", lambda e, c=c, kk=kk: e.scalar_tensor_tensor(out=S.y[:, c, 0:T], in0=S.xp[:, c, kk:kk + T], scalar=Wd["dw"][:, c, kk:kk + 1], in1=S.y[:, c, 0:T],
                                                                     op0=ALU.mult, op1=ALU.add), reads=[S.xp, Wd["dw"], S.y], writes=[S.y])
    if carry:
        k.op("act", lambda e: e.copy(out=S.hist_tmp[:, :, :], in_=S.xp[:, :, T:T + H]), reads=[S.xp], writes=[S.hist_tmp])
        k.op("act", lambda e: e.copy(out=S.xp[:, :, 0:H], in_=S.hist_tmp[:, :, :]), reads=[S.hist_tmp], writes=[S.xp])
    k.op("act", lambda e: e.copy(out=S.yb[:, :, 0:T], in_=S.y[:, :, 0:T]), reads=[S.y], writes=[S.yb])
    k.op("act", lambda e: e.activation(out=S.y2b[:, :, 0:T], in_=S.y[:, :, 0:T], func=AF.Square), reads=[S.y], writes=[S.y2b])
    pb = C.bank[0]
    for (src, off) in ((S.yb, 0), (S.y2b, 128)):
        for c in range(NCH):
            k.op("pe", lambda e, src=src, off=off, c=c, pb=pb: e.matmul(pb[:, off:off + T], lhsT=C.ones_bf[:], rhs=src[:, c, 0:T], start=(c == 0), stop=(c == NCH - 1)),
                 reads=[src, C.ones_bf], writes=[pb])
    k.op("dve", lambda e, pb=pb: e.tensor_scalar(out=S.mv[:, 0:256], in0=pb[:, 0:256], scalar1=1.0 / D, scalar2=None, op0=ALU.mult), reads=[pb], writes=[S.mv])
    k.op("dve", lambda e: e.tensor_tensor(out=S.mv[:, 256:256 + T], in0=S.mv[:, 0:T], in1=S.mv[:, 0:T], op=ALU.mult), reads=[S.mv], writes=[S.mv])
    k.op("dve", lambda e: e.tensor_tensor(out=S.mv[:, 256:256 + T], in0=S.mv[:, 128:128 + T], in1=S.mv[:, 256:256 + T], op=ALU.subtract), reads=[S.mv], writes=[S.mv])
    k.op("dve", lambda e: e.tensor_scalar(out=S.mv[:, 256:256 + T], in0=S.mv[:, 256:256 + T], scalar1=1e-6, scalar2=None, op0=ALU.add), reads=[S.mv], writes=[S.mv])
    k.op("act", lambda e: e.activation(out=S.mv[:, 256:256 + T], in_=S.mv[:, 256:256 + T], func=AF.Sqrt), reads=[S.mv], writes=[S.mv])
    k.op("dve", lambda e: e.reciprocal(out=S.mv[:, 256:256 + T], in_=S.mv[:, 256:256 + T]), reads=[S.mv], writes=[S.mv])
    k.op("dve", lambda e: e.tensor_tensor(out=S.y[:, :, 0:T], in0=S.y[:, :, 0:T], in1=S.mv[:, 0:T].unsqueeze(1).to_broadcast([128, NCH, T]), op=ALU.subtract), reads=[S.y, S.mv], writes=[S.y])
    k.op("dve", lambda e: e.tensor_tensor(out=S.y[:, :, 0:T], in0=S.y[:, :, 0:T], in1=S.mv[:, 256:256 + T].unsqueeze(1).to_broadcast([128, NCH, T]), op=ALU.mult), reads=[S.y, S.mv], writes=[S.y])
    for c in range(NCH):
        k.op("act", lambda e, c=c: e.activation(out=S.yb[:, c, 0:T], in_=S.y[:, c, 0:T], func=AF.Silu, scale=Wd["ln_g"][:, c:c + 1], bias=Wd["ln_b"][:, c:c + 1]),
             reads=[S.y, Wd["ln_g"], Wd["ln_b"]], writes=[S.yb])
    for nb in range(4):
        slot = ws.block(Wd["w_out"], 0, nb * 512)
        brow = ws.row_bcast(Wd["b_out"], nb * 512)
        pb = C.bank[3 + nb % 2]
        for c in range(NCH):
            k.op("pe", lambda e, c=c, pb=pb, slot=slot: e.matmul(pb[0:T, :], lhsT=S.yb[:, c, 0:T], rhs=slot[:, c, :], start=(c == 0), stop=(c == NCH - 1)), reads=[S.yb, slot], writes=[pb])
        k.op("dve", lambda e, nb=nb, pb=pb, brow=brow: e.tensor_tensor(out=S.out_tm[0:T, nb * 512:(nb + 1) * 512], in0=pb[0:T, :], in1=brow[0:T, :], op=ALU.add), reads=[pb, brow], writes=[S.out_tm])
    gated_residual_add(k, C, xT, S.out_tm, gate, T)


def conv_scratch(k):
    S = NS()
    S.hT32 = k.sb_once("p_hT32", [128, NCH, 128], F32)
    S.hTb = k.sb_once("p_hTb", [128, NCH, 128], BF16)
    S.out_tm = k.sb_once("p_acc", [128, D], F32)
    S.xp = k.sb("c_xp", [128, NCH, CW - 1 + 128], F32)
    S.hist_tmp = k.sb("c_hist", [128, NCH, CW - 1], F32)
    S.y = k.sb("c_y", [128, NCH, 128], F32)
    S.yb = k.sb("c_yb", [128, NCH, 128], BF16)
    S.y2b = k.sb("c_y2b", [128, NCH, 128], BF16)
    S.sg = k.sb("c_sg", [128, 128], F32)
    S.mv = k.sb("c_mv", [128, 384], F32)
    S.tm30 = k.sb("c_tm30", [32, D], F32)
    return S


NH, NKV, HD = 32, 4, 64
BIGD = 1e32


def attn_consts(k):
    A = NS()
    di = k.sb("a_di", [128, 256], I32)
    A.Dmid = k.sb("a_Dmid", [128, 256], F32)
    A.Dfirst = k.sb("a_Dfirst", [128, 256], F32)
    A.Dsamp = k.sb("a_Dsamp", [128, 256], F32)
    k.op("pool", lambda e: e.iota(di[:], pattern=[[-1, 256]], base=128, channel_multiplier=1), writes=[di])
    k.op("dve", lambda e: e.tensor_copy(out=A.Dsamp[:], in_=di[:]), reads=[di], writes=[A.Dsamp])
    k.op("dve", lambda e: e.tensor_scalar(out=A.Dmid[:], in0=A.Dsamp[:], scalar1=-1.0, scalar2=None, op0=ALU.mult), reads=[A.Dsamp], writes=[A.Dmid])
    k.op("dve", lambda e: e.tensor_max(out=A.Dsamp[:], in0=A.Dsamp[:], in1=A.Dmid[:]), reads=[A.Dsamp, A.Dmid], writes=[A.Dsamp])
    for Dm in (A.Dmid, A.Dfirst):
        k.op("dve", lambda e, Dm=Dm: e.tensor_copy(out=Dm[:], in_=A.Dsamp[:]), reads=[A.Dsamp], writes=[Dm])
        k.op("dve", lambda e, Dm=Dm: e.memset(Dm[0:64, 192:256], BIGD), writes=[Dm])
        k.op("dve", lambda e, Dm=Dm: e.memset(Dm[64:128, 0:64], BIGD), writes=[Dm])
    k.op("dve", lambda e: e.memset(A.Dfirst[:, 0:128], BIGD), writes=[A.Dfirst])
    return A


def attn_load_prev(k, C, S, ck_dram, cv_dram):
    k.dma("sp", lambda e: e.dma_start(out=S.kv_tm[:, 0:256], in_=ck_dram), S.kv_tm, writes=[S.kv_tm])
    k.dma("sp", lambda e: e.dma_start(out=S.kv_tm[:, 256:512], in_=cv_dram), S.kv_tm, writes=[S.kv_tm])
    pb = C.bank[1]
    for g in range(NKV):
        k.op("pe", lambda e, g=g: e.transpose(pb[0:HD, g * 128:(g + 1) * 128], S.kv_tm[:, g * HD:(g + 1) * HD], C.ident[:]), reads=[S.kv_tm, C.ident], writes=[pb])
    k.op("act", lambda e: e.copy(out=S.kTprev[:, :, :], in_=pb[0:HD, :].rearrange("p (a b) -> p a b", a=NKV)), reads=[pb], writes=[S.kTprev])
    k.op("act", lambda e: e.copy(out=S.vprev[:, :], in_=S.kv_tm[:, 256:512]), reads=[S.kv_tm], writes=[S.vprev])


def attn(k, C, ws, xT, gm, sh, gate, Wd, S, Dm, T=128, kwin_out=None, vwin_out=None, win_rows=None, carry=True):
    slopes = [2.0 ** (-8.0 * (h + 1) / NH) for h in range(NH)]
    rms_modulate(k, C, xT, gm, sh, S.hT32, S.hTb, T)
    for nb in range(4):
        slot = ws.block(Wd["w_qkv"], 0, nb * 512)
        for half in range(2):
            pb = C.bank[1 + half]
            for j in range(4):
                hl = half * 4 + j
                for c in range(NCH):
                    k.op("pe", lambda e, hl=hl, j=j, c=c, pb=pb, slot=slot: e.matmul(pb[0:HD, j * 128:j * 128 + T], lhsT=slot[:, c, hl * HD:(hl + 1) * HD], rhs=S.hTb[:, c, 0:T],
                                                                                start=(c == 0), stop=(c == NCH - 1)), reads=[slot, S.hTb], writes=[pb])
            for j in range(4):
                h = nb * 8 + half * 4 + j
                k.op("dve", lambda e, h=h, j=j, pb=pb: e.tensor_scalar(out=S.qT[:, h, 0:T], in0=pb[0:HD, j * 128:j * 128 + T], scalar1=Wd["bq"][:, h:h + 1], scalar2=HD ** -0.5,
                                                                       op0=ALU.add, op1=ALU.mult), reads=[pb, Wd["bq"]], writes=[S.qT])
    slot = ws.block(Wd["w_qkv"], 0, 2048)
    brow = ws.row_bcast(Wd["b_qkv"], 2048)
    pb = C.bank[3]
    for c in range(NCH):
        k.op("pe", lambda e, c=c, pb=pb, slot=slot: e.matmul(pb[0:T, :], lhsT=S.hTb[:, c, 0:T], rhs=slot[:, c, :], start=(c == 0), stop=(c == NCH - 1)), reads=[S.hTb, slot], writes=[pb])
    k.op("dve", lambda e, pb=pb, brow=brow: e.tensor_tensor(out=S.kv_tm[0:T, :], in0=pb[0:T, :], in1=brow[0:T, :], op=ALU.add), reads=[pb, brow], writes=[S.kv_tm])
    if kwin_out is not None:
        r0, n = win_rows
        k.dma("sp", lambda e: e.dma_start(out=kwin_out[r0:r0 + n, :], in_=S.kv_tm[T - n:T, 0:256]), S.kv_tm, reads=[S.kv_tm])
        k.dma("sp", lambda e: e.dma_start(out=vwin_out[r0:r0 + n, :], in_=S.kv_tm[T - n:T, 256:512]), S.kv_tm, reads=[S.kv_tm])
    k.op("act", lambda e: e.copy(out=S.vcur[0:T, :], in_=S.kv_tm[0:T, 256:512]), reads=[S.kv_tm], writes=[S.vcur])
    pb = C.bank[4]
    for g in range(NKV):
        k.op("pe", lambda e, g=g, pb=pb: e.transpose(pb[0:HD, g * 128:g * 128 + T], S.kv_tm[0:T, g * HD:(g + 1) * HD], C.ident[0:T, 0:T]), reads=[S.kv_tm, C.ident], writes=[pb])
    k.op("act", lambda e, pb=pb: e.copy(out=S.kTcur[:, :, 0:T], in_=pb[0:HD, :].rearrange("p (a b) -> p a b", a=NKV)[:, :, 0:T]), reads=[pb], writes=[S.kTcur])
    NKEY = 128 + T
    for h in range(NH):
        g = h // (NH // NKV)
        ps_, pt_, po_ = C.bank[5 + h % 2], C.bank[1 + h % 2], C.bank[7]
        k.op("pe", lambda e, h=h, g=g, ps_=ps_: e.matmul(ps_[0:T, 0:128], lhsT=S.qT[:, h, 0:T], rhs=S.kTprev[:, g, :], start=True, stop=True), reads=[S.qT, S.kTprev], writes=[ps_])
        k.op("pe", lambda e, h=h, g=g, ps_=ps_: e.matmul(ps_[0:T, 128:NKEY], lhsT=S.qT[:, h, 0:T], rhs=S.kTcur[:, g, 0:T], start=True, stop=True), reads=[S.qT, S.kTcur], writes=[ps_])
        k.op("dve", lambda e, h=h, ps_=ps_: e.scalar_tensor_tensor(out=S.sc[0:T, 0:NKEY], in0=Dm[0:T, 0:NKEY], scalar=-slopes[h], in1=ps_[0:T, 0:NKEY], op0=ALU.mult, op1=ALU.add),
             reads=[Dm, ps_], writes=[S.sc])
        k.op("dve", lambda e: e.tensor_reduce(out=S.m[0:T, 0:1], in_=S.sc[0:T, 0:NKEY], axis=AX.X, op=ALU.max), reads=[S.sc], writes=[S.m])
        k.op("dve", lambda e, h=h: e.tensor_tensor(out=S.m[0:T, 0:1], in0=S.m[0:T, 0:1], in1=Wd["sink"][0:T, h:h + 1], op=ALU.max), reads=[S.m, Wd["sink"]], writes=[S.m])
        k.op("dve", lambda e, h=h: e.tensor_tensor(out=S.m[0:T, 2:3], in0=Wd["sink"][0:T, h:h + 1], in1=S.m[0:T, 0:1], op=ALU.subtract), reads=[S.m, Wd["sink"]], writes=[S.m])
        k.op("dve", lambda e: e.tensor_scalar(out=S.m[0:T, 1:2], in0=S.m[0:T, 0:1], scalar1=-1.0, scalar2=None, op0=ALU.mult), reads=[S.m], writes=[S.m])
        k.op("act", lambda e: e.activation(out=S.p[0:T, 0:NKEY], in_=S.sc[0:T, 0:NKEY], func=AF.Exp, bias=S.m[0:T, 1:2], accum_out=S.m[0:T, 3:4]), reads=[S.sc, S.m], writes=[S.p, S.m])
        k.op("act", lambda e: e.activation(out=S.m[0:T, 2:3], in_=S.m[0:T, 2:3], func=AF.Exp), reads=[S.m], writes=[S.m])
        k.op("dve", lambda e: e.tensor_tensor(out=S.m[0:T, 3:4], in0=S.m[0:T, 3:4], in1=S.m[0:T, 2:3], op=ALU.add), reads=[S.m], writes=[S.m])
        k.op("dve", lambda e: e.reciprocal(out=S.m[0:T, 3:4], in_=S.m[0:T, 3:4]), reads=[S.m], writes=[S.m])
        k.op("pe", lambda e, pt_=pt_: e.transpose(pt_[:, 0:T], S.p[0:T, 0:128], C.ident[0:T, 0:T]), reads=[S.p, C.ident], writes=[pt_])
        k.op("pe", lambda e, pt_=pt_: e.transpose(pt_[0:T, 128:128 + T], S.p[0:T, 128:NKEY], C.ident[0:T, 0:T]), reads=[S.p, C.ident], writes=[pt_])
        k.op("act", lambda e, pt_=pt_: e.copy(out=S.pT[:, 0, 0:T], in_=pt_[:, 0:T]), reads=[pt_], writes=[S.pT])
        k.op("act", lambda e, pt_=pt_: e.copy(out=S.pT[0:T, 1, 0:T], in_=pt_[0:T, 128:128 + T]), reads=[pt_], writes=[S.pT])
        oc = (h % 8) * HD
        k.op("pe", lambda e, g=g, oc=oc, po_=po_: e.matmul(po_[0:T, oc:oc + HD], lhsT=S.pT[:, 0, 0:T], rhs=S.vprev[:, g * HD:(g + 1) * HD], start=True, stop=False), reads=[S.pT, S.vprev], writes=[po_])
        k.op("pe", lambda e, g=g, oc=oc, po_=po_: e.matmul(po_[0:T, oc:oc + HD], lhsT=S.pT[0:T, 1, 0:T], rhs=S.vcur[0:T, g * HD:(g + 1) * HD], start=False, stop=True), reads=[S.pT, S.vcur], writes=[po_])
        k.op("dve", lambda e, h=h, oc=oc, po_=po_: e.tensor_scalar(out=S.o_tm[0:T, h * HD:(h + 1) * HD], in0=po_[0:T, oc:oc + HD], scalar1=S.m[0:T, 3:4], scalar2=None, op0=ALU.mult),
             reads=[po_, S.m], writes=[S.o_tm])
    if carry:
        k.op("act", lambda e: e.copy(out=S.kTprev[:, :, :], in_=S.kTcur[:, :, :]), reads=[S.kTcur], writes=[S.kTprev])
        k.op("act", lambda e: e.copy(out=S.vprev[:, :], in_=S.vcur[:, :]), reads=[S.vcur], writes=[S.vprev])
    for b4 in range(4):
        pb = C.bank[1 + b4 % 2]
        for j in range(4):
            c = b4 * 4 + j
            k.op("pe", lambda e, c=c, j=j, pb=pb: e.transpose(pb[:, j * 128:j * 128 + T], S.o_tm[0:T, c * 128:(c + 1) * 128], C.ident[0:T, 0:T]), reads=[S.o_tm, C.ident], writes=[pb])
        k.op("act", lambda e, b4=b4, pb=pb: e.copy(out=S.oT[:, b4 * 4:(b4 + 1) * 4, 0:T], in_=pb[:, :].rearrange("p (a b) -> p a b", a=4)[:, :, 0:T]), reads=[pb], writes=[S.oT])
    for nb in range(4):
        slot = ws.block(Wd["w_o"], 0, nb * 512)
        pb = C.bank[3 + nb % 2]
        for c in range(NCH):
            k.op("pe", lambda e, c=c, pb=pb, slot=slot: e.matmul(pb[0:T, :], lhsT=S.oT[:, c, 0:T], rhs=slot[:, c, :], start=(c == 0), stop=(c == NCH - 1)), reads=[S.oT, slot], writes=[pb])
        k.op("act", lambda e, nb=nb, pb=pb: e.copy(out=S.out_tm[0:T, nb * 512:(nb + 1) * 512], in_=pb[0:T, :]), reads=[pb], writes=[S.out_tm])
    gated_residual_add(k, C, xT, S.out_tm, gate, T)


def attn_scratch(k):
    S = NS()
    S.hT32 = k.sb_once("p_hT32", [128, NCH, 128], F32)
    S.hTb = k.sb_once("p_hTb", [128, NCH, 128], BF16)
    S.out_tm = k.sb_once("p_acc", [128, D], F32)
    S.qT = k.sb("t_qT", [HD, NH, 128], BF16)
    S.kv_tm = k.sb("t_kv", [128, 512], F32)
    S.kTcur = k.sb("t_kTc", [HD, NKV, 128], BF16)
    S.kTprev = k.sb("t_kTp", [HD, NKV, 128], BF16)
    S.vcur = k.sb("t_vc", [128, 256], BF16)
    S.vprev = k.sb("t_vp", [128, 256], BF16)
    S.sc = k.sb("t_sc", [128, 256], F32)
    S.p = k.sb("t_p", [128, 256], F32)
    S.pT = k.sb("t_pT", [128, 2, 128], BF16)
    S.m = k.sb("t_m", [128, 4], F32)
    S.o_tm = k.sb("t_o", [128, D], F32)
    S.oT = k.sb("t_oT", [128, NCH, 128], BF16)
    return S


def ada_layer(k, C, ws, scT, adaw, adab_fm, modv, l, ng):
    for nb in range(24):
        slot = ws.block(adaw, 0, nb * 512)
        pb = C.bank[3 + nb % 2]
        for j in range(4):
            for c in range(NCH):
                k.op("pe", lambda e, j=j, c=c, pb=pb, slot=slot: e.matmul(pb[:, j * 8:j * 8 + ng], lhsT=slot[:, c, j * 128:(j + 1) * 128], rhs=scT[:, c, 0:ng],
                                                                        start=(c == 0), stop=(c == NCH - 1)), reads=[slot, scT], writes=[pb])
        for j in range(4):
            ch = nb * 4 + j
            k.op("dve", lambda e, j=j, ch=ch, pb=pb: e.tensor_scalar(out=modv[:, l, 0:ng, ch], in0=pb[:, j * 8:j * 8 + ng], scalar1=adab_fm[:, ch:ch + 1], scalar2=None, op0=ALU.add),
                 reads=[pb, adab_fm], writes=[modv])


def final_norm_out(k, C, xT, gfin, zero16, S, out_rows, T=128):
    rms_modulate(k, C, xT, gfin, zero16, S.hT32, None, T)
    to_token_major(k, C, S.hT32, S.out_tm, T)
    k.dma("sp", lambda e: e.dma_start(out=out_rows, in_=S.out_tm[0:T, :]), S.out_tm, reads=[S.out_tm])


def peer_q_streamed(k, C, ws, wq_dram, S, T=128):
    for nb in range(4):
        slot = ws.block(wq_dram, 0, nb * 512)
        for j in range(4):
            hc = nb * 4 + j
            pb = C.bank[3 + (hc % 2)]
            for c in range(NCH):
                k.op("pe", lambda e, j=j, c=c, pb=pb, slot=slot: e.matmul(pb[:, 0:T], lhsT=slot[:, c, j * 128:(j + 1) * 128], rhs=S.hTb[:, c, 0:T],
                                                                        start=(c == 0), stop=(c == NCH - 1)), reads=[slot, S.hTb], writes=[pb])
            k.op("act", lambda e, hc=hc, pb=pb: e.copy(out=S.qT[:, hc, 0:T], in_=pb[:, 0:T]), reads=[pb], writes=[S.qT])


NCORES = 8
NTILE = 16
NHALO = 2
DEPTH = 4
KIND = [0, 1, 2, 0]


def _view(k, arena, p0, p1, a, b, name, shape3=None):
    ap = arena.ap[p0:p1, a:b]
    if shape3 is not None:
        ap = ap.rearrange("p (a b) -> p a b", a=shape3[0])
    return k.view(ap, name)


def build_nc(ntile=NTILE, nsamp=1, halo=True):
    nc = bass.Bass("TRN2", target_bir_lowering=False)

    def din(n, s, dt=F32):
        return nc.dram_tensor(n, list(s), dt, kind="ExternalInput").ap()

    def dout(n, s, dt=F32):
        return nc.dram_tensor(n, list(s), dt, kind="ExternalOutput").ap()

    NG = 1 + nsamp
    nh = NHALO if halo else 0
    xp_d = din("xp_fm", [nh + ntile, 128, 16, 128]); flag_d = din("flag", [128, 1]); xs_d = din("xs_fm", [nsamp, 128, 16, 128]); cT_d = din("cT", [128, 16, NG])
    ck_d = din("cache_k", [nsamp, 128, 256]); cv_d = din("cache_v", [nsamp, 128, 256]); st_d = din("state_conv", [nsamp, 30, 2048])
    gmix_d = din("g_mix_fm", [128, DEPTH, 16]); gch_d = din("g_ch_fm", [128, DEPTH, 16]); gfin_d = din("g_fin_fm", [128, 16])
    adaw_d = [din(f"ada_w{l}", [2048, 12288]) for l in range(DEPTH)]; adab_d = din("ada_b_fm", [128, DEPTH, 96])
    sgu_win_d = [din(f"sgu_w_in{i}", [2048, 12288]) for i in range(2)]; sgu_wout_d = [din(f"sgu_w_out{i}", [6144, 2048]) for i in range(2)]
    sgu_bin_d = din("sgu_b_in", [2, 12288]); sgu_lng_d = din("sgu_ln_g", [2, 6144]); sgu_lnb_d = din("sgu_ln_b", [2, 6144])
    wsT_d = din("sgu_wsT", [128, 2, 8, 128]); bs_d = din("sgu_bs_tm", [128, 2, 8])
    cwin_d = din("conv_w_in", [2048, 4096]); cwout_d = din("conv_w_out", [2048, 2048]); cbout_d = din("conv_b_out", [2048])
    cbin_d = din("conv_b_in_fm", [128, 32]); cdw_d = din("conv_dwT", [128, 16, 31]); cdwb_d = din("conv_dw_b_fm", [128, 16])
    clng_d = din("conv_ln_g_fm", [128, 16]); clnb_d = din("conv_ln_b_fm", [128, 16])
    wqkv_d = din("attn_w_qkv", [2048, 2560]); bqkv_d = din("attn_b_qkv", [2560]); bq_d = din("attn_bq_fm", [64, 32]); sink_d = din("attn_sinks", [32]); wo_d = din("attn_w_o", [2048, 2048])
    pwq_d = [din(f"peer_w_q{l}", [2048, 2048]) for l in range(DEPTH)]; skT_d = din("peer_skT", [128, DEPTH, 16, 128])
    pu_d = [din(f"peer_u{l}", [16384, 2048]) for l in range(DEPTH)]; pv_d = [din(f"peer_v{l}", [16384, 2048]) for l in range(DEPTH)]
    y_p = dout("y_p", [ntile * 128, 2048]); y_s = dout("y_s", [nsamp, 16, 2048]); cs_p = dout("cs_p", [30, 2048]); kw_p = dout("kw_p", [128, 256]); vw_p = dout("vw_p", [128, 256])
    cs_s = dout("cs_s", [nsamp, 30, 2048]); kw_s = dout("kw_s", [nsamp, 128, 256]); vw_s = dout("vw_s", [nsamp, 128, 256]); sv_s = dout("sv_s", [2, nsamp, 16, 6144])

    with ExitStack() as st:
        k = K(nc, st)
        C = Common(k)
        A = attn_consts(k)
        def load(name, shape, src, dt=F32, q="sp"):
            b = k.sb(name, shape, dt)
            k.dma(q, lambda e, b=b, src=src: e.dma_start(out=b[:], in_=src), b, writes=[b])
            return b
        cT = load("cT_sb", [128, 16, NG], cT_d)
        gmix = load("gmix_sb", [128, DEPTH, 16], gmix_d); gch = load("gch_sb", [128, DEPTH, 16], gch_d); gfin = load("gfin_sb", [128, 16], gfin_d)
        adab = load("adab_sb", [128, DEPTH, 96], adab_d)
        wsT = load("wsT_sb", [128, 2, 8, 128], wsT_d, BF16, q="pool")
        bs = load("bs_sb", [128, 2, 8], bs_d)
        cbin = load("cbin_sb", [128, 32], cbin_d); cdw = load("cdw_sb", [128, 16, 31], cdw_d); cdwb = load("cdwb_sb", [128, 16], cdwb_d)
        clng = load("clng_sb", [128, 16], clng_d); clnb = load("clnb_sb", [128, 16], clnb_d)
        bq = load("bq_sb", [64, 32], bq_d)
        sink = k.sb("sink_sb", [128, 32], F32)
        k.dma("sp", lambda e: e.dma_start(out=sink[:], in_=sink_d.unsqueeze(0).partition_broadcast(128)), sink, writes=[sink])
        zero16 = k.sb("zero16", [128, 16], F32)
        k.op("dve", lambda e: e.memset(zero16[:], 0.0), writes=[zero16])
        flag = load("flag_sb", [128, 1], flag_d)
        nf = k.sb("nf_sb", [128, 1], F32)
        k.op("dve", lambda e: e.tensor_scalar(out=nf[:], in0=flag[:], scalar1=-BIGD, scalar2=BIGD, op0=ALU.mult, op1=ALU.add), reads=[flag], writes=[nf])
        Dsel = k.sb("a_Dsel", [128, 256], F32)
        k.op("dve", lambda e: e.tensor_scalar(out=Dsel[:, 0:128], in0=A.Dmid[:, 0:128], scalar1=nf[:, 0:1], scalar2=None, op0=ALU.add), reads=[A.Dmid, nf], writes=[Dsel])
        k.op("dve", lambda e: e.tensor_copy(out=Dsel[:, 128:256], in_=A.Dmid[:, 128:256]), reads=[A.Dmid], writes=[Dsel])
        k.op("dve", lambda e: e.memset(wsT[64:128, :, :, 0:64], 0.0), writes=[wsT])
        xT = k.sb("xT", [128, 16, 128], F32)
        hT32 = k.sb_once("p_hT32", [128, NCH, 128], F32); hTb = k.sb_once("p_hTb", [128, NCH, 128], BF16); acc = k.sb_once("p_acc", [128, D], F32)
        NF, NB = 10240, 18432
        AFa = k.sb("arena_f32", [128, NF], F32); ABa = k.sb("arena_bf16", [128, NB], BF16)
        skslot = k.sb("skslot", [128, 16, 128], BF16)
        SG = NS(); SG.hT32, SG.hTb, SG.out_tm = hT32, hTb, acc
        SG.v = _view(k, AFa, 0, 128, 0, 6144, "g_v"); SG.prod = SG.v
        SG.u = _view(k, ABa, 0, 128, 0, 6144, "g_u"); SG.vb = _view(k, ABa, 0, 128, 6144, 12288, "g_vb")
        SG.prodT = _view(k, ABa, 0, 128, 12288, 18432, "g_prodT", (48, 128)); SG.st = k.sb("g_st", [128, 4], F32)
        SG.tmp = _view(k, AFa, 0, 128, 6144, 6656, "g_tmp")
        CV = NS(); CV.hT32, CV.hTb, CV.out_tm = hT32, hTb, acc
        CV.xp = k.sb("c_xp", [128, NCH, CW - 1 + 128], F32); CV.hist_tmp = k.sb("c_hist", [128, NCH, CW - 1], F32)
        CV.y = _view(k, AFa, 0, 128, 0, 2048, "c_y", (16, 128)); CV.tm30 = _view(k, AFa, 0, 32, 2048, 4096, "c_tm30")
        CV.sg = _view(k, AFa, 0, 128, 4096, 4224, "c_sg"); CV.mv = _view(k, AFa, 0, 128, 4224, 4608, "c_mv")
        CV.yb = _view(k, ABa, 0, 128, 0, 2048, "c_yb", (16, 128)); CV.y2b = _view(k, ABa, 0, 128, 2048, 4096, "c_y2b", (16, 128))
        AT = NS(); AT.hT32, AT.hTb, AT.out_tm = hT32, hTb, acc
        AT.kTprev = k.sb("t_kTp", [HD, NKV, 128], BF16); AT.vprev = k.sb("t_vp", [128, 256], BF16); AT.m = k.sb("t_m", [128, 4], F32)
        AT.o_tm = _view(k, AFa, 0, 128, 0, 2048, "t_o"); AT.kv_tm = _view(k, AFa, 0, 128, 2048, 2560, "t_kv")
        AT.sc = _view(k, AFa, 0, 128, 2560, 2816, "t_sc"); AT.p = _view(k, AFa, 0, 128, 2816, 3072, "t_p")
        AT.qT = _view(k, ABa, 0, HD, 0, 4096, "t_qT", (NH, 128)); AT.kTcur = _view(k, ABa, 0, HD, 4096, 4608, "t_kTc", (NKV, 128))
        AT.vcur = _view(k, ABa, 0, 128, 4608, 4864, "t_vc"); AT.pT = _view(k, ABa, 0, 128, 4864, 5120, "t_pT", (2, 128))
        AT.oT = _view(k, ABa, 0, 128, 5120, 7168, "t_oT", (16, 128))
        PE = NS(); PE.hT32, PE.hTb, PE.acc = hT32, hTb, acc
        PE.h_tm = _view(k, AFa, 0, 128, 0, 2048, "p_h_tm")
        PE.s = _view(k, AFa, 0, 128, 2048, 4096, "p_s", (NHC, PK)); PE.s2 = _view(k, AFa, 0, 128, 4096, 6144, "p_s2", (NHC, PK))
        PE.cand = k.view(AFa.ap[:, 6144:8192].rearrange("p (h a b) -> p h a b", h=PH, a=16), "p_cand")
        PE.cand2 = k.view(AFa.ap[:, 8192:10240].rearrange("p (h a b) -> p h a b", h=PH, a=16), "p_cand2")
        PE.gbuf = [_view(k, AFa, 0, 128, 2048 * (i + 1), 2048 * (i + 2), f"p_gb{i}") for i in range(3)]
        PE.junk = _view(k, AFa, 0, 128, 8192, 10240, "p_junk")
        PE.qT = _view(k, ABa, 0, 128, 0, 2048, "p_qT", (NHC, 128))
        for n_, shp, dt_ in (("sv", [128, NHC, 16], F32), ("si", [128, NHC, 16], U32), ("sif", [128, NHC, 16], F32), ("best", [128, PH, 16], F32), ("bi", [128, PH, 16], U32),
                             ("b1", [128, PH, 16], U32), ("b2", [128, PH, 16], U32), ("b1f", [128, PH, 16], F32), ("b2f", [128, PH, 16], F32), ("oh", [128, 16, 16], F32),
                             ("I1", [128, PH, 16], F32), ("I2", [128, PH, 16], F32), ("ef", [128, PH, 16], F32), ("ei", [128, PH, 16], I32), ("g", [128, PH, 16], F32),
                             ("gs", [128, PH], F32), ("a", [128, PH, 16], F32), ("w", [128, PH, 16], F32)):
            setattr(PE, n_, k.sb("p_" + n_, shp, dt_))
        scT = k.sb("scT", [128, 16, NG], BF16)
        modv = k.sb("modv", [128, DEPTH, NG, 96], F32)
        der = k.sb("der", [128, DEPTH, NG, 2, 16], F32)
        k.sb_once("rm_sq", [128, NCH, 128], BF16); k.sb_once("rm_rstd", [128, 128], F32)
        left = nc.sbuf_bytes_remaining - 4 * 2048 - 1024
        nsl = max(1, min(3, left // 16384))
        print("[kernel] sbuf left before weight slots:", nc.sbuf_bytes_remaining, "-> slots:", nsl, flush=True)
        assert left >= 16384, "no room for a weight slot"
        ws = WS(k, nslots=nsl, q="pool")
        k.op("act", lambda e: e.activation(out=scT[:], in_=cT[:], func=AF.Silu), reads=[cT], writes=[scT])
        for l in range(DEPTH):
            adab_l = k.view(adab.ap[:, l, :], f"adab{l}")
            k.barrier()
            ada_layer(k, C, ws, scT, adaw_d[l], adab_l, modv, l, NG)
        for l in range(DEPTH):
            for g in range(NG):
                for (j, off, gn) in ((0, 16, gmix), (1, 64, gch)):
                    k.op("dve", lambda e, l=l, g=g, j=j, off=off: e.tensor_scalar(out=der[:, l, g, j, :], in0=modv[:, l, g, off:off + 16], scalar1=1.0, scalar2=None, op0=ALU.add),
                         reads=[modv], writes=[der])
                    k.op("dve", lambda e, l=l, g=g, j=j, gn=gn: e.tensor_tensor(out=der[:, l, g, j, :], in0=der[:, l, g, j, :], in1=gn[:, l, :], op=ALU.mult), reads=[der, gn], writes=[der])
        k.barrier()

        def vec(ap, name):
            return k.view(ap, name)

        def mods(l, g):
            return dict(gm1=vec(der.ap[:, l, g, 0, :], "gm1"), sh1=vec(modv.ap[:, l, g, 0:16], "sh1"), g1=vec(modv.ap[:, l, g, 32:48], "g1"),
                        gm2=vec(der.ap[:, l, g, 1, :], "gm2"), sh2=vec(modv.ap[:, l, g, 48:64], "sh2"), g2=vec(modv.ap[:, l, g, 80:96], "g2"))
        MODS = [[mods(l, g) for g in range(NG)] for l in range(DEPTH)]
        SGW = [dict(w_in=sgu_win_d[i], b_in=sgu_bin_d[i], ln_g=sgu_lng_d[i], ln_b=sgu_lnb_d[i], w_out=sgu_wout_d[i],
                    wsT=vec(wsT.ap[:, i], f"wsT{i}"), bs=vec(bs.ap[:, i], f"bs{i}")) for i in range(2)]
        CVW = dict(w_in=cwin_d, w_out=cwout_d, b_out=cbout_d, b_in=cbin, dw=cdw, dw_b=cdwb, ln_g=clng, ln_b=clnb)
        ATW = dict(w_qkv=wqkv_d, b_qkv=bqkv_d, w_o=wo_d, bq=bq, sink=sink)

        def run_tile(grp, T, x_src, first, last, si, mode="full", attn_first=None, Dm_first=None):
            k.dma("sp", lambda e: e.dma_start(out=xT[:], in_=x_src), xT, writes=[xT])
            isgu = 0
            afirst = first if attn_first is None else attn_first
            for l in range(DEPTH):
                M = MODS[l][grp]
                k.barrier()
                if KIND[l] == 0:
                    vout = sv_s[isgu, si] if si is not None else None
                    sgu(k, C, ws, xT, M["gm1"], M["sh1"], M["g1"], SGW[isgu], SG, T=T, v_out=vout)
                    isgu += 1
                elif KIND[l] == 1:
                    if si is not None:
                        conv_load_hist_state(k, C, CV, st_d[si])
                    elif first:
                        conv_load_hist_zero(k, CV)
                    so = cs_s[si] if si is not None else (cs_p if last else None)
                    conv(k, C, ws, xT, M["gm1"], M["sh1"], M["g1"], CVW, CV, T=T, state_out=so, carry=(si is None))
                    if mode == "h0":
                        k.barrier()
                        return
                else:
                    if si is not None:
                        attn_load_prev(k, C, AT, ck_d[si], cv_d[si])
                        cpy = k.view(kw_s[si], f"kws_cpy{si}")
                        k.dma("sp", lambda e, si=si: e.dma_start(out=kw_s[si, 0:112, :], in_=ck_d[si, 16:128, :]), cpy, writes=[cpy])
                        k.dma("sp", lambda e, si=si: e.dma_start(out=vw_s[si, 0:112, :], in_=cv_d[si, 16:128, :]), cpy, writes=[cpy])
                        attn(k, C, ws, xT, M["gm1"], M["sh1"], M["g1"], ATW, AT, A.Dsamp, T=T, kwin_out=kw_s[si], vwin_out=vw_s[si], win_rows=(112, 16), carry=False)
                    else:
                        if afirst:
                            k.op("dve", lambda e: e.memset(AT.kTprev[:, :, :], 0.0), writes=[AT.kTprev])
                            k.op("dve", lambda e: e.memset(AT.vprev[:, :], 0.0), writes=[AT.vprev])
                        Dm = A.Dfirst if afirst else (Dm_first if Dm_first is not None else A.Dmid)
                        attn(k, C, ws, xT, M["gm1"], M["sh1"], M["g1"], ATW, AT, Dm, T=T,
                             kwin_out=kw_p if last else None, vwin_out=vw_p if last else None, win_rows=(0, 128) if last else None)
                        if mode == "h1":
                            k.barrier()
                            return
                k.barrier()
                k.dma("pool", lambda e, l=l: e.dma_start(out=skslot[:], in_=skT_d[:, l]), skslot, writes=[skslot])
                peer(k, C, xT, M["gm2"], M["sh2"], M["g2"], PeerW(k, pwq_d[l], skslot, pu_d[l], pv_d[l]), PE, T=128, ws=ws, mid_barrier=True)
            k.barrier()
            return

        FN = NS(); FN.hT32, FN.out_tm = hT32, acc
        if halo:
            run_tile(0, 128, xp_d[0], True, False, None, mode="h0")
            run_tile(0, 128, xp_d[1], False, False, None, mode="h1", attn_first=True)
            k.op("dve", lambda e: e.tensor_scalar(out=CV.xp[:, :, 0:CW - 1], in0=CV.xp[:, :, 0:CW - 1], scalar1=flag[:, 0:1], scalar2=None, op0=ALU.mult),
                 reads=[CV.xp, flag], writes=[CV.xp])
        for t in range(ntile):
            if halo:
                run_tile(0, 128, xp_d[nh + t], False, t == ntile - 1, None, attn_first=False, Dm_first=(Dsel if t == 0 else None))
            else:
                run_tile(0, 128, xp_d[t], t == 0, t == ntile - 1, None)
            final_norm_out(k, C, xT, gfin, zero16, FN, y_p[t * 128:(t + 1) * 128, :], T=128)
        for si in range(nsamp):
            run_tile(1 + si, 16, xs_d[si], False, False, si)
            final_norm_out(k, C, xT, gfin, zero16, FN, y_s[si], T=16)
        k.finish()
        print("[kernel] instructions:", k.n, "sbuf bytes left:", nc.sbuf_bytes_remaining, "weight slots:", nsl, flush=True)
    return nc


def _fm(v):
    v = np.asarray(v)
    lead = v.shape[:-1]
    return np.ascontiguousarray(np.moveaxis(v.reshape(*lead, -1, 128), -1, 0))


def make_in_maps(I, ncores, ntile, nsamp, halo=True):
    f32 = lambda a: np.ascontiguousarray(np.asarray(a), dtype=np.float32)
    shared = {}
    shared["g_mix_fm"] = _fm(I["norm_mix_g"]); shared["g_ch_fm"] = _fm(I["norm_ch_g"]); shared["g_fin_fm"] = _fm(I["norm_final_g"])
    shared["ada_b_fm"] = _fm(I["ada_b"])
    for l in range(DEPTH):
        shared[f"ada_w{l}"] = f32(I["ada_w"][l]); shared[f"peer_w_q{l}"] = f32(I["peer_w_q"][l])
        shared[f"peer_u{l}"] = f32(I["peer_u"][l]); shared[f"peer_v{l}"] = f32(I["peer_v"][l])
    for i in range(2):
        shared[f"sgu_w_in{i}"] = f32(I["sgu_w_in"][i]); shared[f"sgu_w_out{i}"] = f32(I["sgu_w_out"][i])
    shared["sgu_b_in"] = f32(I["sgu_b_in"]); shared["sgu_ln_g"] = f32(I["sgu_ln_g"]); shared["sgu_ln_b"] = f32(I["sgu_ln_b"])
    shared["sgu_wsT"] = np.ascontiguousarray(np.asarray(I["sgu_w_s"]).transpose(3, 0, 1, 2))
    shared["sgu_bs_tm"] = np.ascontiguousarray(np.asarray(I["sgu_b_s"]).transpose(2, 0, 1))
    shared["conv_w_in"] = f32(I["conv_w_in"][0]); shared["conv_w_out"] = f32(I["conv_w_out"][0]); shared["conv_b_out"] = f32(I["conv_b_out"][0])
    shared["conv_b_in_fm"] = _fm(I["conv_b_in"][0]); shared["conv_dw_b_fm"] = _fm(I["conv_dw_b"][0])
    shared["conv_ln_g_fm"] = _fm(I["conv_ln_g"][0]); shared["conv_ln_b_fm"] = _fm(I["conv_ln_b"][0])
    shared["conv_dwT"] = np.ascontiguousarray(np.asarray(I["conv_dw"][0]).T.reshape(16, 128, 31).transpose(1, 0, 2))
    shared["attn_w_qkv"] = f32(I["attn_w_qkv"][0]); shared["attn_b_qkv"] = f32(I["attn_b_qkv"][0]); shared["attn_w_o"] = f32(I["attn_w_o"][0])
    shared["attn_bq_fm"] = np.ascontiguousarray(np.asarray(I["attn_b_qkv"][0][:2048]).reshape(32, 64).T); shared["attn_sinks"] = f32(I["attn_sinks"][0])
    shared["peer_skT"] = np.ascontiguousarray(np.asarray(I["peer_subkeys"]).reshape(DEPTH, 16, 128, 128).transpose(3, 0, 1, 2))
    xp = np.asarray(I["x_prompt"]); xs = np.asarray(I["x_sample"])
    in_maps = []
    for c in range(ncores):
        m = dict(shared)
        if halo:
            b, half = c // 2, c % 2
            rows = np.zeros(((NHALO + ntile) * 128, 2048), np.float32)
            if half:
                rows[:NHALO * 128] = xp[b, ntile * 128 - NHALO * 128:ntile * 128]
            rows[NHALO * 128:] = xp[b, half * ntile * 128:(half + 1) * ntile * 128]
            m["xp_fm"] = np.ascontiguousarray(rows.reshape(NHALO + ntile, 128, 16, 128).transpose(0, 3, 2, 1))
            m["flag"] = np.full((128, 1), float(half), np.float32)
        else:
            m["xp_fm"] = np.ascontiguousarray(xp[c].reshape(ntile, 128, 16, 128).transpose(0, 3, 2, 1))
            m["flag"] = np.ones((128, 1), np.float32)
        xsp = np.zeros((nsamp, 128, 2048), np.float32); xsp[:, :16] = xs[nsamp * c:nsamp * c + nsamp]
        m["xs_fm"] = np.ascontiguousarray(xsp.reshape(nsamp, 128, 16, 128).transpose(0, 3, 2, 1))
        cc = np.stack([np.asarray(I["c_prompt"])[c // 2 if halo else c]] + [np.asarray(I["c_sample"])[nsamp * c + i] for i in range(nsamp)])
        m["cT"] = np.ascontiguousarray(cc.reshape(1 + nsamp, 16, 128).transpose(2, 1, 0))
        m["cache_k"] = f32(np.asarray(I["cache_k_win"])[0, nsamp * c:nsamp * c + nsamp].reshape(nsamp, 128, 256))
        m["cache_v"] = f32(np.asarray(I["cache_v_win"])[0, nsamp * c:nsamp * c + nsamp].reshape(nsamp, 128, 256))
        m["state_conv"] = f32(np.asarray(I["state_conv"])[0, nsamp * c:nsamp * c + nsamp])
        in_maps.append(m)
    return in_maps


def kernel(**I):
    in_maps = make_in_maps(I, NCORES, NTILE, 1, halo=True)
    nc = build_nc(NTILE, 1, True)
    res = run_bass_kernel_spmd(nc, in_maps, core_ids=list(range(NCORES))).results
    R = lambda key: np.stack([np.asarray(r[key]) for r in res])
    odd = lambda key: np.stack([np.asarray(res[c][key]) for c in range(1, NCORES, 2)])
    y_prompt = R("y_p").reshape(4, 4096, 2048)
    y_sample = R("y_s").reshape(8, 16, 2048)
    conv_state_prompt = odd("cs_p").reshape(1, 4, 30, 2048)
    k_win_prompt = odd("kw_p").reshape(1, 4, 128, 4, 64); v_win_prompt = odd("vw_p").reshape(1, 4, 128, 4, 64)
    conv_state_sample = R("cs_s").reshape(1, 8, 30, 2048)
    k_win_sample = R("kw_s").reshape(1, 8, 128, 4, 64); v_win_sample = R("vw_s").reshape(1, 8, 128, 4, 64)
    sgu_v_sample = np.ascontiguousarray(R("sv_s").transpose(1, 0, 2, 3, 4)).reshape(2, 8, 16, 6144)
    return tuple(np.ascontiguousarray(a, dtype=np.float32) for a in (y_prompt, y_sample, conv_state_prompt, k_win_prompt, v_win_prompt,
                                                                     conv_state_sample, k_win_sample, v_win_sample, sgu_v_sample))
```

```python
import numpy as np
from contextlib import ExitStack
import concourse.bass as bass
import concourse.mybir as mybir
from concourse.bass_utils import run_bass_kernel_spmd

F32 = mybir.dt.float32
BF16 = mybir.dt.bfloat16
I32 = mybir.dt.int32
U32 = mybir.dt.uint32
ALU = mybir.AluOpType
AF = mybir.ActivationFunctionType
AX = mybir.AxisListType


class Ctr:
    LIM = 30000

    def __init__(self, K, name, step):
        self.K, self.name, self.step = K, name, step
        self.sems = []
        self._new()

    def _new(self):
        s = self.K.stack.enter_context(self.K.nc.semaphore(f"{self.name}_{len(self.sems)}"))
        self.sems.append(s)
        self.val = 0

    def next(self):
        if self.val + self.step > self.LIM:
            self._new()
        self.val += self.step
        return (self.sems[-1], self.val)


class Buf:
    def __init__(self, K, ap, name):
        self.K, self.ap, self.name = K, ap, name
        self.w = None
        self.r = {}
        self._d = None

    @property
    def dctr(self):
        if self._d is None:
            self._d = Ctr(self.K, "d_" + self.name, 16)
        return self._d

    def __getitem__(self, k):
        return self.ap[k]


class Eng:
    def __init__(self, K, name, obj, counted):
        self.name, self.obj = name, obj
        self.ctr = Ctr(K, "e_" + name, 1) if counted else None
        self.known = {}
        self.prog = []

    def filt(self, deps):
        best = {}
        for (s, v) in deps:
            if self.name == "pe" and self.ctr is not None and s in self.ctr.sems:
                continue
            if self.known.get(s, 0) >= v:
                continue
            if best.get(s, 0) < v:
                best[s] = v
        for s, v in best.items():
            self.known[s] = v
        return list(best.items())


class K:
    def __init__(self, nc, stack):
        self.nc, self.stack = nc, stack
        self.eng = {
            "pe": Eng(self, "pe", nc.tensor, True),
            "act": Eng(self, "act", nc.scalar, True),
            "dve": Eng(self, "dve", nc.vector, True),
            "pool": Eng(self, "pool", nc.gpsimd, True),
            "sp": Eng(self, "sp", nc.sync, False),
        }
        self.bufs = []
        self.n = 0

    def sb(self, name, shape, dt):
        t = self.stack.enter_context(self.nc.sbuf_tensor(name, list(shape), dt))
        b = Buf(self, t, name)
        self.bufs.append(b)
        return b

    def sb_once(self, name, shape, dt):
        if not hasattr(self, "_once"):
            self._once = {}
        if name not in self._once:
            self._once[name] = self.sb(name, shape, dt)
        return self._once[name]

    def ps(self, name, shape, dt):
        t = self.stack.enter_context(self.nc.psum_tensor(name, list(shape), dt))
        b = Buf(self, t, name)
        self.bufs.append(b)
        return b

    def view(self, ap, name):
        b = Buf(self, ap, name)
        self.bufs.append(b)
        return b

    def _deps(self, reads, writes):
        deps = []
        for b in reads:
            if b.w:
                deps.append(b.w)
        for b in writes:
            if b.w:
                deps.append(b.w)
            deps.extend(b.r.items())
        return deps

    def _commit(self, tok, reads, writes):
        for b in reads:
            if b.r.get(tok[0], 0) < tok[1]:
                b.r[tok[0]] = tok[1]
        for b in writes:
            b.w = tok
            b.r = {}

    def op(self, eng, fn, reads=(), writes=()):
        E = self.eng[eng]
        waits = E.filt(self._deps(reads, writes))
        tok = E.ctr.next()
        E.prog.append((waits, fn, tok, 1))
        self._commit(tok, reads, writes)
        self.n += 1

    def dma(self, q, fn, cbuf, reads=(), writes=()):
        E = self.eng[q]
        waits = E.filt(self._deps(reads, writes))
        tok = cbuf.dctr.next()
        E.prog.append((waits, fn, tok, 16))
        self._commit(tok, reads, writes)
        self.n += 1

    def barrier(self):
        deps = []
        for b in self.bufs:
            if b.w:
                deps.append(b.w)
            deps.extend(b.r.items())
        for E in self.eng.values():
            waits = E.filt(deps)
            if waits:
                E.prog.append((waits, None, None, 0))
        for b in self.bufs:
            b.w = None
            b.r = {}

    def finish(self):
        E = self.eng["sp"]
        deps = []
        for b in self.bufs:
            if b.w:
                deps.append(b.w)
            deps.extend(b.r.items())
        waits = E.filt(deps)
        E.prog.append((waits, None, None, 0))
        nc = self.nc

        def replay(name, e):
            for (waits, fn, tok, inc) in self.eng[name].prog:
                for (s, v) in waits:
                    e.wait_ge(s, v)
                if fn is not None:
                    ins = fn(e)
                    ins.then_inc(tok[0], inc)

        with nc.Block() as block:
            @block.sync
            def _(e):
                replay("sp", e)

            @block.tensor
            def _(e):
                replay("pe", e)

            @block.scalar
            def _(e):
                replay("act", e)

            @block.vector
            def _(e):
                replay("dve", e)

            @block.gpsimd
            def _(e):
                replay("pool", e)


D = 2048
NCH = 16
PH, PK, PTOP = 8, 128, 16
NHC = 16
NEG = -1e30


class Common:
    def __init__(self, k):
        self.k = k
        identi = k.sb("identi", [128, 128], I32)
        self.ident = k.sb("ident", [128, 128], F32)
        k.op("pool", lambda e: e.iota(identi[:], pattern=[[1, 128]], base=0, channel_multiplier=-1), writes=[identi])
        k.op("dve", lambda e: e.tensor_single_scalar(out=self.ident[:], in_=identi[:], scalar=0, op=ALU.is_equal),
             reads=[identi], writes=[self.ident])
        self.ones_bf = k.sb("ones_bf", [128, 128], BF16)
        k.op("dve", lambda e: e.memset(self.ones_bf[:], 1.0), writes=[self.ones_bf])
        io16 = k.sb("io16i", [128, 16], I32)
        self.iota16 = k.sb("iota16", [128, 16], F32)
        k.op("pool", lambda e: e.iota(io16[:], pattern=[[1, 16]], base=0, channel_multiplier=0), writes=[io16])
        k.op("dve", lambda e: e.tensor_copy(out=self.iota16[:], in_=io16[:]), reads=[io16], writes=[self.iota16])
        self.bank = [k.ps(f"bank{i}", [128, 512], F32) for i in range(8)]


def rms_modulate(k, C, xT, gm, sh, hT32, hTb, T=128):
    sq = k.sb_once("rm_sq", [128, NCH, 128], BF16)
    rs = k.sb_once("rm_rstd", [128, 128], F32)
    pb = C.bank[0]
    k.op("act", lambda e: e.activation(out=sq[:, :, 0:T], in_=xT[:, :, 0:T], func=AF.Square), reads=[xT], writes=[sq])
    for c in range(NCH):
        k.op("pe", lambda e, c=c: e.matmul(pb[:, 0:T], lhsT=C.ones_bf[:], rhs=sq[:, c, 0:T], start=(c == 0), stop=(c == NCH - 1)),
             reads=[sq, C.ones_bf], writes=[pb])
    k.op("dve", lambda e: e.tensor_scalar(out=rs[:, 0:T], in0=pb[:, 0:T], scalar1=1.0 / D, scalar2=1e-6, op0=ALU.mult, op1=ALU.add),
         reads=[pb], writes=[rs])
    k.op("act", lambda e: e.activation(out=rs[:, 0:T], in_=rs[:, 0:T], func=AF.Sqrt), reads=[rs], writes=[rs])
    k.op("dve", lambda e: e.reciprocal(out=rs[:, 0:T], in_=rs[:, 0:T]), reads=[rs], writes=[rs])
    k.op("dve", lambda e: e.tensor_tensor(out=hT32[:, :, 0:T], in0=xT[:, :, 0:T],
                                          in1=rs[:, 0:T].unsqueeze(1).to_broadcast([128, NCH, T]), op=ALU.mult),
         reads=[xT, rs], writes=[hT32])
    k.op("dve", lambda e: e.tensor_tensor(out=hT32[:, :, 0:T], in0=hT32[:, :, 0:T],
                                          in1=gm[:, :].unsqueeze(2).to_broadcast([128, NCH, T]), op=ALU.mult),
         reads=[hT32, gm], writes=[hT32])
    k.op("dve", lambda e: e.tensor_tensor(out=hT32[:, :, 0:T], in0=hT32[:, :, 0:T],
                                          in1=sh[:, :].unsqueeze(2).to_broadcast([128, NCH, T]), op=ALU.add),
         reads=[hT32, sh], writes=[hT32])
    if hTb is not None:
        k.op("act", lambda e: e.copy(out=hTb[:, :, 0:T], in_=hT32[:, :, 0:T]), reads=[hT32], writes=[hTb])


def to_token_major(k, C, srcT, dst, T=128):
    for b4 in range(4):
        pb = C.bank[1 + (b4 % 2)]
        for j in range(4):
            c = b4 * 4 + j
            k.op("pe", lambda e, c=c, j=j, pb=pb: e.transpose(pb[0:T, j * 128:(j + 1) * 128], srcT[:, c, 0:T], C.ident[:]),
                 reads=[srcT, C.ident], writes=[pb])
        k.op("act", lambda e, b4=b4, pb=pb: e.copy(out=dst[0:T, b4 * 512:(b4 + 1) * 512], in_=pb[0:T, :]), reads=[pb], writes=[dst])


def gated_residual_add(k, C, xT, out_tm, gate, T=128):
    for b4 in range(4):
        pb = C.bank[1 + (b4 % 2)]
        for j in range(4):
            c = b4 * 4 + j
            k.op("pe", lambda e, c=c, j=j, pb=pb: e.transpose(pb[:, j * 128:j * 128 + T], out_tm[0:T, c * 128:(c + 1) * 128], C.ident[0:T, 0:T]),
                 reads=[out_tm, C.ident], writes=[pb])
        for j in range(4):
            c = b4 * 4 + j
            k.op("dve", lambda e, c=c, j=j, pb=pb: e.scalar_tensor_tensor(out=xT[:, c, 0:T], in0=pb[:, j * 128:j * 128 + T], scalar=gate[:, c:c + 1],
                                                                          in1=xT[:, c, 0:T], op0=ALU.mult, op1=ALU.add),
                 reads=[pb, gate, xT], writes=[xT])


class PeerW:
    def __init__(self, k, wq_b, skT_b, u_tab, v_tab):
        self.wq, self.skT, self.u, self.v = wq_b, skT_b, u_tab, v_tab


def peer(k, C, xT, gm, sh, gate, W, S, T=128, ws=None, mid_barrier=False):
    rms_modulate(k, C, xT, gm, sh, S.hT32, S.hTb, T)
    to_token_major(k, C, S.hT32, S.h_tm, T)
    if ws is not None:
        peer_q_streamed(k, C, ws, W.wq, S, T)
    else:
        for hc in range(NHC):
            pb = C.bank[3 + (hc % 2)]
            for c in range(NCH):
                k.op("pe", lambda e, hc=hc, c=c, pb=pb: e.matmul(pb[:, 0:T], lhsT=W.wq[:, c, hc * 128:(hc + 1) * 128], rhs=S.hTb[:, c, 0:T],
                                                                start=(c == 0), stop=(c == NCH - 1)), reads=[W.wq, S.hTb], writes=[pb])
            k.op("act", lambda e, hc=hc, pb=pb: e.copy(out=S.qT[:, hc, 0:T], in_=pb[:, 0:T]), reads=[pb], writes=[S.qT])
    for hc in range(NHC):
        pb = C.bank[5 + (hc // 4) % 2]
        k.op("pe", lambda e, hc=hc, pb=pb: e.matmul(pb[0:T, (hc % 4) * 128:(hc % 4 + 1) * 128], lhsT=S.qT[:, hc, 0:T], rhs=W.skT[:, hc, :], start=True, stop=True),
             reads=[S.qT, W.skT], writes=[pb])
        if hc % 4 == 3:
            k.op("act", lambda e, hc=hc, pb=pb: e.copy(out=S.s[0:T, hc - 3:hc + 1, :], in_=pb[0:T, :].rearrange("p (a b) -> p a b", a=4)), reads=[pb], writes=[S.s])
    for hc in range(NHC):
        k.op("dve", lambda e, hc=hc: e.max(out=S.sv[0:T, hc, 0:8], in_=S.s[0:T, hc, :]), reads=[S.s], writes=[S.sv])
        k.op("dve", lambda e, hc=hc: e.max_index(out=S.si[0:T, hc, 0:8], in_max=S.sv[0:T, hc, 0:8], in_values=S.s[0:T, hc, :]), reads=[S.s, S.sv], writes=[S.si])
        k.op("dve", lambda e, hc=hc: e.match_replace(out=S.s2[0:T, hc, :], in_to_replace=S.sv[0:T, hc, 0:8], in_values=S.s[0:T, hc, :], imm_value=NEG), reads=[S.s, S.sv], writes=[S.s2])
        k.op("dve", lambda e, hc=hc: e.max(out=S.sv[0:T, hc, 8:16], in_=S.s2[0:T, hc, :]), reads=[S.s2], writes=[S.sv])
        k.op("dve", lambda e, hc=hc: e.max_index(out=S.si[0:T, hc, 8:16], in_max=S.sv[0:T, hc, 8:16], in_values=S.s2[0:T, hc, :]), reads=[S.s2, S.sv], writes=[S.si])
    k.op("dve", lambda e: e.tensor_copy(out=S.sif[0:T], in_=S.si[0:T]), reads=[S.si], writes=[S.sif])
    sv4 = S.sv.ap.rearrange("p (h c) j -> p h c j", c=2)
    sif4 = S.sif.ap.rearrange("p (h c) j -> p h c j", c=2)
    for h in range(PH):
        k.op("dve", lambda e, h=h: e.tensor_tensor(out=S.cand[0:T, h, :, :], in0=sv4[0:T, h, 0, :].unsqueeze(2).to_broadcast([T, 16, 16]),
                                                   in1=sv4[0:T, h, 1, :].unsqueeze(1).to_broadcast([T, 16, 16]), op=ALU.add), reads=[S.sv], writes=[S.cand])
    candf = S.cand.ap.rearrange("p h a b -> p h (a b)")
    cand2f = S.cand2.ap.rearrange("p h a b -> p h (a b)")
    for h in range(PH):
        k.op("dve", lambda e, h=h: e.max(out=S.best[0:T, h, 0:8], in_=candf[0:T, h, :]), reads=[S.cand], writes=[S.best])
        k.op("dve", lambda e, h=h: e.max_index(out=S.bi[0:T, h, 0:8], in_max=S.best[0:T, h, 0:8], in_values=candf[0:T, h, :]), reads=[S.cand, S.best], writes=[S.bi])
        k.op("dve", lambda e, h=h: e.match_replace(out=cand2f[0:T, h, :], in_to_replace=S.best[0:T, h, 0:8], in_values=candf[0:T, h, :], imm_value=NEG), reads=[S.cand, S.best], writes=[S.cand2])
        k.op("dve", lambda e, h=h: e.max(out=S.best[0:T, h, 8:16], in_=cand2f[0:T, h, :]), reads=[S.cand2], writes=[S.best])
        k.op("dve", lambda e, h=h: e.max_index(out=S.bi[0:T, h, 8:16], in_max=S.best[0:T, h, 8:16], in_values=cand2f[0:T, h, :]), reads=[S.cand2, S.best], writes=[S.bi])
    k.op("dve", lambda e: e.tensor_single_scalar(out=S.b1[0:T], in_=S.bi[0:T], scalar=4, op=ALU.logical_shift_right), reads=[S.bi], writes=[S.b1])
    k.op("dve", lambda e: e.tensor_single_scalar(out=S.b2[0:T], in_=S.bi[0:T], scalar=15, op=ALU.bitwise_and), reads=[S.bi], writes=[S.b2])
    k.op("dve", lambda e: e.tensor_copy(out=S.b1f[0:T], in_=S.b1[0:T]), reads=[S.b1], writes=[S.b1f])
    k.op("dve", lambda e: e.tensor_copy(out=S.b2f[0:T], in_=S.b2[0:T]), reads=[S.b2], writes=[S.b2f])
    for (bf, half, dst) in ((S.b1f, 0, S.I1), (S.b2f, 1, S.I2)):
        for h in range(PH):
            k.op("dve", lambda e, bf=bf, h=h: e.tensor_tensor(out=S.oh[0:T], in0=C.iota16[0:T, :].unsqueeze(1).to_broadcast([T, 16, 16]),
                                                              in1=bf[0:T, h, :].unsqueeze(2).to_broadcast([T, 16, 16]), op=ALU.is_equal), reads=[C.iota16, bf], writes=[S.oh])
            k.op("dve", lambda e, h=h, half=half: e.tensor_tensor(out=S.oh[0:T], in0=S.oh[0:T], in1=sif4[0:T, h, half, :].unsqueeze(1).to_broadcast([T, 16, 16]), op=ALU.mult),
                 reads=[S.oh, S.sif], writes=[S.oh])
            k.op("dve", lambda e, h=h, dst=dst: e.tensor_reduce(out=dst[0:T, h, :], in_=S.oh[0:T], axis=AX.X, op=ALU.add), reads=[S.oh], writes=[dst])
    k.op("dve", lambda e: e.scalar_tensor_tensor(out=S.ef[0:T], in0=S.I1[0:T], scalar=float(PK), in1=S.I2[0:T], op0=ALU.mult, op1=ALU.add), reads=[S.I1, S.I2], writes=[S.ef])
    k.op("dve", lambda e: e.tensor_copy(out=S.ei[0:T], in_=S.ef[0:T]), reads=[S.ef], writes=[S.ei])
    k.op("dve", lambda e: e.tensor_tensor(out=S.g[0:T], in0=S.best[0:T], in1=S.best[0:T, :, 0:1].to_broadcast([T, PH, 16]), op=ALU.subtract), reads=[S.best], writes=[S.g])
    k.op("act", lambda e: e.activation(out=S.g[0:T], in_=S.g[0:T], func=AF.Exp), reads=[S.g], writes=[S.g])
    k.op("dve", lambda e: e.tensor_reduce(out=S.gs[0:T], in_=S.g[0:T], axis=AX.X, op=ALU.add), reads=[S.g], writes=[S.gs])
    k.op("dve", lambda e: e.reciprocal(out=S.gs[0:T], in_=S.gs[0:T]), reads=[S.gs], writes=[S.gs])
    k.op("dve", lambda e: e.tensor_tensor(out=S.g[0:T], in0=S.g[0:T], in1=S.gs[0:T].unsqueeze(2).to_broadcast([T, PH, 16]), op=ALU.mult), reads=[S.g, S.gs], writes=[S.g])
    if mid_barrier:
        k.barrier()
    eif = S.ei.ap.rearrange("p h k -> p (h k)")
    af = S.a.ap.rearrange("p h k -> p (h k)")
    nb = len(S.gbuf)
    for hk in range(PH * PTOP):
        gb = S.gbuf[hk % nb]
        k.dma("pool", lambda e, hk=hk, gb=gb: e.indirect_dma_start(out=gb[0:T, :], out_offset=None, in_=W.u,
              in_offset=bass.IndirectOffsetOnAxis(ap=eif[0:T, hk:hk + 1], axis=0)), gb, reads=[S.ei], writes=[gb])
        k.op("dve", lambda e, hk=hk, gb=gb: e.scalar_tensor_tensor(out=S.junk[0:T], in0=gb[0:T, :], scalar=1.0, in1=S.h_tm[0:T, :], op0=ALU.mult, op1=ALU.mult,
                                                                   accum_out=af[0:T, hk:hk + 1]), reads=[gb, S.h_tm], writes=[S.junk, S.a])
    k.op("act", lambda e: e.activation(out=S.w[0:T], in_=S.a[0:T], func=AF.Gelu_apprx_tanh), reads=[S.a], writes=[S.w])
    k.op("dve", lambda e: e.tensor_tensor(out=S.w[0:T], in0=S.w[0:T], in1=S.g[0:T], op=ALU.mult), reads=[S.w, S.g], writes=[S.w])
    wf = S.w.ap.rearrange("p h k -> p (h k)")
    for hk in range(PH * PTOP):
        gb = S.gbuf[hk % nb]
        k.dma("pool", lambda e, hk=hk, gb=gb: e.indirect_dma_start(out=gb[0:T, :], out_offset=None, in_=W.v,
              in_offset=bass.IndirectOffsetOnAxis(ap=eif[0:T, hk:hk + 1], axis=0)), gb, reads=[S.ei], writes=[gb])
        if hk == 0:
            k.op("dve", lambda e, gb=gb: e.tensor_scalar(out=S.acc[0:T, :], in0=gb[0:T, :], scalar1=wf[0:T, 0:1], scalar2=None, op0=ALU.mult), reads=[gb, S.w], writes=[S.acc])
        else:
            k.op("dve", lambda e, hk=hk, gb=gb: e.scalar_tensor_tensor(out=S.acc[0:T, :], in0=gb[0:T, :], scalar=wf[0:T, hk:hk + 1], in1=S.acc[0:T, :], op0=ALU.mult, op1=ALU.add),
                 reads=[gb, S.w, S.acc], writes=[S.acc])
    gated_residual_add(k, C, xT, S.acc, gate, T)


class NS:
    pass


def peer_scratch(k, ngb=4):
    S = NS()
    S.hT32 = k.sb_once("p_hT32", [128, NCH, 128], F32)
    S.hTb = k.sb_once("p_hTb", [128, NCH, 128], BF16)
    S.h_tm = k.sb("p_h_tm", [128, D], F32)
    S.qT = k.sb("p_qT", [128, NHC, 128], BF16)
    S.s = k.sb("p_s", [128, NHC, PK], F32)
    S.s2 = k.sb("p_s2", [128, NHC, PK], F32)
    S.sv = k.sb("p_sv", [128, NHC, 16], F32)
    S.si = k.sb("p_si", [128, NHC, 16], U32)
    S.sif = k.sb("p_sif", [128, NHC, 16], F32)
    S.cand = k.sb("p_cand", [128, PH, 16, 16], F32)
    S.cand2 = k.sb("p_cand2", [128, PH, 16, 16], F32)
    S.best = k.sb("p_best", [128, PH, 16], F32)
    S.bi = k.sb("p_bi", [128, PH, 16], U32)
    S.b1 = k.sb("p_b1", [128, PH, 16], U32)
    S.b2 = k.sb("p_b2", [128, PH, 16], U32)
    S.b1f = k.sb("p_b1f", [128, PH, 16], F32)
    S.b2f = k.sb("p_b2f", [128, PH, 16], F32)
    S.oh = k.sb("p_oh", [128, 16, 16], F32)
    S.I1 = k.sb("p_I1", [128, PH, 16], F32)
    S.I2 = k.sb("p_I2", [128, PH, 16], F32)
    S.ef = k.sb("p_ef", [128, PH, 16], F32)
    S.ei = k.sb("p_ei", [128, PH, 16], I32)
    S.g = k.sb("p_g", [128, PH, 16], F32)
    S.gs = k.sb("p_gs", [128, PH], F32)
    S.a = k.sb("p_a", [128, PH, 16], F32)
    S.w = k.sb("p_w", [128, PH, 16], F32)
    S.junk = k.sb("p_junk", [128, D], F32)
    S.acc = k.sb_once("p_acc", [128, D], F32)
    S.gbuf = [k.sb(f"p_gb{i}", [128, D], F32) for i in range(ngb)]
    return S


class WS:
    def __init__(self, k, nslots=4, q="sp"):
        self.k, self.q, self.i = k, q, 0
        self.slots = [k.sb(f"wslot{i}", [128, 16, 512], BF16) for i in range(nslots)]
        self.bslots = [k.sb(f"bslot{i}", [128, 512], F32) for i in range(4)]
        self.bi = 0

    def block(self, w2d, kc0, col0, nkc=16, ncols=512):
        s = self.slots[self.i % len(self.slots)]
        self.i += 1
        src = w2d[kc0 * 128:(kc0 + nkc) * 128, col0:col0 + ncols].rearrange("(c p) n -> p c n", p=128)
        self.k.dma(self.q, lambda e: e.dma_start(out=s[:, 0:nkc, 0:ncols], in_=src), s, writes=[s])
        return s

    def row_bcast(self, row1d, col0, ncols=512):
        s = self.bslots[self.bi % len(self.bslots)]
        self.bi += 1
        src = row1d[col0:col0 + ncols].unsqueeze(0).partition_broadcast(128)
        self.k.dma(self.q, lambda e: e.dma_start(out=s[:, 0:ncols], in_=src), s, writes=[s])
        return s


SGD = 6144


def sgu(k, C, ws, xT, gm, sh, gate, Wd, S, T=128, v_out=None):
    rms_modulate(k, C, xT, gm, sh, S.hT32, S.hTb, T)
    for nb in range(24):
        slot = ws.block(Wd["w_in"], 0, nb * 512)
        brow = ws.row_bcast(Wd["b_in"], nb * 512)
        pb = C.bank[3 + nb % 2]
        for c in range(NCH):
            k.op("pe", lambda e, c=c, pb=pb, slot=slot: e.matmul(pb[0:T, :], lhsT=S.hTb[:, c, 0:T], rhs=slot[:, c, :], start=(c == 0), stop=(c == NCH - 1)),
                 reads=[S.hTb, slot], writes=[pb])
        off = (nb % 12) * 512
        if nb < 12:
            k.op("dve", lambda e, pb=pb, brow=brow: e.tensor_tensor(out=S.tmp[0:T, :], in0=pb[0:T, :], in1=brow[0:T, :], op=ALU.add), reads=[pb, brow], writes=[S.tmp])
            k.op("act", lambda e, off=off: e.activation(out=S.u[0:T, off:off + 512], in_=S.tmp[0:T, :], func=AF.Gelu_apprx_tanh), reads=[S.tmp], writes=[S.u])
        else:
            k.op("dve", lambda e, pb=pb, brow=brow, off=off: e.tensor_tensor(out=S.v[0:T, off:off + 512], in0=pb[0:T, :], in1=brow[0:T, :], op=ALU.add), reads=[pb, brow], writes=[S.v])
            k.op("act", lambda e, off=off: e.activation(out=S.v[0:T, off:off + 512], in_=S.v[0:T, off:off + 512], func=AF.Gelu_apprx_tanh), reads=[S.v], writes=[S.v])
    k.op("dve", lambda e: e.tensor_reduce(out=S.st[0:T, 0:1], in_=S.v[0:T, :], axis=AX.X, op=ALU.add), reads=[S.v], writes=[S.st])
    k.op("act", lambda e: e.activation(out=S.vb[0:T, :], in_=S.v[0:T, :], func=AF.Square, accum_out=S.st[0:T, 1:2]), reads=[S.v, S.st], writes=[S.vb, S.st])
    k.op("dve", lambda e: e.tensor_scalar(out=S.st[0:T, 0:2], in0=S.st[0:T, 0:2], scalar1=1.0 / SGD, scalar2=None, op0=ALU.mult), reads=[S.st], writes=[S.st])
    k.op("dve", lambda e: e.tensor_tensor(out=S.st[0:T, 2:3], in0=S.st[0:T, 0:1], in1=S.st[0:T, 0:1], op=ALU.mult), reads=[S.st], writes=[S.st])
    k.op("dve", lambda e: e.tensor_tensor(out=S.st[0:T, 2:3], in0=S.st[0:T, 1:2], in1=S.st[0:T, 2:3], op=ALU.subtract), reads=[S.st], writes=[S.st])
    k.op("dve", lambda e: e.tensor_scalar(out=S.st[0:T, 2:3], in0=S.st[0:T, 2:3], scalar1=1e-6, scalar2=None, op0=ALU.add), reads=[S.st], writes=[S.st])
    k.op("act", lambda e: e.activation(out=S.st[0:T, 2:3], in_=S.st[0:T, 2:3], func=AF.Sqrt), reads=[S.st], writes=[S.st])
    k.op("dve", lambda e: e.reciprocal(out=S.st[0:T, 2:3], in_=S.st[0:T, 2:3]), reads=[S.st], writes=[S.st])
    k.op("dve", lambda e: e.tensor_scalar(out=S.v[0:T, :], in0=S.v[0:T, :], scalar1=S.st[0:T, 0:1], scalar2=S.st[0:T, 2:3], op0=ALU.subtract, op1=ALU.mult),
         reads=[S.v, S.st], writes=[S.v])
    for nb in range(12):
        grow = ws.row_bcast(Wd["ln_g"], nb * 512)
        brow = ws.row_bcast(Wd["ln_b"], nb * 512)
        k.op("dve", lambda e, nb=nb, grow=grow: e.tensor_tensor(out=S.v[0:T, nb * 512:(nb + 1) * 512], in0=S.v[0:T, nb * 512:(nb + 1) * 512], in1=grow[0:T, :], op=ALU.mult),
             reads=[S.v, grow], writes=[S.v])
        k.op("dve", lambda e, nb=nb, brow=brow: e.tensor_tensor(out=S.v[0:T, nb * 512:(nb + 1) * 512], in0=S.v[0:T, nb * 512:(nb + 1) * 512], in1=brow[0:T, :], op=ALU.add),
             reads=[S.v, brow], writes=[S.v])
    if v_out is not None:
        k.dma("sp", lambda e: e.dma_start(out=v_out, in_=S.v[0:T, :]), S.v, reads=[S.v])
    k.op("act", lambda e: e.copy(out=S.vb[0:T, :], in_=S.v[0:T, :]), reads=[S.v], writes=[S.vb])
    for g in range(8):
        for (o, n) in ((0, 512), (512, 256)):
            pb = C.bank[5 + (2 * g + (o > 0)) % 2]
            c0 = g * 768 + o
            k.op("pe", lambda e, g=g, pb=pb, c0=c0, n=n: e.matmul(pb[0:T, 0:n], lhsT=Wd["wsT"][0:T, g, 0:T], rhs=S.vb[0:T, c0:c0 + n], start=True, stop=True),
                 reads=[Wd["wsT"], S.vb], writes=[pb])
            k.op("dve", lambda e, g=g, pb=pb, c0=c0, n=n: e.scalar_tensor_tensor(out=S.prod[0:T, c0:c0 + n], in0=pb[0:T, 0:n], scalar=Wd["bs"][0:T, g:g + 1],
                                                                                in1=S.u[0:T, c0:c0 + n], op0=ALU.add, op1=ALU.mult), reads=[pb, Wd["bs"], S.u], writes=[S.prod])
    for b4 in range(12):
        pb = C.bank[1 + b4 % 2]
        for j in range(4):
            c = b4 * 4 + j
            k.op("pe", lambda e, c=c, j=j, pb=pb: e.transpose(pb[:, j * 128:j * 128 + T], S.prod[0:T, c * 128:(c + 1) * 128], C.ident[0:T, 0:T]), reads=[S.prod, C.ident], writes=[pb])
        k.op("act", lambda e, b4=b4, pb=pb: e.copy(out=S.prodT[:, b4 * 4:(b4 + 1) * 4, 0:T], in_=pb[:, :].rearrange("p (a b) -> p a b", a=4)[:, :, 0:T]), reads=[pb], writes=[S.prodT])
    for nb in range(4):
        pb = C.bank[3 + nb % 2]
        for ku in range(3):
            slot = ws.block(Wd["w_out"], ku * 16, nb * 512)
            for c in range(16):
                kc = ku * 16 + c
                k.op("pe", lambda e, kc=kc, c=c, pb=pb, slot=slot: e.matmul(pb[0:T, :], lhsT=S.prodT[:, kc, 0:T], rhs=slot[:, c, :], start=(kc == 0), stop=(kc == 47)),
                     reads=[S.prodT, slot], writes=[pb])
        k.op("act", lambda e, nb=nb, pb=pb: e.copy(out=S.out_tm[0:T, nb * 512:(nb + 1) * 512], in_=pb[0:T, :]), reads=[pb], writes=[S.out_tm])
    gated_residual_add(k, C, xT, S.out_tm, gate, T)


def sgu_scratch(k):
    S = NS()
    S.hT32 = k.sb_once("p_hT32", [128, NCH, 128], F32)
    S.hTb = k.sb_once("p_hTb", [128, NCH, 128], BF16)
    S.u = k.sb("g_u", [128, SGD], BF16)
    S.v = k.sb("g_v", [128, SGD], F32)
    S.vb = k.sb("g_vb", [128, SGD], BF16)
    S.prod = S.v
    S.tmp = k.sb("g_tmp", [128, 512], F32)
    S.prodT = k.sb("g_prodT", [128, 48, 128], BF16)
    S.st = k.sb("g_st", [128, 4], F32)
    S.out_tm = k.sb_once("p_acc", [128, D], F32)
    return S


CW = 31


def conv_load_hist_zero(k, S):
    k.op("dve", lambda e: e.memset(S.xp[:, :, 0:CW - 1], 0.0), writes=[S.xp])


def conv_load_hist_state(k, C, S, state_dram):
    k.dma("sp", lambda e: e.dma_start(out=S.tm30[0:CW - 1, :], in_=state_dram), S.tm30, writes=[S.tm30])
    for b4 in range(4):
        pb = C.bank[1 + b4 % 2]
        for j in range(4):
            c = b4 * 4 + j
            k.op("pe", lambda e, c=c, j=j, pb=pb: e.transpose(pb[:, j * 128:j * 128 + CW - 1], S.tm30[0:CW - 1, c * 128:(c + 1) * 128], C.ident[0:CW - 1, 0:CW - 1]),
                 reads=[S.tm30, C.ident], writes=[pb])
        k.op("act", lambda e, b4=b4, pb=pb: e.copy(out=S.xp[:, b4 * 4:(b4 + 1) * 4, 0:CW - 1], in_=pb[:, :].rearrange("p (a b) -> p a b", a=4)[:, :, 0:CW - 1]),
             reads=[pb], writes=[S.xp])


def conv(k, C, ws, xT, gm, sh, gate, Wd, S, T=128, state_out=None, carry=True):
    H = CW - 1
    rms_modulate(k, C, xT, gm, sh, S.hT32, S.hTb, T)
    for b in range(4):
        sl = ws.block(Wd["w_in"], 0, b * 512)
        sg = ws.block(Wd["w_in"], 0, (b + 4) * 512)
        pl, pg = C.bank[3 + b % 2], C.bank[5 + b % 2]
        for j in range(4):
            for c in range(NCH):
                k.op("pe", lambda e, j=j, c=c, pl=pl, sl=sl: e.matmul(pl[:, j * 128:j * 128 + T], lhsT=sl[:, c, j * 128:(j + 1) * 128], rhs=S.hTb[:, c, 0:T], start=(c == 0), stop=(c == NCH - 1)),
                     reads=[sl, S.hTb], writes=[pl])
            for c in range(NCH):
                k.op("pe", lambda e, j=j, c=c, pg=pg, sg=sg: e.matmul(pg[:, j * 128:j * 128 + T], lhsT=sg[:, c, j * 128:(j + 1) * 128], rhs=S.hTb[:, c, 0:T], start=(c == 0), stop=(c == NCH - 1)),
                     reads=[sg, S.hTb], writes=[pg])
        for j in range(4):
            ch = b * 4 + j
            k.op("act", lambda e, j=j, ch=ch, pg=pg: e.activation(out=S.sg[:, 0:T], in_=pg[:, j * 128:j * 128 + T], func=AF.Sigmoid, bias=Wd["b_in"][:, 16 + ch:17 + ch]),
                 reads=[pg, Wd["b_in"]], writes=[S.sg])
            k.op("dve", lambda e, j=j, ch=ch, pl=pl: e.scalar_tensor_tensor(out=S.xp[:, ch, H:H + T], in0=pl[:, j * 128:j * 128 + T], scalar=Wd["b_in"][:, ch:ch + 1], in1=S.sg[:, 0:T],
                                                                            op0=ALU.add, op1=ALU.mult), reads=[pl, Wd["b_in"], S.sg], writes=[S.xp])
    if state_out is not None:
        for b4 in range(4):
            pb = C.bank[1 + b4 % 2]
            for j in range(4):
                c = b4 * 4 + j
                k.op("pe", lambda e, c=c, j=j, pb=pb: e.transpose(pb[0:H, j * 128:(j + 1) * 128], S.xp[:, c, T:T + H], C.ident[:]), reads=[S.xp, C.ident], writes=[pb])
            k.op("act", lambda e, b4=b4, pb=pb: e.copy(out=S.tm30[0:H, b4 * 512:(b4 + 1) * 512], in_=pb[0:H, :]), reads=[pb], writes=[S.tm30])
        k.dma("sp", lambda e: e.dma_start(out=state_out, in_=S.tm30[0:H, :]), S.tm30, reads=[S.tm30])
    for c in range(NCH):
        k.op("act", lambda e, c=c: e.activation(out=S.y[:, c, 0:T], in_=S.xp[:, c, 0:T], func=AF.Identity, scale=Wd["dw"][:, c, 0:1], bias=Wd["dw_b"][:, c:c + 1]),
             reads=[S.xp, Wd["dw"], Wd["dw_b"]], writes=[S.y])
        for kk in range(1, CW):
            k.op("dve", lambda e, c=c, kk=kk: e.scalar_tensor_tensor(out=S.y[:, c, 0:T], in0=S.xp[:, c, kk:kk + T], scalar=Wd["dw"][:, c, kk:kk + 1], in1=S.y[:, c, 0:T],
                                                                     op0=ALU.mult, op1=ALU.add), reads=[S.xp, Wd["dw"], S.y], writes=[S.y])
    if carry:
        k.op("act", lambda e: e.copy(out=S.hist_tmp[:, :, :], in_=S.xp[:, :, T:T + H]), reads=[S.xp], writes=[S.hist_tmp])
        k.op("act", lambda e: e.copy(out=S.xp[:, :, 0:H], in_=S.hist_tmp[:, :, :]), reads=[S.hist_tmp], writes=[S.xp])
    k.op("act", lambda e: e.copy(out=S.yb[:, :, 0:T], in_=S.y[:, :, 0:T]), reads=[S.y], writes=[S.yb])
    k.op("act", lambda e: e.activation(out=S.y2b[:, :, 0:T], in_=S.y[:, :, 0:T], func=AF.Square), reads=[S.y], writes=[S.y2b])
    pb = C.bank[0]
    for (src, off) in ((S.yb, 0), (S.y2b, 128)):
        for c in range(NCH):
            k.op("pe", lambda e, src=src, off=off, c=c, pb=pb: e.matmul(pb[:, off:off + T], lhsT=C.ones_bf[:], rhs=src[:, c, 0:T], start=(c == 0), stop=(c == NCH - 1)),
                 reads=[src, C.ones_bf], writes=[pb])
    k.op("dve", lambda e, pb=pb: e.tensor_scalar(out=S.mv[:, 0:256], in0=pb[:, 0:256], scalar1=1.0 / D, scalar2=None, op0=ALU.mult), reads=[pb], writes=[S.mv])
    k.op("dve", lambda e: e.tensor_tensor(out=S.mv[:, 256:256 + T], in0=S.mv[:, 0:T], in1=S.mv[:, 0:T], op=ALU.mult), reads=[S.mv], writes=[S.mv])
    k.op("dve", lambda e: e.tensor_tensor(out=S.mv[:, 256:256 + T], in0=S.mv[:, 128:128 + T], in1=S.mv[:, 256:256 + T], op=ALU.subtract), reads=[S.mv], writes=[S.mv])
    k.op("dve", lambda e: e.tensor_scalar(out=S.mv[:, 256:256 + T], in0=S.mv[:, 256:256 + T], scalar1=1e-6, scalar2=None, op0=ALU.add), reads=[S.mv], writes=[S.mv])
    k.op("act", lambda e: e.activation(out=S.mv[:, 256:256 + T], in_=S.mv[:, 256:256 + T], func=AF.Sqrt), reads=[S.mv], writes=[S.mv])
    k.op("dve", lambda e: e.reciprocal(out=S.mv[:, 256:256 + T], in_=S.mv[:, 256:256 + T]), reads=[S.mv], writes=[S.mv])
    k.op("dve", lambda e: e.tensor_tensor(out=S.y[:, :, 0:T], in0=S.y[:, :, 0:T], in1=S.mv[:, 0:T].unsqueeze(1).to_broadcast([128, NCH, T]), op=ALU.subtract), reads=[S.y, S.mv], writes=[S.y])
    k.op("dve", lambda e: e.tensor_tensor(out=S.y[:, :, 0:T], in0=S.y[:, :, 0:T], in1=S.mv[:, 256:256 + T].unsqueeze(1).to_broadcast([128, NCH, T]), op=ALU.mult), reads=[S.y, S.mv], writes=[S.y])
    for c in range(NCH):
        k.op("act", lambda e, c=c: e.activation(out=S.yb[:, c, 0:T], in_=S.y[:, c, 0:T], func=AF.Silu, scale=Wd["ln_g"][:, c:c + 1], bias=Wd["ln_b"][:, c:c + 1]),
             reads=[S.y, Wd["ln_g"], Wd["ln_b"]], writes=[S.yb])
    for nb in range(4):
        slot = ws.block(Wd["w_out"], 0, nb * 512)
        brow = ws.row_bcast(Wd["b_out"], nb * 512)
        pb = C.bank[3 + nb % 2]
        for c in range(NCH):
            k.op("pe", lambda e, c=c, pb=pb, slot=slot: e.matmul(pb[0:T, :], lhsT=S.yb[:, c, 0:T], rhs=slot[:, c, :], start=(c == 0), stop=(c == NCH - 1)), reads=[S.yb, slot], writes=[pb])
        k.op("dve", lambda e, nb=nb, pb=pb, brow=brow: e.tensor_tensor(out=S.out_tm[0:T, nb * 512:(nb + 1) * 512], in0=pb[0:T, :], in1=brow[0:T, :], op=ALU.add), reads=[pb, brow], writes=[S.out_tm])
    gated_residual_add(k, C, xT, S.out_tm, gate, T)


def conv_scratch(k):
    S = NS()
    S.hT32 = k.sb_once("p_hT32", [128, NCH, 128], F32)
    S.hTb = k.sb_once("p_hTb", [128, NCH, 128], BF16)
    S.out_tm = k.sb_once("p_acc", [128, D], F32)
    S.xp = k.sb("c_xp", [128, NCH, CW - 1 + 128], F32)
    S.hist_tmp = k.sb("c_hist", [128, NCH, CW - 1], F32)
    S.y = k.sb("c_y", [128, NCH, 128], F32)
    S.yb = k.sb("c_yb", [128, NCH, 128], BF16)
    S.y2b = k.sb("c_y2b", [128, NCH, 128], BF16)
    S.sg = k.sb("c_sg", [128, 128], F32)
    S.mv = k.sb("c_mv", [128, 384], F32)
    S.tm30 = k.sb("c_tm30", [32, D], F32)
    return S


NH, NKV, HD = 32, 4, 64
BIGD = 1e32


def attn_consts(k):
    A = NS()
    di = k.sb("a_di", [128, 256], I32)
    A.Dmid = k.sb("a_Dmid", [128, 256], F32)
    A.Dfirst = k.sb("a_Dfirst", [128, 256], F32)
    A.Dsamp = k.sb("a_Dsamp", [128, 256], F32)
    k.op("pool", lambda e: e.iota(di[:], pattern=[[-1, 256]], base=128, channel_multiplier=1), writes=[di])
    k.op("dve", lambda e: e.tensor_copy(out=A.Dsamp[:], in_=di[:]), reads=[di], writes=[A.Dsamp])
    k.op("dve", lambda e: e.tensor_scalar(out=A.Dmid[:], in0=A.Dsamp[:], scalar1=-1.0, scalar2=None, op0=ALU.mult), reads=[A.Dsamp], writes=[A.Dmid])
    k.op("dve", lambda e: e.tensor_max(out=A.Dsamp[:], in0=A.Dsamp[:], in1=A.Dmid[:]), reads=[A.Dsamp, A.Dmid], writes=[A.Dsamp])
    for Dm in (A.Dmid, A.Dfirst):
        k.op("dve", lambda e, Dm=Dm: e.tensor_copy(out=Dm[:], in_=A.Dsamp[:]), reads=[A.Dsamp], writes=[Dm])
        k.op("dve", lambda e, Dm=Dm: e.memset(Dm[0:64, 192:256], BIGD), writes=[Dm])
        k.op("dve", lambda e, Dm=Dm: e.memset(Dm[64:128, 0:64], BIGD), writes=[Dm])
    k.op("dve", lambda e: e.memset(A.Dfirst[:, 0:128], BIGD), writes=[A.Dfirst])
    return A


def attn_load_prev(k, C, S, ck_dram, cv_dram):
    k.dma("sp", lambda e: e.dma_start(out=S.kv_tm[:, 0:256], in_=ck_dram), S.kv_tm, writes=[S.kv_tm])
    k.dma("sp", lambda e: e.dma_start(out=S.kv_tm[:, 256:512], in_=cv_dram), S.kv_tm, writes=[S.kv_tm])
    pb = C.bank[1]
    for g in range(NKV):
        k.op("pe", lambda e, g=g: e.transpose(pb[0:HD, g * 128:(g + 1) * 128], S.kv_tm[:, g * HD:(g + 1) * HD], C.ident[:]), reads=[S.kv_tm, C.ident], writes=[pb])
    k.op("act", lambda e: e.copy(out=S.kTprev[:, :, :], in_=pb[0:HD, :].rearrange("p (a b) -> p a b", a=NKV)), reads=[pb], writes=[S.kTprev])
    k.op("act", lambda e: e.copy(out=S.vprev[:, :], in_=S.kv_tm[:, 256:512]), reads=[S.kv_tm], writes=[S.vprev])


def attn(k, C, ws, xT, gm, sh, gate, Wd, S, Dm, T=128, kwin_out=None, vwin_out=None, win_rows=None, carry=True):
    slopes = [2.0 ** (-8.0 * (h + 1) / NH) for h in range(NH)]
    rms_modulate(k, C, xT, gm, sh, S.hT32, S.hTb, T)
    for nb in range(4):
        slot = ws.block(Wd["w_qkv"], 0, nb * 512)
        for half in range(2):
            pb = C.bank[1 + half]
            for j in range(4):
                hl = half * 4 + j
                for c in range(NCH):
                    k.op("pe", lambda e, hl=hl, j=j, c=c, pb=pb, slot=slot: e.matmul(pb[0:HD, j * 128:j * 128 + T], lhsT=slot[:, c, hl * HD:(hl + 1) * HD], rhs=S.hTb[:, c, 0:T],
                                                                                start=(c == 0), stop=(c == NCH - 1)), reads=[slot, S.hTb], writes=[pb])
            for j in range(4):
                h = nb * 8 + half * 4 + j
                k.op("dve", lambda e, h=h, j=j, pb=pb: e.tensor_scalar(out=S.qT[:, h, 0:T], in0=pb[0:HD, j * 128:j * 128 + T], scalar1=Wd["bq"][:, h:h + 1], scalar2=HD ** -0.5,
                                                                       op0=ALU.add, op1=ALU.mult), reads=[pb, Wd["bq"]], writes=[S.qT])
    slot = ws.block(Wd["w_qkv"], 0, 2048)
    brow = ws.row_bcast(Wd["b_qkv"], 2048)
    pb = C.bank[3]
    for c in range(NCH):
        k.op("pe", lambda e, c=c, pb=pb, slot=slot: e.matmul(pb[0:T, :], lhsT=S.hTb[:, c, 0:T], rhs=slot[:, c, :], start=(c == 0), stop=(c == NCH - 1)), reads=[S.hTb, slot], writes=[pb])
    k.op("dve", lambda e, pb=pb, brow=brow: e.tensor_tensor(out=S.kv_tm[0:T, :], in0=pb[0:T, :], in1=brow[0:T, :], op=ALU.add), reads=[pb, brow], writes=[S.kv_tm])
    if kwin_out is not None:
        r0, n = win_rows
        k.dma("sp", lambda e: e.dma_start(out=kwin_out[r0:r0 + n, :], in_=S.kv_tm[T - n:T, 0:256]), S.kv_tm, reads=[S.kv_tm])
        k.dma("sp", lambda e: e.dma_start(out=vwin_out[r0:r0 + n, :], in_=S.kv_tm[T - n:T, 256:512]), S.kv_tm, reads=[S.kv_tm])
    k.op("act", lambda e: e.copy(out=S.vcur[0:T, :], in_=S.kv_tm[0:T, 256:512]), reads=[S.kv_tm], writes=[S.vcur])
    pb = C.bank[4]
    for g in range(NKV):
        k.op("pe", lambda e, g=g, pb=pb: e.transpose(pb[0:HD, g * 128:g * 128 + T], S.kv_tm[0:T, g * HD:(g + 1) * HD], C.ident[0:T, 0:T]), reads=[S.kv_tm, C.ident], writes=[pb])
    k.op("act", lambda e, pb=pb: e.copy(out=S.kTcur[:, :, 0:T], in_=pb[0:HD, :].rearrange("p (a b) -> p a b", a=NKV)[:, :, 0:T]), reads=[pb], writes=[S.kTcur])
    NKEY = 128 + T
    for h in range(NH):
        g = h // (NH // NKV)
        ps_, pt_, po_ = C.bank[5 + h % 2], C.bank[1 + h % 2], C.bank[7]
        k.op("pe", lambda e, h=h, g=g, ps_=ps_: e.matmul(ps_[0:T, 0:128], lhsT=S.qT[:, h, 0:T], rhs=S.kTprev[:, g, :], start=True, stop=True), reads=[S.qT, S.kTprev], writes=[ps_])
        k.op("pe", lambda e, h=h, g=g, ps_=ps_: e.matmul(ps_[0:T, 128:NKEY], lhsT=S.qT[:, h, 0:T], rhs=S.kTcur[:, g, 0:T], start=True, stop=True), reads=[S.qT, S.kTcur], writes=[ps_])
        k.op("dve", lambda e, h=h, ps_=ps_: e.scalar_tensor_tensor(out=S.sc[0:T, 0:NKEY], in0=Dm[0:T, 0:NKEY], scalar=-slopes[h], in1=ps_[0:T, 0:NKEY], op0=ALU.mult, op1=ALU.add),
             reads=[Dm, ps_], writes=[S.sc])
        k.op("dve", lambda e: e.tensor_reduce(out=S.m[0:T, 0:1], in_=S.sc[0:T, 0:NKEY], axis=AX.X, op=ALU.max), reads=[S.sc], writes=[S.m])
        k.op("dve", lambda e, h=h: e.tensor_tensor(out=S.m[0:T, 0:1], in0=S.m[0:T, 0:1], in1=Wd["sink"][0:T, h:h + 1], op=ALU.max), reads=[S.m, Wd["sink"]], writes=[S.m])
        k.op("dve", lambda e, h=h: e.tensor_tensor(out=S.m[0:T, 2:3], in0=Wd["sink"][0:T, h:h + 1], in1=S.m[0:T, 0:1], op=ALU.subtract), reads=[S.m, Wd["sink"]], writes=[S.m])
        k.op("dve", lambda e: e.tensor_scalar(out=S.m[0:T, 1:2], in0=S.m[0:T, 0:1], scalar1=-1.0, scalar2=None, op0=ALU.mult), reads=[S.m], writes=[S.m])
        k.op("act", lambda e: e.activation(out=S.p[0:T, 0:NKEY], in_=S.sc[0:T, 0:NKEY], func=AF.Exp, bias=S.m[0:T, 1:2], accum_out=S.m[0:T, 3:4]), reads=[S.sc, S.m], writes=[S.p, S.m])
        k.op("act", lambda e: e.activation(out=S.m[0:T, 2:3], in_=S.m[0:T, 2:3], func=AF.Exp), reads=[S.m], writes=[S.m])
        k.op("dve", lambda e: e.tensor_tensor(out=S.m[0:T, 3:4], in0=S.m[0:T, 3:4], in1=S.m[0:T, 2:3], op=ALU.add), reads=[S.m], writes=[S.m])
        k.op("dve", lambda e: e.reciprocal(out=S.m[0:T, 3:4], in_=S.m[0:T, 3:4]), reads=[S.m], writes=[S.m])
        k.op("pe", lambda e, pt_=pt_: e.transpose(pt_[:, 0:T], S.p[0:T, 0:128], C.ident[0:T, 0:T]), reads=[S.p, C.ident], writes=[pt_])
        k.op("pe", lambda e, pt_=pt_: e.transpose(pt_[0:T, 128:128 + T], S.p[0:T, 128:NKEY], C.ident[0:T, 0:T]), reads=[S.p, C.ident], writes=[pt_])
        k.op("act", lambda e, pt_=pt_: e.copy(out=S.pT[:, 0, 0:T], in_=pt_[:, 0:T]), reads=[pt_], writes=[S.pT])
        k.op("act", lambda e, pt_=pt_: e.copy(out=S.pT[0:T, 1, 0:T], in_=pt_[0:T, 128:128 + T]), reads=[pt_], writes=[S.pT])
        oc = (h % 8) * HD
        k.op("pe", lambda e, g=g, oc=oc, po_=po_: e.matmul(po_[0:T, oc:oc + HD], lhsT=S.pT[:, 0, 0:T], rhs=S.vprev[:, g * HD:(g + 1) * HD], start=True, stop=False), reads=[S.pT, S.vprev], writes=[po_])
        k.op("pe", lambda e, g=g, oc=oc, po_=po_: e.matmul(po_[0:T, oc:oc + HD], lhsT=S.pT[0:T, 1, 0:T], rhs=S.vcur[0:T, g * HD:(g + 1) * HD], start=False, stop=True), reads=[S.pT, S.vcur], writes=[po_])
        k.op("dve", lambda e, h=h, oc=oc, po_=po_: e.tensor_scalar(out=S.o_tm[0:T, h * HD:(h + 1) * HD], in0=po_[0:T, oc:oc + HD], scalar1=S.m[0:T, 3:4], scalar2=None, op0=ALU.mult),
             reads=[po_, S.m], writes=[S.o_tm])
    if carry:
        k.op("act", lambda e: e.copy(out=S.kTprev[:, :, :], in_=S.kTcur[:, :, :]), reads=[S.kTcur], writes=[S.kTprev])
        k.op("act", lambda e: e.copy(out=S.vprev[:, :], in_=S.vcur[:, :]), reads=[S.vcur], writes=[S.vprev])
    for b4 in range(4):
        pb = C.bank[1 + b4 % 2]
        for j in range(4):
            c = b4 * 4 + j
            k.op("pe", lambda e, c=c, j=j, pb=pb: e.transpose(pb[:, j * 128:j * 128 + T], S.o_tm[0:T, c * 128:(c + 1) * 128], C.ident[0:T, 0:T]), reads=[S.o_tm, C.ident], writes=[pb])
        k.op("act", lambda e, b4=b4, pb=pb: e.copy(out=S.oT[:, b4 * 4:(b4 + 1) * 4, 0:T], in_=pb[:, :].rearrange("p (a b) -> p a b", a=4)[:, :, 0:T]), reads=[pb], writes=[S.oT])
    for nb in range(4):
        slot = ws.block(Wd["w_o"], 0, nb * 512)
        pb = C.bank[3 + nb % 2]
        for c in range(NCH):
            k.op("pe", lambda e, c=c, pb=pb, slot=slot: e.matmul(pb[0:T, :], lhsT=S.oT[:, c, 0:T], rhs=slot[:, c, :], start=(c == 0), stop=(c == NCH - 1)), reads=[S.oT, slot], writes=[pb])
        k.op("act", lambda e, nb=nb, pb=pb: e.copy(out=S.out_tm[0:T, nb * 512:(nb + 1) * 512], in_=pb[0:T, :]), reads=[pb], writes=[S.out_tm])
    gated_residual_add(k, C, xT, S.out_tm, gate, T)


def attn_scratch(k):
    S = NS()
    S.hT32 = k.sb_once("p_hT32", [128, NCH, 128], F32)
    S.hTb = k.sb_once("p_hTb", [128, NCH, 128], BF16)
    S.out_tm = k.sb_once("p_acc", [128, D], F32)
    S.qT = k.sb("t_qT", [HD, NH, 128], BF16)
    S.kv_tm = k.sb("t_kv", [128, 512], F32)
    S.kTcur = k.sb("t_kTc", [HD, NKV, 128], BF16)
    S.kTprev = k.sb("t_kTp", [HD, NKV, 128], BF16)
    S.vcur = k.sb("t_vc", [128, 256], BF16)
    S.vprev = k.sb("t_vp", [128, 256], BF16)
    S.sc = k.sb("t_sc", [128, 256], F32)
    S.p = k.sb("t_p", [128, 256], F32)
    S.pT = k.sb("t_pT", [128, 2, 128], BF16)
    S.m = k.sb("t_m", [128, 4], F32)
    S.o_tm = k.sb("t_o", [128, D], F32)
    S.oT = k.sb("t_oT", [128, NCH, 128], BF16)
    return S


def ada_layer(k, C, ws, scT, adaw, adab_fm, modv, l, ng):
    for nb in range(24):
        slot = ws.block(adaw, 0, nb * 512)
        pb = C.bank[3 + nb % 2]
        for j in range(4):
            for c in range(NCH):
                k.op("pe", lambda e, j=j, c=c, pb=pb, slot=slot: e.matmul(pb[:, j * 8:j * 8 + ng], lhsT=slot[:, c, j * 128:(j + 1) * 128], rhs=scT[:, c, 0:ng],
                                                                        start=(c == 0), stop=(c == NCH - 1)), reads=[slot, scT], writes=[pb])
        for j in range(4):
            ch = nb * 4 + j
            k.op("dve", lambda e, j=j, ch=ch, pb=pb: e.tensor_scalar(out=modv[:, l, 0:ng, ch], in0=pb[:, j * 8:j * 8 + ng], scalar1=adab_fm[:, ch:ch + 1], scalar2=None, op0=ALU.add),
                 reads=[pb, adab_fm], writes=[modv])


def final_norm_out(k, C, xT, gfin, zero16, S, out_rows, T=128):
    rms_modulate(k, C, xT, gfin, zero16, S.hT32, None, T)
    to_token_major(k, C, S.hT32, S.out_tm, T)
    k.dma("sp", lambda e: e.dma_start(out=out_rows, in_=S.out_tm[0:T, :]), S.out_tm, reads=[S.out_tm])


def peer_q_streamed(k, C, ws, wq_dram, S, T=128):
    for nb in range(4):
        slot = ws.block(wq_dram, 0, nb * 512)
        for j in range(4):
            hc = nb * 4 + j
            pb = C.bank[3 + (hc % 2)]
            for c in range(NCH):
                k.op("pe", lambda e, j=j, c=c, pb=pb, slot=slot: e.matmul(pb[:, 0:T], lhsT=slot[:, c, j * 128:(j + 1) * 128], rhs=S.hTb[:, c, 0:T],
                                                                        start=(c == 0), stop=(c == NCH - 1)), reads=[slot, S.hTb], writes=[pb])
            k.op("act", lambda e, hc=hc, pb=pb: e.copy(out=S.qT[:, hc, 0:T], in_=pb[:, 0:T]), reads=[pb], writes=[S.qT])


NCORES = 8
NTILE = 16
NHALO = 2
DEPTH = 4
KIND = [0, 1, 2, 0]


def _view(k, arena, p0, p1, a, b, name, shape3=None):
    ap = arena.ap[p0:p1, a:b]
    if shape3 is not None:
        ap = ap.rearrange("p (a b) -> p a b", a=shape3[0])
    return k.view(ap, name)


def build_nc(ntile=NTILE, nsamp=1, halo=True):
    nc = bass.Bass("TRN2", target_bir_lowering=False)

    def din(n, s, dt=F32):
        return nc.dram_tensor(n, list(s), dt, kind="ExternalInput").ap()

    def dout(n, s, dt=F32):
        return nc.dram_tensor(n, list(s), dt, kind="ExternalOutput").ap()

    NG = 1 + nsamp
    nh = NHALO if halo else 0
    xp_d = din("xp_fm", [nh + ntile, 128, 16, 128]); flag_d = din("flag", [128, 1]); xs_d = din("xs_fm", [nsamp, 128, 16, 128]); cT_d = din("cT", [128, 16, NG])
    ck_d = din("cache_k", [nsamp, 128, 256]); cv_d = din("cache_v", [nsamp, 128, 256]); st_d = din("state_conv", [nsamp, 30, 2048])
    gmix_d = din("g_mix_fm", [128, DEPTH, 16]); gch_d = din("g_ch_fm", [128, DEPTH, 16]); gfin_d = din("g_fin_fm", [128, 16])
    adaw_d = [din(f"ada_w{l}", [2048, 12288]) for l in range(DEPTH)]; adab_d = din("ada_b_fm", [128, DEPTH, 96])
    sgu_win_d = [din(f"sgu_w_in{i}", [2048, 12288]) for i in range(2)]; sgu_wout_d = [din(f"sgu_w_out{i}", [6144, 2048]) for i in range(2)]
    sgu_bin_d = din("sgu_b_in", [2, 12288]); sgu_lng_d = din("sgu_ln_g", [2, 6144]); sgu_lnb_d = din("sgu_ln_b", [2, 6144])
    wsT_d = din("sgu_wsT", [128, 2, 8, 128]); bs_d = din("sgu_bs_tm", [128, 2, 8])
    cwin_d = din("conv_w_in", [2048, 4096]); cwout_d = din("conv_w_out", [2048, 2048]); cbout_d = din("conv_b_out", [2048])
    cbin_d = din("conv_b_in_fm", [128, 32]); cdw_d = din("conv_dwT", [128, 16, 31]); cdwb_d = din("conv_dw_b_fm", [128, 16])
    clng_d = din("conv_ln_g_fm", [128, 16]); clnb_d = din("conv_ln_b_fm", [128, 16])
    wqkv_d = din("attn_w_qkv", [2048, 2560]); bqkv_d = din("attn_b_qkv", [2560]); bq_d = din("attn_bq_fm", [64, 32]); sink_d = din("attn_sinks", [32]); wo_d = din("attn_w_o", [2048, 2048])
    pwq_d = [din(f"peer_w_q{l}", [2048, 2048]) for l in range(DEPTH)]; skT_d = din("peer_skT", [128, DEPTH, 16, 128])
    pu_d = [din(f"peer_u{l}", [16384, 2048]) for l in range(DEPTH)]; pv_d = [din(f"peer_v{l}", [16384, 2048]) for l in range(DEPTH)]
    y_p = dout("y_p", [ntile * 128, 2048]); y_s = dout("y_s", [nsamp, 16, 2048]); cs_p = dout("cs_p", [30, 2048]); kw_p = dout("kw_p", [128, 256]); vw_p = dout("vw_p", [128, 256])
    cs_s = dout("cs_s", [nsamp, 30, 2048]); kw_s = dout("kw_s", [nsamp, 128, 256]); vw_s = dout("vw_s", [nsamp, 128, 256]); sv_s = dout("sv_s", [2, nsamp, 16, 6144])

    with ExitStack() as st:
        k = K(nc, st)
        C = Common(k)
        A = attn_consts(k)
        def load(name, shape, src, dt=F32, q="sp"):
            b = k.sb(name, shape, dt)
            k.dma(q, lambda e, b=b, src=src: e.dma_start(out=b[:], in_=src), b, writes=[b])
            return b
        cT = load("cT_sb", [128, 16, NG], cT_d)
        gmix = load("gmix_sb", [128, DEPTH, 16], gmix_d); gch = load("gch_sb", [128, DEPTH, 16], gch_d); gfin = load("gfin_sb", [128, 16], gfin_d)
        adab = load("adab_sb", [128, DEPTH, 96], adab_d)
        wsT = load("wsT_sb", [128, 2, 8, 128], wsT_d, BF16, q="pool")
        bs = load("bs_sb", [128, 2, 8], bs_d)
        cbin = load("cbin_sb", [128, 32], cbin_d); cdw = load("cdw_sb", [128, 16, 31], cdw_d); cdwb = load("cdwb_sb", [128, 16], cdwb_d)
        clng = load("clng_sb", [128, 16], clng_d); clnb = load("clnb_sb", [128, 16], clnb_d)
        bq = load("bq_sb", [64, 32], bq_d)
        sink = k.sb("sink_sb", [128, 32], F32)
        k.dma("sp", lambda e: e.dma_start(out=sink[:], in_=sink_d.unsqueeze(0).partition_broadcast(128)), sink, writes=[sink])
        zero16 = k.sb("zero16", [128, 16], F32)
        k.op("dve", lambda e: e.memset(zero16[:], 0.0), writes=[zero16])
        flag = load("flag_sb", [128, 1], flag_d)
        nf = k.sb("nf_sb", [128, 1], F32)
        k.op("dve", lambda e: e.tensor_scalar(out=nf[:], in0=flag[:], scalar1=-BIGD, scalar2=BIGD, op0=ALU.mult, op1=ALU.add), reads=[flag], writes=[nf])
        Dsel = k.sb("a_Dsel", [128, 256], F32)
        k.op("dve", lambda e: e.tensor_scalar(out=Dsel[:, 0:128], in0=A.Dmid[:, 0:128], scalar1=nf[:, 0:1], scalar2=None, op0=ALU.add), reads=[A.Dmid, nf], writes=[Dsel])
        k.op("dve", lambda e: e.tensor_copy(out=Dsel[:, 128:256], in_=A.Dmid[:, 128:256]), reads=[A.Dmid], writes=[Dsel])
        k.op("dve", lambda e: e.memset(wsT[64:128, :, :, 0:64], 0.0), writes=[wsT])
        xT = k.sb("xT", [128, 16, 128], F32)
        hT32 = k.sb_once("p_hT32", [128, NCH, 128], F32); hTb = k.sb_once("p_hTb", [128, NCH, 128], BF16); acc = k.sb_once("p_acc", [128, D], F32)
        NF, NB = 10240, 18432
        AFa = k.sb("arena_f32", [128, NF], F32); ABa = k.sb("arena_bf16", [128, NB], BF16)
        skslot = k.sb("skslot", [128, 16, 128], BF16)
        SG = NS(); SG.hT32, SG.hTb, SG.out_tm = hT32, hTb, acc
        SG.v = _view(k, AFa, 0, 128, 0, 6144, "g_v"); SG.prod = SG.v
        SG.u = _view(k, ABa, 0, 128, 0, 6144, "g_u"); SG.vb = _view(k, ABa, 0, 128, 6144, 12288, "g_vb")
        SG.prodT = _view(k, ABa, 0, 128, 12288, 18432, "g_prodT", (48, 128)); SG.st = k.sb("g_st", [128, 4], F32)
        SG.tmp = _view(k, AFa, 0, 128, 6144, 6656, "g_tmp")
        CV = NS(); CV.hT32, CV.hTb, CV.out_tm = hT32, hTb, acc
        CV.xp = k.sb("c_xp", [128, NCH, CW - 1 + 128], F32); CV.hist_tmp = k.sb("c_hist", [128, NCH, CW - 1], F32)
        CV.y = _view(k, AFa, 0, 128, 0, 2048, "c_y", (16, 128)); CV.tm30 = _view(k, AFa, 0, 32, 2048, 4096, "c_tm30")
        CV.sg = _view(k, AFa, 0, 128, 4096, 4224, "c_sg"); CV.mv = _view(k, AFa, 0, 128, 4224, 4608, "c_mv")
        CV.yb = _view(k, ABa, 0, 128, 0, 2048, "c_yb", (16, 128)); CV.y2b = _view(k, ABa, 0, 128, 2048, 4096, "c_y2b", (16, 128))
        AT = NS(); AT.hT32, AT.hTb, AT.out_tm = hT32, hTb, acc
        AT.kTprev = k.sb("t_kTp", [HD, NKV, 128], BF16); AT.vprev = k.sb("t_vp", [128, 256], BF16); AT.m = k.sb("t_m", [128, 4], F32)
        AT.o_tm = _view(k, AFa, 0, 128, 0, 2048, "t_o"); AT.kv_tm = _view(k, AFa, 0, 128, 2048, 2560, "t_kv")
        AT.sc = _view(k, AFa, 0, 128, 2560, 2816, "t_sc"); AT.p = _view(k, AFa, 0, 128, 2816, 3072, "t_p")
        AT.qT = _view(k, ABa, 0, HD, 0, 4096, "t_qT", (NH, 128)); AT.kTcur = _view(k, ABa, 0, HD, 4096, 4608, "t_kTc", (NKV, 128))
        AT.vcur = _view(k, ABa, 0, 128, 4608, 4864, "t_vc"); AT.pT = _view(k, ABa, 0, 128, 4864, 5120, "t_pT", (2, 128))
        AT.oT = _view(k, ABa, 0, 128, 5120, 7168, "t_oT", (16, 128))
        PE = NS(); PE.hT32, PE.hTb, PE.acc = hT32, hTb, acc
        PE.h_tm = _view(k, AFa, 0, 128, 0, 2048, "p_h_tm")
        PE.s = _view(k, AFa, 0, 128, 2048, 4096, "p_s", (NHC, PK)); PE.s2 = _view(k, AFa, 0, 128, 4096, 6144, "p_s2", (NHC, PK))
        PE.cand = k.view(AFa.ap[:, 6144:8192].rearrange("p (h a b) -> p h a b", h=PH, a=16), "p_cand")
        PE.cand2 = k.view(AFa.ap[:, 8192:10240].rearrange("p (h a b) -> p h a b", h=PH, a=16), "p_cand2")
        PE.gbuf = [_view(k, ABa, 0, 128, 2048 * (i + 1), 2048 * (i + 2), f"p_gb{i}") for i in range(6)]
        PE.junk = _view(k, AFa, 0, 128, 8192, 10240, "p_junk")
        PE.qT = _view(k, ABa, 0, 128, 0, 2048, "p_qT", (NHC, 128))
        for n_, shp, dt_ in (("sv", [128, NHC, 16], F32), ("si", [128, NHC, 16], U32), ("sif", [128, NHC, 16], F32), ("best", [128, PH, 16], F32), ("bi", [128, PH, 16], U32),
                             ("b1", [128, PH, 16], U32), ("b2", [128, PH, 16], U32), ("b1f", [128, PH, 16], F32), ("b2f", [128, PH, 16], F32), ("oh", [128, 16, 16], F32),
                             ("I1", [128, PH, 16], F32), ("I2", [128, PH, 16], F32), ("ef", [128, PH, 16], F32), ("ei", [128, PH, 16], I32), ("g", [128, PH, 16], F32),
                             ("gs", [128, PH], F32), ("a", [128, PH, 16], F32), ("w", [128, PH, 16], F32)):
            setattr(PE, n_, k.sb("p_" + n_, shp, dt_))
        tb_u = [nc.dram_tensor(f"ub{l}", [16384, 2048], BF16).ap() for l in range(DEPTH)]
        tb_v = [nc.dram_tensor(f"vb{l}", [16384, 2048], BF16).ap() for l in range(DEPTH)]
        cbufs = [k.view(ABa.ap[:, q * 4608:q * 4608 + 4096].rearrange("p (a b) -> p a b", a=2), f"cvt{q}") for q in range(4)]
        ci = 0
        for (src_l, dst_l) in ((pu_d, tb_u), (pv_d, tb_v)):
            for l in range(DEPTH):
                sv_ = src_l[l].rearrange("(p r) d -> p r d", p=128)
                dv_ = dst_l[l].rearrange("(p r) d -> p r d", p=128)
                dbuf = k.view(dst_l[l], f"tb{ci}")
                for r0 in range(0, 128, 2):
                    cb = cbufs[ci % 4]
                    ci += 1
                    k.dma("pool", lambda e, cb=cb, sv_=sv_, r0=r0: e.dma_start(out=cb[:, :, :], in_=sv_[:, r0:r0 + 2, :]), cb, writes=[cb])
                    k.dma("sp", lambda e, cb=cb, dv_=dv_, r0=r0: e.dma_start(out=dv_[:, r0:r0 + 2, :], in_=cb[:, :, :]), cb, reads=[cb], writes=[dbuf])
        k.barrier()
        scT = k.sb("scT", [128, 16, NG], BF16)
        modv = k.sb("modv", [128, DEPTH, NG, 96], F32)
        der = k.sb("der", [128, DEPTH, NG, 2, 16], F32)
        k.sb_once("rm_sq", [128, NCH, 128], BF16); k.sb_once("rm_rstd", [128, 128], F32)
        left = nc.sbuf_bytes_remaining - 4 * 2048 - 1024
        nsl = max(1, min(3, left // 16384))
        print("[kernel] sbuf left before weight slots:", nc.sbuf_bytes_remaining, "-> slots:", nsl, flush=True)
        assert left >= 16384, "no room for a weight slot"
        ws = WS(k, nslots=nsl, q="pool")
        k.op("act", lambda e: e.activation(out=scT[:], in_=cT[:], func=AF.Silu), reads=[cT], writes=[scT])
        for l in range(DEPTH):
            adab_l = k.view(adab.ap[:, l, :], f"adab{l}")
            k.barrier()
            ada_layer(k, C, ws, scT, adaw_d[l], adab_l, modv, l, NG)
        for l in range(DEPTH):
            for g in range(NG):
                for (j, off, gn) in ((0, 16, gmix), (1, 64, gch)):
                    k.op("dve", lambda e, l=l, g=g, j=j, off=off: e.tensor_scalar(out=der[:, l, g, j, :], in0=modv[:, l, g, off:off + 16], scalar1=1.0, scalar2=None, op0=ALU.add),
                         reads=[modv], writes=[der])
                    k.op("dve", lambda e, l=l, g=g, j=j, gn=gn: e.tensor_tensor(out=der[:, l, g, j, :], in0=der[:, l, g, j, :], in1=gn[:, l, :], op=ALU.mult), reads=[der, gn], writes=[der])
        k.barrier()

        def vec(ap, name):
            return k.view(ap, name)

        def mods(l, g):
            return dict(gm1=vec(der.ap[:, l, g, 0, :], "gm1"), sh1=vec(modv.ap[:, l, g, 0:16], "sh1"), g1=vec(modv.ap[:, l, g, 32:48], "g1"),
                        gm2=vec(der.ap[:, l, g, 1, :], "gm2"), sh2=vec(modv.ap[:, l, g, 48:64], "sh2"), g2=vec(modv.ap[:, l, g, 80:96], "g2"))
        MODS = [[mods(l, g) for g in range(NG)] for l in range(DEPTH)]
        SGW = [dict(w_in=sgu_win_d[i], b_in=sgu_bin_d[i], ln_g=sgu_lng_d[i], ln_b=sgu_lnb_d[i], w_out=sgu_wout_d[i],
                    wsT=vec(wsT.ap[:, i], f"wsT{i}"), bs=vec(bs.ap[:, i], f"bs{i}")) for i in range(2)]
        CVW = dict(w_in=cwin_d, w_out=cwout_d, b_out=cbout_d, b_in=cbin, dw=cdw, dw_b=cdwb, ln_g=clng, ln_b=clnb)
        ATW = dict(w_qkv=wqkv_d, b_qkv=bqkv_d, w_o=wo_d, bq=bq, sink=sink)

        def run_tile(grp, T, x_src, first, last, si, mode="full", attn_first=None, Dm_first=None):
            k.dma("sp", lambda e: e.dma_start(out=xT[:], in_=x_src), xT, writes=[xT])
            isgu = 0
            afirst = first if attn_first is None else attn_first
            for l in range(DEPTH):
                M = MODS[l][grp]
                k.barrier()
                if KIND[l] == 0:
                    vout = sv_s[isgu, si] if si is not None else None
                    sgu(k, C, ws, xT, M["gm1"], M["sh1"], M["g1"], SGW[isgu], SG, T=T, v_out=vout)
                    isgu += 1
                elif KIND[l] == 1:
                    if si is not None:
                        conv_load_hist_state(k, C, CV, st_d[si])
                    elif first:
                        conv_load_hist_zero(k, CV)
                    so = cs_s[si] if si is not None else (cs_p if last else None)
                    conv(k, C, ws, xT, M["gm1"], M["sh1"], M["g1"], CVW, CV, T=T, state_out=so, carry=(si is None))
                    if mode == "h0":
                        k.barrier()
                        return
                else:
                    if si is not None:
                        attn_load_prev(k, C, AT, ck_d[si], cv_d[si])
                        cpy = k.view(kw_s[si], f"kws_cpy{si}")
                        k.dma("sp", lambda e, si=si: e.dma_start(out=kw_s[si, 0:112, :], in_=ck_d[si, 16:128, :]), cpy, writes=[cpy])
                        k.dma("sp", lambda e, si=si: e.dma_start(out=vw_s[si, 0:112, :], in_=cv_d[si, 16:128, :]), cpy, writes=[cpy])
                        attn(k, C, ws, xT, M["gm1"], M["sh1"], M["g1"], ATW, AT, A.Dsamp, T=T, kwin_out=kw_s[si], vwin_out=vw_s[si], win_rows=(112, 16), carry=False)
                    else:
                        if afirst:
                            k.op("dve", lambda e: e.memset(AT.kTprev[:, :, :], 0.0), writes=[AT.kTprev])
                            k.op("dve", lambda e: e.memset(AT.vprev[:, :], 0.0), writes=[AT.vprev])
                        Dm = A.Dfirst if afirst else (Dm_first if Dm_first is not None else A.Dmid)
                        attn(k, C, ws, xT, M["gm1"], M["sh1"], M["g1"], ATW, AT, Dm, T=T,
                             kwin_out=kw_p if last else None, vwin_out=vw_p if last else None, win_rows=(0, 128) if last else None)
                        if mode == "h1":
                            k.barrier()
                            return
                k.barrier()
                k.dma("pool", lambda e, l=l: e.dma_start(out=skslot[:], in_=skT_d[:, l]), skslot, writes=[skslot])
                peer(k, C, xT, M["gm2"], M["sh2"], M["g2"], PeerW(k, pwq_d[l], skslot, tb_u[l], tb_v[l]), PE, T=128, ws=ws, mid_barrier=True)
            k.barrier()
            return

        FN = NS(); FN.hT32, FN.out_tm = hT32, acc
        if halo:
            run_tile(0, 128, xp_d[0], True, False, None, mode="h0")
            run_tile(0, 128, xp_d[1], False, False, None, mode="h1", attn_first=True)
            k.op("dve", lambda e: e.tensor_scalar(out=CV.xp[:, :, 0:CW - 1], in0=CV.xp[:, :, 0:CW - 1], scalar1=flag[:, 0:1], scalar2=None, op0=ALU.mult),
                 reads=[CV.xp, flag], writes=[CV.xp])
        for t in range(ntile):
            if halo:
                run_tile(0, 128, xp_d[nh + t], False, t == ntile - 1, None, attn_first=False, Dm_first=(Dsel if t == 0 else None))
            else:
                run_tile(0, 128, xp_d[t], t == 0, t == ntile - 1, None)
            final_norm_out(k, C, xT, gfin, zero16, FN, y_p[t * 128:(t + 1) * 128, :], T=128)
        for si in range(nsamp):
            run_tile(1 + si, 16, xs_d[si], False, False, si)
            final_norm_out(k, C, xT, gfin, zero16, FN, y_s[si], T=16)
        k.finish()
        print("[kernel] instructions:", k.n, "sbuf bytes left:", nc.sbuf_bytes_remaining, "weight slots:", nsl, flush=True)
    return nc


def _fm(v):
    v = np.asarray(v)
    lead = v.shape[:-1]
    return np.ascontiguousarray(np.moveaxis(v.reshape(*lead, -1, 128), -1, 0))


def make_in_maps(I, ncores, ntile, nsamp, halo=True):
    f32 = lambda a: np.ascontiguousarray(np.asarray(a), dtype=np.float32)
    shared = {}
    shared["g_mix_fm"] = _fm(I["norm_mix_g"]); shared["g_ch_fm"] = _fm(I["norm_ch_g"]); shared["g_fin_fm"] = _fm(I["norm_final_g"])
    shared["ada_b_fm"] = _fm(I["ada_b"])
    for l in range(DEPTH):
        shared[f"ada_w{l}"] = f32(I["ada_w"][l]); shared[f"peer_w_q{l}"] = f32(I["peer_w_q"][l])
        shared[f"peer_u{l}"] = f32(I["peer_u"][l]); shared[f"peer_v{l}"] = f32(I["peer_v"][l])
    for i in range(2):
        shared[f"sgu_w_in{i}"] = f32(I["sgu_w_in"][i]); shared[f"sgu_w_out{i}"] = f32(I["sgu_w_out"][i])
    shared["sgu_b_in"] = f32(I["sgu_b_in"]); shared["sgu_ln_g"] = f32(I["sgu_ln_g"]); shared["sgu_ln_b"] = f32(I["sgu_ln_b"])
    shared["sgu_wsT"] = np.ascontiguousarray(np.asarray(I["sgu_w_s"]).transpose(3, 0, 1, 2))
    shared["sgu_bs_tm"] = np.ascontiguousarray(np.asarray(I["sgu_b_s"]).transpose(2, 0, 1))
    shared["conv_w_in"] = f32(I["conv_w_in"][0]); shared["conv_w_out"] = f32(I["conv_w_out"][0]); shared["conv_b_out"] = f32(I["conv_b_out"][0])
    shared["conv_b_in_fm"] = _fm(I["conv_b_in"][0]); shared["conv_dw_b_fm"] = _fm(I["conv_dw_b"][0])
    shared["conv_ln_g_fm"] = _fm(I["conv_ln_g"][0]); shared["conv_ln_b_fm"] = _fm(I["conv_ln_b"][0])
    shared["conv_dwT"] = np.ascontiguousarray(np.asarray(I["conv_dw"][0]).T.reshape(16, 128, 31).transpose(1, 0, 2))
    shared["attn_w_qkv"] = f32(I["attn_w_qkv"][0]); shared["attn_b_qkv"] = f32(I["attn_b_qkv"][0]); shared["attn_w_o"] = f32(I["attn_w_o"][0])
    shared["attn_bq_fm"] = np.ascontiguousarray(np.asarray(I["attn_b_qkv"][0][:2048]).reshape(32, 64).T); shared["attn_sinks"] = f32(I["attn_sinks"][0])
    shared["peer_skT"] = np.ascontiguousarray(np.asarray(I["peer_subkeys"]).reshape(DEPTH, 16, 128, 128).transpose(3, 0, 1, 2))
    xp = np.asarray(I["x_prompt"]); xs = np.asarray(I["x_sample"])
    in_maps = []
    for c in range(ncores):
        m = dict(shared)
        if halo:
            b, half = c // 2, c % 2
            rows = np.zeros(((NHALO + ntile) * 128, 2048), np.float32)
            if half:
                rows[:NHALO * 128] = xp[b, ntile * 128 - NHALO * 128:ntile * 128]
            rows[NHALO * 128:] = xp[b, half * ntile * 128:(half + 1) * ntile * 128]
            m["xp_fm"] = np.ascontiguousarray(rows.reshape(NHALO + ntile, 128, 16, 128).transpose(0, 3, 2, 1))
            m["flag"] = np.full((128, 1), float(half), np.float32)
        else:
            m["xp_fm"] = np.ascontiguousarray(xp[c].reshape(ntile, 128, 16, 128).transpose(0, 3, 2, 1))
            m["flag"] = np.ones((128, 1), np.float32)
        xsp = np.zeros((nsamp, 128, 2048), np.float32); xsp[:, :16] = xs[nsamp * c:nsamp * c + nsamp]
        m["xs_fm"] = np.ascontiguousarray(xsp.reshape(nsamp, 128, 16, 128).transpose(0, 3, 2, 1))
        cc = np.stack([np.asarray(I["c_prompt"])[c // 2 if halo else c]] + [np.asarray(I["c_sample"])[nsamp * c + i] for i in range(nsamp)])
        m["cT"] = np.ascontiguousarray(cc.reshape(1 + nsamp, 16, 128).transpose(2, 1, 0))
        m["cache_k"] = f32(np.asarray(I["cache_k_win"])[0, nsamp * c:nsamp * c + nsamp].reshape(nsamp, 128, 256))
        m["cache_v"] = f32(np.asarray(I["cache_v_win"])[0, nsamp * c:nsamp * c + nsamp].reshape(nsamp, 128, 256))
        m["state_conv"] = f32(np.asarray(I["state_conv"])[0, nsamp * c:nsamp * c + nsamp])
        in_maps.append(m)
    return in_maps


def kernel(**I):
    in_maps = make_in_maps(I, NCORES, NTILE, 1, halo=True)
    nc = build_nc(NTILE, 1, True)
    res = run_bass_kernel_spmd(nc, in_maps, core_ids=list(range(NCORES))).results
    R = lambda key: np.stack([np.asarray(r[key]) for r in res])
    odd = lambda key: np.stack([np.asarray(res[c][key]) for c in range(1, NCORES, 2)])
    y_prompt = R("y_p").reshape(4, 4096, 2048)
    y_sample = R("y_s").reshape(8, 16, 2048)
    conv_state_prompt = odd("cs_p").reshape(1, 4, 30, 2048)
    k_win_prompt = odd("kw_p").reshape(1, 4, 128, 4, 64); v_win_prompt = odd("vw_p").reshape(1, 4, 128, 4, 64)
    conv_state_sample = R("cs_s").reshape(1, 8, 30, 2048)
    k_win_sample = R("kw_s").reshape(1, 8, 128, 4, 64); v_win_sample = R("vw_s").reshape(1, 8, 128, 4, 64)
    sgu_v_sample = np.ascontiguousarray(R("sv_s").transpose(1, 0, 2, 3, 4)).reshape(2, 8, 16, 6144)
    return tuple(np.ascontiguousarray(a, dtype=np.float32) for a in (y_prompt, y_sample, conv_state_prompt, k_win_prompt, v_win_prompt,
                                                                     conv_state_sample, k_win_sample, v_win_sample, sgu_v_sample))
```

```python
import numpy as np
from contextlib import ExitStack
import concourse.bass as bass
import concourse.mybir as mybir
from concourse.bass_utils import run_bass_kernel_spmd

F32 = mybir.dt.float32
BF16 = mybir.dt.bfloat16
I32 = mybir.dt.int32
U32 = mybir.dt.uint32
ALU = mybir.AluOpType
AF = mybir.ActivationFunctionType
AX = mybir.AxisListType


class Ctr:
    LIM = 30000

    def __init__(self, K, name, step):
        self.K, self.name, self.step = K, name, step
        self.sems = []
        self._new()

    def _new(self):
        s = self.K.stack.enter_context(self.K.nc.semaphore(f"{self.name}_{len(self.sems)}"))
        self.sems.append(s)
        self.val = 0

    def next(self):
        if self.val + self.step > self.LIM:
            self._new()
        self.val += self.step
        return (self.sems[-1], self.val)


class Buf:
    def __init__(self, K, ap, name):
        self.K, self.ap, self.name = K, ap, name
        self.w = None
        self.r = {}
        self._d = None

    @property
    def dctr(self):
        if self._d is None:
            self._d = Ctr(self.K, "d_" + self.name, 16)
        return self._d

    def __getitem__(self, k):
        return self.ap[k]


class Eng:
    def __init__(self, K, name, obj, counted):
        self.name, self.obj = name, obj
        self.ctr = Ctr(K, "e_" + name, 1) if counted else None
        self.known = {}
        self.prog = []

    def filt(self, deps):
        best = {}
        for (s, v) in deps:
            if self.name == "pe" and self.ctr is not None and s in self.ctr.sems:
                continue
            if self.known.get(s, 0) >= v:
                continue
            if best.get(s, 0) < v:
                best[s] = v
        for s, v in best.items():
            self.known[s] = v
        return list(best.items())


class K:
    def __init__(self, nc, stack):
        self.nc, self.stack = nc, stack
        self.eng = {
            "pe": Eng(self, "pe", nc.tensor, True),
            "act": Eng(self, "act", nc.scalar, True),
            "dve": Eng(self, "dve", nc.vector, True),
            "pool": Eng(self, "pool", nc.gpsimd, True),
            "sp": Eng(self, "sp", nc.sync, False),
        }
        self.bufs = []
        self.n = 0

    def sb(self, name, shape, dt):
        t = self.stack.enter_context(self.nc.sbuf_tensor(name, list(shape), dt))
        b = Buf(self, t, name)
        self.bufs.append(b)
        return b

    def sb_once(self, name, shape, dt):
        if not hasattr(self, "_once"):
            self._once = {}
        if name not in self._once:
            self._once[name] = self.sb(name, shape, dt)
        return self._once[name]

    def ps(self, name, shape, dt):
        t = self.stack.enter_context(self.nc.psum_tensor(name, list(shape), dt))
        b = Buf(self, t, name)
        self.bufs.append(b)
        return b

    def view(self, ap, name):
        b = Buf(self, ap, name)
        self.bufs.append(b)
        return b

    def _deps(self, reads, writes):
        deps = []
        for b in reads:
            if b.w:
                deps.append(b.w)
        for b in writes:
            if b.w:
                deps.append(b.w)
            deps.extend(b.r.items())
        return deps

    def _commit(self, tok, reads, writes):
        for b in reads:
            if b.r.get(tok[0], 0) < tok[1]:
                b.r[tok[0]] = tok[1]
        for b in writes:
            b.w = tok
            b.r = {}

    def op(self, eng, fn, reads=(), writes=()):
        E = self.eng[eng]
        waits = E.filt(self._deps(reads, writes))
        tok = E.ctr.next()
        E.prog.append((waits, fn, tok, 1))
        self._commit(tok, reads, writes)
        self.n += 1

    def dma(self, q, fn, cbuf, reads=(), writes=()):
        E = self.eng[q]
        waits = E.filt(self._deps(reads, writes))
        tok = cbuf.dctr.next()
        E.prog.append((waits, fn, tok, 16))
        self._commit(tok, reads, writes)
        self.n += 1

    def barrier(self):
        deps = []
        for b in self.bufs:
            if b.w:
                deps.append(b.w)
            deps.extend(b.r.items())
        for E in self.eng.values():
            waits = E.filt(deps)
            if waits:
                E.prog.append((waits, None, None, 0))
        for b in self.bufs:
            b.w = None
            b.r = {}

    def finish(self):
        E = self.eng["sp"]
        deps = []
        for b in self.bufs:
            if b.w:
                deps.append(b.w)
            deps.extend(b.r.items())
        waits = E.filt(deps)
        E.prog.append((waits, None, None, 0))
        nc = self.nc

        def replay(name, e):
            for (waits, fn, tok, inc) in self.eng[name].prog:
                for (s, v) in waits:
                    e.wait_ge(s, v)
                if fn is not None:
                    ins = fn(e)
                    ins.then_inc(tok[0], inc)

        with nc.Block() as block:
            @block.sync
            def _(e):
                replay("sp", e)

            @block.tensor
            def _(e):
                replay("pe", e)

            @block.scalar
            def _(e):
                replay("act", e)

            @block.vector
            def _(e):
                replay("dve", e)

            @block.gpsimd
            def _(e):
                replay("pool", e)


D = 2048
NCH = 16
PH, PK, PTOP = 8, 128, 16
NHC = 16
NEG = -1e30


class Common:
    def __init__(self, k):
        self.k = k
        identi = k.sb("identi", [128, 128], I32)
        self.ident = k.sb("ident", [128, 128], F32)
        k.op("pool", lambda e: e.iota(identi[:], pattern=[[1, 128]], base=0, channel_multiplier=-1), writes=[identi])
        k.op("dve", lambda e: e.tensor_single_scalar(out=self.ident[:], in_=identi[:], scalar=0, op=ALU.is_equal),
             reads=[identi], writes=[self.ident])
        self.ones_bf = k.sb("ones_bf", [128, 128], BF16)
        k.op("dve", lambda e: e.memset(self.ones_bf[:], 1.0), writes=[self.ones_bf])
        io16 = k.sb("io16i", [128, 16], I32)
        self.iota16 = k.sb("iota16", [128, 16], F32)
        k.op("pool", lambda e: e.iota(io16[:], pattern=[[1, 16]], base=0, channel_multiplier=0), writes=[io16])
        k.op("dve", lambda e: e.tensor_copy(out=self.iota16[:], in_=io16[:]), reads=[io16], writes=[self.iota16])
        self.bank = [k.ps(f"bank{i}", [128, 512], F32) for i in range(8)]


def rms_modulate(k, C, xT, gm, sh, hT32, hTb, T=128):
    sq = k.sb_once("rm_sq", [128, NCH, 128], BF16)
    rs = k.sb_once("rm_rstd", [128, 128], F32)
    pb = C.bank[0]
    k.op("act", lambda e: e.activation(out=sq[:, :, 0:T], in_=xT[:, :, 0:T], func=AF.Square), reads=[xT], writes=[sq])
    for c in range(NCH):
        k.op("pe", lambda e, c=c: e.matmul(pb[:, 0:T], lhsT=C.ones_bf[:], rhs=sq[:, c, 0:T], start=(c == 0), stop=(c == NCH - 1)),
             reads=[sq, C.ones_bf], writes=[pb])
    k.op("dve", lambda e: e.tensor_scalar(out=rs[:, 0:T], in0=pb[:, 0:T], scalar1=1.0 / D, scalar2=1e-6, op0=ALU.mult, op1=ALU.add),
         reads=[pb], writes=[rs])
    k.op("act", lambda e: e.activation(out=rs[:, 0:T], in_=rs[:, 0:T], func=AF.Sqrt), reads=[rs], writes=[rs])
    k.op("dve", lambda e: e.reciprocal(out=rs[:, 0:T], in_=rs[:, 0:T]), reads=[rs], writes=[rs])
    k.op("dve", lambda e: e.tensor_tensor(out=hT32[:, :, 0:T], in0=xT[:, :, 0:T],
                                          in1=rs[:, 0:T].unsqueeze(1).to_broadcast([128, NCH, T]), op=ALU.mult),
         reads=[xT, rs], writes=[hT32])
    k.op("dve", lambda e: e.tensor_tensor(out=hT32[:, :, 0:T], in0=hT32[:, :, 0:T],
                                          in1=gm[:, :].unsqueeze(2).to_broadcast([128, NCH, T]), op=ALU.mult),
         reads=[hT32, gm], writes=[hT32])
    k.op("dve", lambda e: e.tensor_tensor(out=hT32[:, :, 0:T], in0=hT32[:, :, 0:T],
                                          in1=sh[:, :].unsqueeze(2).to_broadcast([128, NCH, T]), op=ALU.add),
         reads=[hT32, sh], writes=[hT32])
    if hTb is not None:
        k.op("act", lambda e: e.copy(out=hTb[:, :, 0:T], in_=hT32[:, :, 0:T]), reads=[hT32], writes=[hTb])


def to_token_major(k, C, srcT, dst, T=128):
    for b4 in range(4):
        pb = C.bank[1 + (b4 % 2)]
        for j in range(4):
            c = b4 * 4 + j
            k.op("pe", lambda e, c=c, j=j, pb=pb: e.transpose(pb[0:T, j * 128:(j + 1) * 128], srcT[:, c, 0:T], C.ident[:]),
                 reads=[srcT, C.ident], writes=[pb])
        k.op("act", lambda e, b4=b4, pb=pb: e.copy(out=dst[0:T, b4 * 512:(b4 + 1) * 512], in_=pb[0:T, :]), reads=[pb], writes=[dst])


def gated_residual_add(k, C, xT, out_tm, gate, T=128):
    for b4 in range(4):
        pb = C.bank[1 + (b4 % 2)]
        for j in range(4):
            c = b4 * 4 + j
            k.op("pe", lambda e, c=c, j=j, pb=pb: e.transpose(pb[:, j * 128:j * 128 + T], out_tm[0:T, c * 128:(c + 1) * 128], C.ident[0:T, 0:T]),
                 reads=[out_tm, C.ident], writes=[pb])
        for j in range(4):
            c = b4 * 4 + j
            k.op("dve", lambda e, c=c, j=j, pb=pb: e.scalar_tensor_tensor(out=xT[:, c, 0:T], in0=pb[:, j * 128:j * 128 + T], scalar=gate[:, c:c + 1],
                                                                          in1=xT[:, c, 0:T], op0=ALU.mult, op1=ALU.add),
                 reads=[pb, gate, xT], writes=[xT])


class PeerW:
    def __init__(self, k, wq_b, skT_b, u_tab, v_tab):
        self.wq, self.skT, self.u, self.v = wq_b, skT_b, u_tab, v_tab


def peer(k, C, xT, gm, sh, gate, W, S, T=128, ws=None, mid_barrier=False):
    rms_modulate(k, C, xT, gm, sh, S.hT32, S.hTb, T)
    to_token_major(k, C, S.hT32, S.h_tm, T)
    if ws is not None:
        peer_q_streamed(k, C, ws, W.wq, S, T)
    else:
        for hc in range(NHC):
            pb = C.bank[3 + (hc % 2)]
            for c in range(NCH):
                k.op("pe", lambda e, hc=hc, c=c, pb=pb: e.matmul(pb[:, 0:T], lhsT=W.wq[:, c, hc * 128:(hc + 1) * 128], rhs=S.hTb[:, c, 0:T],
                                                                start=(c == 0), stop=(c == NCH - 1)), reads=[W.wq, S.hTb], writes=[pb])
            k.op("act", lambda e, hc=hc, pb=pb: e.copy(out=S.qT[:, hc, 0:T], in_=pb[:, 0:T]), reads=[pb], writes=[S.qT])
    for hc in range(NHC):
        pb = C.bank[5 + (hc // 4) % 2]
        k.op("pe", lambda e, hc=hc, pb=pb: e.matmul(pb[0:T, (hc % 4) * 128:(hc % 4 + 1) * 128], lhsT=S.qT[:, hc, 0:T], rhs=W.skT[:, hc, :], start=True, stop=True),
             reads=[S.qT, W.skT], writes=[pb])
        if hc % 4 == 3:
            k.op("act", lambda e, hc=hc, pb=pb: e.copy(out=S.s[0:T, hc - 3:hc + 1, :], in_=pb[0:T, :].rearrange("p (a b) -> p a b", a=4)), reads=[pb], writes=[S.s])
    for hc in range(NHC):
        k.op("dve", lambda e, hc=hc: e.max(out=S.sv[0:T, hc, 0:8], in_=S.s[0:T, hc, :]), reads=[S.s], writes=[S.sv])
        k.op("dve", lambda e, hc=hc: e.max_index(out=S.si[0:T, hc, 0:8], in_max=S.sv[0:T, hc, 0:8], in_values=S.s[0:T, hc, :]), reads=[S.s, S.sv], writes=[S.si])
        k.op("dve", lambda e, hc=hc: e.match_replace(out=S.s2[0:T, hc, :], in_to_replace=S.sv[0:T, hc, 0:8], in_values=S.s[0:T, hc, :], imm_value=NEG), reads=[S.s, S.sv], writes=[S.s2])
        k.op("dve", lambda e, hc=hc: e.max(out=S.sv[0:T, hc, 8:16], in_=S.s2[0:T, hc, :]), reads=[S.s2], writes=[S.sv])
        k.op("dve", lambda e, hc=hc: e.max_index(out=S.si[0:T, hc, 8:16], in_max=S.sv[0:T, hc, 8:16], in_values=S.s2[0:T, hc, :]), reads=[S.s2, S.sv], writes=[S.si])
    k.op("dve", lambda e: e.tensor_copy(out=S.sif[0:T], in_=S.si[0:T]), reads=[S.si], writes=[S.sif])
    sv4 = S.sv.ap.rearrange("p (h c) j -> p h c j", c=2)
    sif4 = S.sif.ap.rearrange("p (h c) j -> p h c j", c=2)
    for h in range(PH):
        k.op("dve", lambda e, h=h: e.tensor_tensor(out=S.cand[0:T, h, :, :], in0=sv4[0:T, h, 0, :].unsqueeze(2).to_broadcast([T, 16, 16]),
                                                   in1=sv4[0:T, h, 1, :].unsqueeze(1).to_broadcast([T, 16, 16]), op=ALU.add), reads=[S.sv], writes=[S.cand])
    candf = S.cand.ap.rearrange("p h a b -> p h (a b)")
    cand2f = S.cand2.ap.rearrange("p h a b -> p h (a b)")
    for h in range(PH):
        k.op("dve", lambda e, h=h: e.max(out=S.best[0:T, h, 0:8], in_=candf[0:T, h, :]), reads=[S.cand], writes=[S.best])
        k.op("dve", lambda e, h=h: e.max_index(out=S.bi[0:T, h, 0:8], in_max=S.best[0:T, h, 0:8], in_values=candf[0:T, h, :]), reads=[S.cand, S.best], writes=[S.bi])
        k.op("dve", lambda e, h=h: e.match_replace(out=cand2f[0:T, h, :], in_to_replace=S.best[0:T, h, 0:8], in_values=candf[0:T, h, :], imm_value=NEG), reads=[S.cand, S.best], writes=[S.cand2])
        k.op("dve", lambda e, h=h: e.max(out=S.best[0:T, h, 8:16], in_=cand2f[0:T, h, :]), reads=[S.cand2], writes=[S.best])
        k.op("dve", lambda e, h=h: e.max_index(out=S.bi[0:T, h, 8:16], in_max=S.best[0:T, h, 8:16], in_values=cand2f[0:T, h, :]), reads=[S.cand2, S.best], writes=[S.bi])
    k.op("dve", lambda e: e.tensor_single_scalar(out=S.b1[0:T], in_=S.bi[0:T], scalar=4, op=ALU.logical_shift_right), reads=[S.bi], writes=[S.b1])
    k.op("dve", lambda e: e.tensor_single_scalar(out=S.b2[0:T], in_=S.bi[0:T], scalar=15, op=ALU.bitwise_and), reads=[S.bi], writes=[S.b2])
    k.op("dve", lambda e: e.tensor_copy(out=S.b1f[0:T], in_=S.b1[0:T]), reads=[S.b1], writes=[S.b1f])
    k.op("dve", lambda e: e.tensor_copy(out=S.b2f[0:T], in_=S.b2[0:T]), reads=[S.b2], writes=[S.b2f])
    for (bf, half, dst) in ((S.b1f, 0, S.I1), (S.b2f, 1, S.I2)):
        for h in range(PH):
            k.op("dve", lambda e, bf=bf, h=h: e.tensor_tensor(out=S.oh[0:T], in0=C.iota16[0:T, :].unsqueeze(1).to_broadcast([T, 16, 16]),
                                                              in1=bf[0:T, h, :].unsqueeze(2).to_broadcast([T, 16, 16]), op=ALU.is_equal), reads=[C.iota16, bf], writes=[S.oh])
            k.op("dve", lambda e, h=h, half=half: e.tensor_tensor(out=S.oh[0:T], in0=S.oh[0:T], in1=sif4[0:T, h, half, :].unsqueeze(1).to_broadcast([T, 16, 16]), op=ALU.mult),
                 reads=[S.oh, S.sif], writes=[S.oh])
            k.op("dve", lambda e, h=h, dst=dst: e.tensor_reduce(out=dst[0:T, h, :], in_=S.oh[0:T], axis=AX.X, op=ALU.add), reads=[S.oh], writes=[dst])
    k.op("dve", lambda e: e.scalar_tensor_tensor(out=S.ef[0:T], in0=S.I1[0:T], scalar=float(PK), in1=S.I2[0:T], op0=ALU.mult, op1=ALU.add), reads=[S.I1, S.I2], writes=[S.ef])
    k.op("dve", lambda e: e.tensor_copy(out=S.ei[0:T], in_=S.ef[0:T]), reads=[S.ef], writes=[S.ei])
    k.op("dve", lambda e: e.tensor_tensor(out=S.g[0:T], in0=S.best[0:T], in1=S.best[0:T, :, 0:1].to_broadcast([T, PH, 16]), op=ALU.subtract), reads=[S.best], writes=[S.g])
    k.op("act", lambda e: e.activation(out=S.g[0:T], in_=S.g[0:T], func=AF.Exp), reads=[S.g], writes=[S.g])
    k.op("dve", lambda e: e.tensor_reduce(out=S.gs[0:T], in_=S.g[0:T], axis=AX.X, op=ALU.add), reads=[S.g], writes=[S.gs])
    k.op("dve", lambda e: e.reciprocal(out=S.gs[0:T], in_=S.gs[0:T]), reads=[S.gs], writes=[S.gs])
    k.op("dve", lambda e: e.tensor_tensor(out=S.g[0:T], in0=S.g[0:T], in1=S.gs[0:T].unsqueeze(2).to_broadcast([T, PH, 16]), op=ALU.mult), reads=[S.g, S.gs], writes=[S.g])
    if mid_barrier:
        k.barrier()
    eif = S.ei.ap.rearrange("p h k -> p (h k)")
    af = S.a.ap.rearrange("p h k -> p (h k)")
    nb = len(S.gbuf)
    for hk in range(PH * PTOP):
        gb = S.gbuf[hk % nb]
        k.dma("pool", lambda e, hk=hk, gb=gb: e.indirect_dma_start(out=gb[0:T, :], out_offset=None, in_=W.u,
              in_offset=bass.IndirectOffsetOnAxis(ap=eif[0:T, hk:hk + 1], axis=0)), gb, reads=[S.ei], writes=[gb])
        k.op("dve", lambda e, hk=hk, gb=gb: e.scalar_tensor_tensor(out=S.junk[0:T], in0=gb[0:T, :], scalar=1.0, in1=S.h_tm[0:T, :], op0=ALU.mult, op1=ALU.mult,
                                                                   accum_out=af[0:T, hk:hk + 1]), reads=[gb, S.h_tm], writes=[S.junk, S.a])
    k.op("act", lambda e: e.activation(out=S.w[0:T], in_=S.a[0:T], func=AF.Gelu_apprx_tanh), reads=[S.a], writes=[S.w])
    k.op("dve", lambda e: e.tensor_tensor(out=S.w[0:T], in0=S.w[0:T], in1=S.g[0:T], op=ALU.mult), reads=[S.w, S.g], writes=[S.w])
    wf = S.w.ap.rearrange("p h k -> p (h k)")
    for hk in range(PH * PTOP):
        gb = S.gbuf[hk % nb]
        k.dma("pool", lambda e, hk=hk, gb=gb: e.indirect_dma_start(out=gb[0:T, :], out_offset=None, in_=W.v,
              in_offset=bass.IndirectOffsetOnAxis(ap=eif[0:T, hk:hk + 1], axis=0)), gb, reads=[S.ei], writes=[gb])
        if hk == 0:
            k.op("dve", lambda e, gb=gb: e.tensor_scalar(out=S.acc[0:T, :], in0=gb[0:T, :], scalar1=wf[0:T, 0:1], scalar2=None, op0=ALU.mult), reads=[gb, S.w], writes=[S.acc])
        else:
            k.op("dve", lambda e, hk=hk, gb=gb: e.scalar_tensor_tensor(out=S.acc[0:T, :], in0=gb[0:T, :], scalar=wf[0:T, hk:hk + 1], in1=S.acc[0:T, :], op0=ALU.mult, op1=ALU.add),
                 reads=[gb, S.w, S.acc], writes=[S.acc])
    gated_residual_add(k, C, xT, S.acc, gate, T)


class NS:
    pass


def peer_scratch(k, ngb=4):
    S = NS()
    S.hT32 = k.sb_once("p_hT32", [128, NCH, 128], F32)
    S.hTb = k.sb_once("p_hTb", [128, NCH, 128], BF16)
    S.h_tm = k.sb("p_h_tm", [128, D], F32)
    S.qT = k.sb("p_qT", [128, NHC, 128], BF16)
    S.s = k.sb("p_s", [128, NHC, PK], F32)
    S.s2 = k.sb("p_s2", [128, NHC, PK], F32)
    S.sv = k.sb("p_sv", [128, NHC, 16], F32)
    S.si = k.sb("p_si", [128, NHC, 16], U32)
    S.sif = k.sb("p_sif", [128, NHC, 16], F32)
    S.cand = k.sb("p_cand", [128, PH, 16, 16], F32)
    S.cand2 = k.sb("p_cand2", [128, PH, 16, 16], F32)
    S.best = k.sb("p_best", [128, PH, 16], F32)
    S.bi = k.sb("p_bi", [128, PH, 16], U32)
    S.b1 = k.sb("p_b1", [128, PH, 16], U32)
    S.b2 = k.sb("p_b2", [128, PH, 16], U32)
    S.b1f = k.sb("p_b1f", [128, PH, 16], F32)
    S.b2f = k.sb("p_b2f", [128, PH, 16], F32)
    S.oh = k.sb("p_oh", [128, 16, 16], F32)
    S.I1 = k.sb("p_I1", [128, PH, 16], F32)
    S.I2 = k.sb("p_I2", [128, PH, 16], F32)
    S.ef = k.sb("p_ef", [128, PH, 16], F32)
    S.ei = k.sb("p_ei", [128, PH, 16], I32)
    S.g = k.sb("p_g", [128, PH, 16], F32)
    S.gs = k.sb("p_gs", [128, PH], F32)
    S.a = k.sb("p_a", [128, PH, 16], F32)
    S.w = k.sb("p_w", [128, PH, 16], F32)
    S.junk = k.sb("p_junk", [128, D], F32)
    S.acc = k.sb_once("p_acc", [128, D], F32)
    S.gbuf = [k.sb(f"p_gb{i}", [128, D], F32) for i in range(ngb)]
    return S


class WS:
    def __init__(self, k, nslots=4, q="sp"):
        self.k, self.q, self.i = k, q, 0
        self.slots = [k.sb(f"wslot{i}", [128, 16, 512], BF16) for i in range(nslots)]
        self.bslots = [k.sb(f"bslot{i}", [128, 512], F32) for i in range(4)]
        self.bi = 0
        self.cmap = {}

    def block(self, w2d, kc0, col0, nkc=16, ncols=512, cache=True):
        s = self.slots[self.i % len(self.slots)]
        self.i += 1
        ca = getattr(self, "cache_ap", None)
        key = (w2d.tensor.name, kc0, col0)
        full = (nkc == 16 and ncols == 512)
        if ca is not None and cache and full and key in self.cmap:
            cbuf, ci = self.cmap[key]
            self.k.dma("sp", lambda e, s=s, ca=ca, ci=ci: e.dma_start(out=s[:, :, :], in_=ca[ci].rearrange("p (c n) -> p c n", c=16)), s, reads=[cbuf], writes=[s])
            return s
        src = w2d[kc0 * 128:(kc0 + nkc) * 128, col0:col0 + ncols].rearrange("(c p) n -> p c n", p=128)
        self.k.dma(self.q, lambda e: e.dma_start(out=s[:, 0:nkc, 0:ncols], in_=src), s, writes=[s])
        if ca is not None and cache and full and len(self.cmap) < ca.shape[0]:
            ci = len(self.cmap)
            cbuf = self.k.view(ca[ci], f"wc{ci}")
            self.cmap[key] = (cbuf, ci)
            self.k.dma("sp", lambda e, s=s, ca=ca, ci=ci: e.dma_start(out=ca[ci].rearrange("p (c n) -> p c n", c=16), in_=s[:, :, :]), s, reads=[s], writes=[cbuf])
        return s

    def row_bcast(self, row1d, col0, ncols=512):
        s = self.bslots[self.bi % len(self.bslots)]
        self.bi += 1
        src = row1d[col0:col0 + ncols].unsqueeze(0).partition_broadcast(128)
        self.k.dma(self.q, lambda e: e.dma_start(out=s[:, 0:ncols], in_=src), s, writes=[s])
        return s


SGD = 6144


def sgu(k, C, ws, xT, gm, sh, gate, Wd, S, T=128, v_out=None):
    rms_modulate(k, C, xT, gm, sh, S.hT32, S.hTb, T)
    for nb in range(24):
        slot = ws.block(Wd["w_in"], 0, nb * 512)
        brow = ws.row_bcast(Wd["b_in"], nb * 512)
        pb = C.bank[3 + nb % 2]
        for c in range(NCH):
            k.op("pe", lambda e, c=c, pb=pb, slot=slot: e.matmul(pb[0:T, :], lhsT=S.hTb[:, c, 0:T], rhs=slot[:, c, :], start=(c == 0), stop=(c == NCH - 1)),
                 reads=[S.hTb, slot], writes=[pb])
        off = (nb % 12) * 512
        if nb < 12:
            k.op("dve", lambda e, pb=pb, brow=brow: e.tensor_tensor(out=S.tmp[0:T, :], in0=pb[0:T, :], in1=brow[0:T, :], op=ALU.add), reads=[pb, brow], writes=[S.tmp])
            k.op("act", lambda e, off=off: e.activation(out=S.u[0:T, off:off + 512], in_=S.tmp[0:T, :], func=AF.Gelu_apprx_tanh), reads=[S.tmp], writes=[S.u])
        else:
            k.op("dve", lambda e, pb=pb, brow=brow, off=off: e.tensor_tensor(out=S.v[0:T, off:off + 512], in0=pb[0:T, :], in1=brow[0:T, :], op=ALU.add), reads=[pb, brow], writes=[S.v])
            k.op("act", lambda e, off=off: e.activation(out=S.v[0:T, off:off + 512], in_=S.v[0:T, off:off + 512], func=AF.Gelu_apprx_tanh), reads=[S.v], writes=[S.v])
    k.op("dve", lambda e: e.tensor_reduce(out=S.st[0:T, 0:1], in_=S.v[0:T, :], axis=AX.X, op=ALU.add), reads=[S.v], writes=[S.st])
    k.op("act", lambda e: e.activation(out=S.vb[0:T, :], in_=S.v[0:T, :], func=AF.Square, accum_out=S.st[0:T, 1:2]), reads=[S.v, S.st], writes=[S.vb, S.st])
    k.op("dve", lambda e: e.tensor_scalar(out=S.st[0:T, 0:2], in0=S.st[0:T, 0:2], scalar1=1.0 / SGD, scalar2=None, op0=ALU.mult), reads=[S.st], writes=[S.st])
    k.op("dve", lambda e: e.tensor_tensor(out=S.st[0:T, 2:3], in0=S.st[0:T, 0:1], in1=S.st[0:T, 0:1], op=ALU.mult), reads=[S.st], writes=[S.st])
    k.op("dve", lambda e: e.tensor_tensor(out=S.st[0:T, 2:3], in0=S.st[0:T, 1:2], in1=S.st[0:T, 2:3], op=ALU.subtract), reads=[S.st], writes=[S.st])
    k.op("dve", lambda e: e.tensor_scalar(out=S.st[0:T, 2:3], in0=S.st[0:T, 2:3], scalar1=1e-6, scalar2=None, op0=ALU.add), reads=[S.st], writes=[S.st])
    k.op("act", lambda e: e.activation(out=S.st[0:T, 2:3], in_=S.st[0:T, 2:3], func=AF.Sqrt), reads=[S.st], writes=[S.st])
    k.op("dve", lambda e: e.reciprocal(out=S.st[0:T, 2:3], in_=S.st[0:T, 2:3]), reads=[S.st], writes=[S.st])
    k.op("dve", lambda e: e.tensor_scalar(out=S.v[0:T, :], in0=S.v[0:T, :], scalar1=S.st[0:T, 0:1], scalar2=S.st[0:T, 2:3], op0=ALU.subtract, op1=ALU.mult),
         reads=[S.v, S.st], writes=[S.v])
    for nb in range(12):
        grow = ws.row_bcast(Wd["ln_g"], nb * 512)
        brow = ws.row_bcast(Wd["ln_b"], nb * 512)
        k.op("dve", lambda e, nb=nb, grow=grow: e.tensor_tensor(out=S.v[0:T, nb * 512:(nb + 1) * 512], in0=S.v[0:T, nb * 512:(nb + 1) * 512], in1=grow[0:T, :], op=ALU.mult),
             reads=[S.v, grow], writes=[S.v])
        k.op("dve", lambda e, nb=nb, brow=brow: e.tensor_tensor(out=S.v[0:T, nb * 512:(nb + 1) * 512], in0=S.v[0:T, nb * 512:(nb + 1) * 512], in1=brow[0:T, :], op=ALU.add),
             reads=[S.v, brow], writes=[S.v])
    if v_out is not None:
        k.dma("sp", lambda e: e.dma_start(out=v_out, in_=S.v[0:T, :]), S.v, reads=[S.v])
    k.op("act", lambda e: e.copy(out=S.vb[0:T, :], in_=S.v[0:T, :]), reads=[S.v], writes=[S.vb])
    for g in range(8):
        for (o, n) in ((0, 512), (512, 256)):
            pb = C.bank[5 + (2 * g + (o > 0)) % 2]
            c0 = g * 768 + o
            k.op("pe", lambda e, g=g, pb=pb, c0=c0, n=n: e.matmul(pb[0:T, 0:n], lhsT=Wd["wsT"][0:T, g, 0:T], rhs=S.vb[0:T, c0:c0 + n], start=True, stop=True),
                 reads=[Wd["wsT"], S.vb], writes=[pb])
            k.op("dve", lambda e, g=g, pb=pb, c0=c0, n=n: e.scalar_tensor_tensor(out=S.prod[0:T, c0:c0 + n], in0=pb[0:T, 0:n], scalar=Wd["bs"][0:T, g:g + 1],
                                                                                in1=S.u[0:T, c0:c0 + n], op0=ALU.add, op1=ALU.mult), reads=[pb, Wd["bs"], S.u], writes=[S.prod])
    for b4 in range(12):
        pb = C.bank[1 + b4 % 2]
        for j in range(4):
            c = b4 * 4 + j
            k.op("pe", lambda e, c=c, j=j, pb=pb: e.transpose(pb[:, j * 128:j * 128 + T], S.prod[0:T, c * 128:(c + 1) * 128], C.ident[0:T, 0:T]), reads=[S.prod, C.ident], writes=[pb])
        k.op("act", lambda e, b4=b4, pb=pb: e.copy(out=S.prodT[:, b4 * 4:(b4 + 1) * 4, 0:T], in_=pb[:, :].rearrange("p (a b) -> p a b", a=4)[:, :, 0:T]), reads=[pb], writes=[S.prodT])
    for nb in range(4):
        pb = C.bank[3 + nb % 2]
        for ku in range(3):
            slot = ws.block(Wd["w_out"], ku * 16, nb * 512)
            for c in range(16):
                kc = ku * 16 + c
                k.op("pe", lambda e, kc=kc, c=c, pb=pb, slot=slot: e.matmul(pb[0:T, :], lhsT=S.prodT[:, kc, 0:T], rhs=slot[:, c, :], start=(kc == 0), stop=(kc == 47)),
                     reads=[S.prodT, slot], writes=[pb])
        k.op("act", lambda e, nb=nb, pb=pb: e.copy(out=S.out_tm[0:T, nb * 512:(nb + 1) * 512], in_=pb[0:T, :]), reads=[pb], writes=[S.out_tm])
    gated_residual_add(k, C, xT, S.out_tm, gate, T)


def sgu_scratch(k):
    S = NS()
    S.hT32 = k.sb_once("p_hT32", [128, NCH, 128], F32)
    S.hTb = k.sb_once("p_hTb", [128, NCH, 128], BF16)
    S.u = k.sb("g_u", [128, SGD], BF16)
    S.v = k.sb("g_v", [128, SGD], F32)
    S.vb = k.sb("g_vb", [128, SGD], BF16)
    S.prod = S.v
    S.tmp = k.sb("g_tmp", [128, 512], F32)
    S.prodT = k.sb("g_prodT", [128, 48, 128], BF16)
    S.st = k.sb("g_st", [128, 4], F32)
    S.out_tm = k.sb_once("p_acc", [128, D], F32)
    return S


CW = 31


def conv_load_hist_zero(k, S):
    k.op("dve", lambda e: e.memset(S.xp[:, :, 0:CW - 1], 0.0), writes=[S.xp])


def conv_load_hist_state(k, C, S, state_dram):
    k.dma("sp", lambda e: e.dma_start(out=S.tm30[0:CW - 1, :], in_=state_dram), S.tm30, writes=[S.tm30])
    for b4 in range(4):
        pb = C.bank[1 + b4 % 2]
        for j in range(4):
            c = b4 * 4 + j
            k.op("pe", lambda e, c=c, j=j, pb=pb: e.transpose(pb[:, j * 128:j * 128 + CW - 1], S.tm30[0:CW - 1, c * 128:(c + 1) * 128], C.ident[0:CW - 1, 0:CW - 1]),
                 reads=[S.tm30, C.ident], writes=[pb])
        k.op("act", lambda e, b4=b4, pb=pb: e.copy(out=S.xp[:, b4 * 4:(b4 + 1) * 4, 0:CW - 1], in_=pb[:, :].rearrange("p (a b) -> p a b", a=4)[:, :, 0:CW - 1]),
             reads=[pb], writes=[S.xp])


def conv(k, C, ws, xT, gm, sh, gate, Wd, S, T=128, state_out=None, carry=True):
    H = CW - 1
    rms_modulate(k, C, xT, gm, sh, S.hT32, S.hTb, T)
    for b in range(4):
        sl = ws.block(Wd["w_in"], 0, b * 512)
        sg = ws.block(Wd["w_in"], 0, (b + 4) * 512)
        pl, pg = C.bank[3 + b % 2], C.bank[5 + b % 2]
        for j in range(4):
            for c in range(NCH):
                k.op("pe", lambda e, j=j, c=c, pl=pl, sl=sl: e.matmul(pl[:, j * 128:j * 128 + T], lhsT=sl[:, c, j * 128:(j + 1) * 128], rhs=S.hTb[:, c, 0:T], start=(c == 0), stop=(c == NCH - 1)),
                     reads=[sl, S.hTb], writes=[pl])
            for c in range(NCH):
                k.op("pe", lambda e, j=j, c=c, pg=pg, sg=sg: e.matmul(pg[:, j * 128:j * 128 + T], lhsT=sg[:, c, j * 128:(j + 1) * 128], rhs=S.hTb[:, c, 0:T], start=(c == 0), stop=(c == NCH - 1)),
                     reads=[sg, S.hTb], writes=[pg])
        for j in range(4):
            ch = b * 4 + j
            k.op("act", lambda e, j=j, ch=ch, pg=pg: e.activation(out=S.sg[:, 0:T], in_=pg[:, j * 128:j * 128 + T], func=AF.Sigmoid, bias=Wd["b_in"][:, 16 + ch:17 + ch]),
                 reads=[pg, Wd["b_in"]], writes=[S.sg])
            k.op("dve", lambda e, j=j, ch=ch, pl=pl: e.scalar_tensor_tensor(out=S.xp[:, ch, H:H + T], in0=pl[:, j * 128:j * 128 + T], scalar=Wd["b_in"][:, ch:ch + 1], in1=S.sg[:, 0:T],
                                                                            op0=ALU.add, op1=ALU.mult), reads=[pl, Wd["b_in"], S.sg], writes=[S.xp])
    if state_out is not None:
        for b4 in range(4):
            pb = C.bank[1 + b4 % 2]
            for j in range(4):
                c = b4 * 4 + j
                k.op("pe", lambda e, c=c, j=j, pb=pb: e.transpose(pb[0:H, j * 128:(j + 1) * 128], S.xp[:, c, T:T + H], C.ident[:]), reads=[S.xp, C.ident], writes=[pb])
            k.op("act", lambda e, b4=b4, pb=pb: e.copy(out=S.tm30[0:H, b4 * 512:(b4 + 1) * 512], in_=pb[0:H, :]), reads=[pb], writes=[S.tm30])
        k.dma("sp", lambda e: e.dma_start(out=state_out, in_=S.tm30[0:H, :]), S.tm30, reads=[S.tm30])
    for c in range(NCH):
        k.op("act", lambda e, c=c: e.activation(out=S.y[:, c, 0:T], in_=S.xp[:, c, 0:T], func=AF.Identity, scale=Wd["dw"][:, c, 0:1], bias=Wd["dw_b"][:, c:c + 1]),
             reads=[S.xp, Wd["dw"], Wd["dw_b"]], writes=[S.y])
        for kk in range(1, CW):
            k.op("dve", lambda e, c=c, kk=kk: e.scalar_tensor_tensor(out=S.y[:, c, 0:T], in0=S.xp[:, c, kk:kk + T], scalar=Wd["dw"][:, c, kk:kk + 1], in1=S.y[:, c, 0:T],
                                                                     op0=ALU.mult, op1=ALU.add), reads=[S.xp, Wd["dw"], S.y], writes=[S.y])
    if carry:
        k.op("act", lambda e: e.copy(out=S.hist_tmp[:, :, :], in_=S.xp[:, :, T:T + H]), reads=[S.xp], writes=[S.hist_tmp])
        k.op("act", lambda e: e.copy(out=S.xp[:, :, 0:H], in_=S.hist_tmp[:, :, :]), reads=[S.hist_tmp], writes=[S.xp])
    k.op("act", lambda e: e.copy(out=S.yb[:, :, 0:T], in_=S.y[:, :, 0:T]), reads=[S.y], writes=[S.yb])
    k.op("act", lambda e: e.activation(out=S.y2b[:, :, 0:T], in_=S.y[:, :, 0:T], func=AF.Square), reads=[S.y], writes=[S.y2b])
    pb = C.bank[0]
    for (src, off) in ((S.yb, 0), (S.y2b, 128)):
        for c in range(NCH):
            k.op("pe", lambda e, src=src, off=off, c=c, pb=pb: e.matmul(pb[:, off:off + T], lhsT=C.ones_bf[:], rhs=src[:, c, 0:T], start=(c == 0), stop=(c == NCH - 1)),
                 reads=[src, C.ones_bf], writes=[pb])
    k.op("dve", lambda e, pb=pb: e.tensor_scalar(out=S.mv[:, 0:256], in0=pb[:, 0:256], scalar1=1.0 / D, scalar2=None, op0=ALU.mult), reads=[pb], writes=[S.mv])
    k.op("dve", lambda e: e.tensor_tensor(out=S.mv[:, 256:256 + T], in0=S.mv[:, 0:T], in1=S.mv[:, 0:T], op=ALU.mult), reads=[S.mv], writes=[S.mv])
    k.op("dve", lambda e: e.tensor_tensor(out=S.mv[:, 256:256 + T], in0=S.mv[:, 128:128 + T], in1=S.mv[:, 256:256 + T], op=ALU.subtract), reads=[S.mv], writes=[S.mv])
    k.op("dve", lambda e: e.tensor_scalar(out=S.mv[:, 256:256 + T], in0=S.mv[:, 256:256 + T], scalar1=1e-6, scalar2=None, op0=ALU.add), reads=[S.mv], writes=[S.mv])
    k.op("act", lambda e: e.activation(out=S.mv[:, 256:256 + T], in_=S.mv[:, 256:256 + T], func=AF.Sqrt), reads=[S.mv], writes=[S.mv])
    k.op("dve", lambda e: e.reciprocal(out=S.mv[:, 256:256 + T], in_=S.mv[:, 256:256 + T]), reads=[S.mv], writes=[S.mv])
    k.op("dve", lambda e: e.tensor_tensor(out=S.y[:, :, 0:T], in0=S.y[:, :, 0:T], in1=S.mv[:, 0:T].unsqueeze(1).to_broadcast([128, NCH, T]), op=ALU.subtract), reads=[S.y, S.mv], writes=[S.y])
    k.op("dve", lambda e: e.tensor_tensor(out=S.y[:, :, 0:T], in0=S.y[:, :, 0:T], in1=S.mv[:, 256:256 + T].unsqueeze(1).to_broadcast([128, NCH, T]), op=ALU.mult), reads=[S.y, S.mv], writes=[S.y])
    for c in range(NCH):
        k.op("act", lambda e, c=c: e.activation(out=S.yb[:, c, 0:T], in_=S.y[:, c, 0:T], func=AF.Silu, scale=Wd["ln_g"][:, c:c + 1], bias=Wd["ln_b"][:, c:c + 1]),
             reads=[S.y, Wd["ln_g"], Wd["ln_b"]], writes=[S.yb])
    for nb in range(4):
        slot = ws.block(Wd["w_out"], 0, nb * 512)
        brow = ws.row_bcast(Wd["b_out"], nb * 512)
        pb = C.bank[3 + nb % 2]
        for c in range(NCH):
            k.op("pe", lambda e, c=c, pb=pb, slot=slot: e.matmul(pb[0:T, :], lhsT=S.yb[:, c, 0:T], rhs=slot[:, c, :], start=(c == 0), stop=(c == NCH - 1)), reads=[S.yb, slot], writes=[pb])
        k.op("dve", lambda e, nb=nb, pb=pb, brow=brow: e.tensor_tensor(out=S.out_tm[0:T, nb * 512:(nb + 1) * 512], in0=pb[0:T, :], in1=brow[0:T, :], op=ALU.add), reads=[pb, brow], writes=[S.out_tm])
    gated_residual_add(k, C, xT, S.out_tm, gate, T)


def conv_scratch(k):
    S = NS()
    S.hT32 = k.sb_once("p_hT32", [128, NCH, 128], F32)
    S.hTb = k.sb_once("p_hTb", [128, NCH, 128], BF16)
    S.out_tm = k.sb_once("p_acc", [128, D], F32)
    S.xp = k.sb("c_xp", [128, NCH, CW - 1 + 128], F32)
    S.hist_tmp = k.sb("c_hist", [128, NCH, CW - 1], F32)
    S.y = k.sb("c_y", [128, NCH, 128], F32)
    S.yb = k.sb("c_yb", [128, NCH, 128], BF16)
    S.y2b = k.sb("c_y2b", [128, NCH, 128], BF16)
    S.sg = k.sb("c_sg", [128, 128], F32)
    S.mv = k.sb("c_mv", [128, 384], F32)
    S.tm30 = k.sb("c_tm30", [32, D], F32)
    return S


NH, NKV, HD = 32, 4, 64
BIGD = 1e32


def attn_consts(k):
    A = NS()
    di = k.sb("a_di", [128, 256], I32)
    A.Dmid = k.sb("a_Dmid", [128, 256], F32)
    A.Dfirst = k.sb("a_Dfirst", [128, 256], F32)
    A.Dsamp = k.sb("a_Dsamp", [128, 256], F32)
    k.op("pool", lambda e: e.iota(di[:], pattern=[[-1, 256]], base=128, channel_multiplier=1), writes=[di])
    k.op("dve", lambda e: e.tensor_copy(out=A.Dsamp[:], in_=di[:]), reads=[di], writes=[A.Dsamp])
    k.op("dve", lambda e: e.tensor_scalar(out=A.Dmid[:], in0=A.Dsamp[:], scalar1=-1.0, scalar2=None, op0=ALU.mult), reads=[A.Dsamp], writes=[A.Dmid])
    k.op("dve", lambda e: e.tensor_max(out=A.Dsamp[:], in0=A.Dsamp[:], in1=A.Dmid[:]), reads=[A.Dsamp, A.Dmid], writes=[A.Dsamp])
    for Dm in (A.Dmid, A.Dfirst):
        k.op("dve", lambda e, Dm=Dm: e.tensor_copy(out=Dm[:], in_=A.Dsamp[:]), reads=[A.Dsamp], writes=[Dm])
        k.op("dve", lambda e, Dm=Dm: e.memset(Dm[0:64, 192:256], BIGD), writes=[Dm])
        k.op("dve", lambda e, Dm=Dm: e.memset(Dm[64:128, 0:64], BIGD), writes=[Dm])
    k.op("dve", lambda e: e.memset(A.Dfirst[:, 0:128], BIGD), writes=[A.Dfirst])
    return A


def attn_load_prev(k, C, S, ck_dram, cv_dram):
    k.dma("sp", lambda e: e.dma_start(out=S.kv_tm[:, 0:256], in_=ck_dram), S.kv_tm, writes=[S.kv_tm])
    k.dma("sp", lambda e: e.dma_start(out=S.kv_tm[:, 256:512], in_=cv_dram), S.kv_tm, writes=[S.kv_tm])
    pb = C.bank[1]
    for g in range(NKV):
        k.op("pe", lambda e, g=g: e.transpose(pb[0:HD, g * 128:(g + 1) * 128], S.kv_tm[:, g * HD:(g + 1) * HD], C.ident[:]), reads=[S.kv_tm, C.ident], writes=[pb])
    k.op("act", lambda e: e.copy(out=S.kTprev[:, :, :], in_=pb[0:HD, :].rearrange("p (a b) -> p a b", a=NKV)), reads=[pb], writes=[S.kTprev])
    k.op("act", lambda e: e.copy(out=S.vprev[:, :], in_=S.kv_tm[:, 256:512]), reads=[S.kv_tm], writes=[S.vprev])


def attn(k, C, ws, xT, gm, sh, gate, Wd, S, Dm, T=128, kwin_out=None, vwin_out=None, win_rows=None, carry=True):
    slopes = [2.0 ** (-8.0 * (h + 1) / NH) for h in range(NH)]
    rms_modulate(k, C, xT, gm, sh, S.hT32, S.hTb, T)
    for nb in range(4):
        slot = ws.block(Wd["w_qkv"], 0, nb * 512)
        for half in range(2):
            pb = C.bank[1 + half]
            for j in range(4):
                hl = half * 4 + j
                for c in range(NCH):
                    k.op("pe", lambda e, hl=hl, j=j, c=c, pb=pb, slot=slot: e.matmul(pb[0:HD, j * 128:j * 128 + T], lhsT=slot[:, c, hl * HD:(hl + 1) * HD], rhs=S.hTb[:, c, 0:T],
                                                                                start=(c == 0), stop=(c == NCH - 1)), reads=[slot, S.hTb], writes=[pb])
            for j in range(4):
                h = nb * 8 + half * 4 + j
                k.op("dve", lambda e, h=h, j=j, pb=pb: e.tensor_scalar(out=S.qT[:, h, 0:T], in0=pb[0:HD, j * 128:j * 128 + T], scalar1=Wd["bq"][:, h:h + 1], scalar2=HD ** -0.5,
                                                                       op0=ALU.add, op1=ALU.mult), reads=[pb, Wd["bq"]], writes=[S.qT])
    slot = ws.block(Wd["w_qkv"], 0, 2048)
    brow = ws.row_bcast(Wd["b_qkv"], 2048)
    pb = C.bank[3]
    for c in range(NCH):
        k.op("pe", lambda e, c=c, pb=pb, slot=slot: e.matmul(pb[0:T, :], lhsT=S.hTb[:, c, 0:T], rhs=slot[:, c, :], start=(c == 0), stop=(c == NCH - 1)), reads=[S.hTb, slot], writes=[pb])
    k.op("dve", lambda e, pb=pb, brow=brow: e.tensor_tensor(out=S.kv_tm[0:T, :], in0=pb[0:T, :], in1=brow[0:T, :], op=ALU.add), reads=[pb, brow], writes=[S.kv_tm])
    if kwin_out is not None:
        r0, n = win_rows
        k.dma("sp", lambda e: e.dma_start(out=kwin_out[r0:r0 + n, :], in_=S.kv_tm[T - n:T, 0:256]), S.kv_tm, reads=[S.kv_tm])
        k.dma("sp", lambda e: e.dma_start(out=vwin_out[r0:r0 + n, :], in_=S.kv_tm[T - n:T, 256:512]), S.kv_tm, reads=[S.kv_tm])
    k.op("act", lambda e: e.copy(out=S.vcur[0:T, :], in_=S.kv_tm[0:T, 256:512]), reads=[S.kv_tm], writes=[S.vcur])
    pb = C.bank[4]
    for g in range(NKV):
        k.op("pe", lambda e, g=g, pb=pb: e.transpose(pb[0:HD, g * 128:g * 128 + T], S.kv_tm[0:T, g * HD:(g + 1) * HD], C.ident[0:T, 0:T]), reads=[S.kv_tm, C.ident], writes=[pb])
    k.op("act", lambda e, pb=pb: e.copy(out=S.kTcur[:, :, 0:T], in_=pb[0:HD, :].rearrange("p (a b) -> p a b", a=NKV)[:, :, 0:T]), reads=[pb], writes=[S.kTcur])
    NKEY = 128 + T
    for h in range(NH):
        g = h // (NH // NKV)
        ps_, pt_, po_ = C.bank[5 + h % 2], C.bank[1 + h % 2], C.bank[7]
        k.op("pe", lambda e, h=h, g=g, ps_=ps_: e.matmul(ps_[0:T, 0:128], lhsT=S.qT[:, h, 0:T], rhs=S.kTprev[:, g, :], start=True, stop=True), reads=[S.qT, S.kTprev], writes=[ps_])
        k.op("pe", lambda e, h=h, g=g, ps_=ps_: e.matmul(ps_[0:T, 128:NKEY], lhsT=S.qT[:, h, 0:T], rhs=S.kTcur[:, g, 0:T], start=True, stop=True), reads=[S.qT, S.kTcur], writes=[ps_])
        k.op("dve", lambda e, h=h, ps_=ps_: e.scalar_tensor_tensor(out=S.sc[0:T, 0:NKEY], in0=Dm[0:T, 0:NKEY], scalar=-slopes[h], in1=ps_[0:T, 0:NKEY], op0=ALU.mult, op1=ALU.add),
             reads=[Dm, ps_], writes=[S.sc])
        k.op("dve", lambda e: e.tensor_reduce(out=S.m[0:T, 0:1], in_=S.sc[0:T, 0:NKEY], axis=AX.X, op=ALU.max), reads=[S.sc], writes=[S.m])
        k.op("dve", lambda e, h=h: e.tensor_tensor(out=S.m[0:T, 0:1], in0=S.m[0:T, 0:1], in1=Wd["sink"][0:T, h:h + 1], op=ALU.max), reads=[S.m, Wd["sink"]], writes=[S.m])
        k.op("dve", lambda e, h=h: e.tensor_tensor(out=S.m[0:T, 2:3], in0=Wd["sink"][0:T, h:h + 1], in1=S.m[0:T, 0:1], op=ALU.subtract), reads=[S.m, Wd["sink"]], writes=[S.m])
        k.op("dve", lambda e: e.tensor_scalar(out=S.m[0:T, 1:2], in0=S.m[0:T, 0:1], scalar1=-1.0, scalar2=None, op0=ALU.mult), reads=[S.m], writes=[S.m])
        k.op("act", lambda e: e.activation(out=S.p[0:T, 0:NKEY], in_=S.sc[0:T, 0:NKEY], func=AF.Exp, bias=S.m[0:T, 1:2], accum_out=S.m[0:T, 3:4]), reads=[S.sc, S.m], writes=[S.p, S.m])
        k.op("act", lambda e: e.activation(out=S.m[0:T, 2:3], in_=S.m[0:T, 2:3], func=AF.Exp), reads=[S.m], writes=[S.m])
        k.op("dve", lambda e: e.tensor_tensor(out=S.m[0:T, 3:4], in0=S.m[0:T, 3:4], in1=S.m[0:T, 2:3], op=ALU.add), reads=[S.m], writes=[S.m])
        k.op("dve", lambda e: e.reciprocal(out=S.m[0:T, 3:4], in_=S.m[0:T, 3:4]), reads=[S.m], writes=[S.m])
        k.op("pe", lambda e, pt_=pt_: e.transpose(pt_[:, 0:T], S.p[0:T, 0:128], C.ident[0:T, 0:T]), reads=[S.p, C.ident], writes=[pt_])
        k.op("pe", lambda e, pt_=pt_: e.transpose(pt_[0:T, 128:128 + T], S.p[0:T, 128:NKEY], C.ident[0:T, 0:T]), reads=[S.p, C.ident], writes=[pt_])
        k.op("act", lambda e, pt_=pt_: e.copy(out=S.pT[:, 0, 0:T], in_=pt_[:, 0:T]), reads=[pt_], writes=[S.pT])
        k.op("act", lambda e, pt_=pt_: e.copy(out=S.pT[0:T, 1, 0:T], in_=pt_[0:T, 128:128 + T]), reads=[pt_], writes=[S.pT])
        oc = (h % 8) * HD
        k.op("pe", lambda e, g=g, oc=oc, po_=po_: e.matmul(po_[0:T, oc:oc + HD], lhsT=S.pT[:, 0, 0:T], rhs=S.vprev[:, g * HD:(g + 1) * HD], start=True, stop=False), reads=[S.pT, S.vprev], writes=[po_])
        k.op("pe", lambda e, g=g, oc=oc, po_=po_: e.matmul(po_[0:T, oc:oc + HD], lhsT=S.pT[0:T, 1, 0:T], rhs=S.vcur[0:T, g * HD:(g + 1) * HD], start=False, stop=True), reads=[S.pT, S.vcur], writes=[po_])
        k.op("dve", lambda e, h=h, oc=oc, po_=po_: e.tensor_scalar(out=S.o_tm[0:T, h * HD:(h + 1) * HD], in0=po_[0:T, oc:oc + HD], scalar1=S.m[0:T, 3:4], scalar2=None, op0=ALU.mult),
             reads=[po_, S.m], writes=[S.o_tm])
    if carry:
        k.op("act", lambda e: e.copy(out=S.kTprev[:, :, :], in_=S.kTcur[:, :, :]), reads=[S.kTcur], writes=[S.kTprev])
        k.op("act", lambda e: e.copy(out=S.vprev[:, :], in_=S.vcur[:, :]), reads=[S.vcur], writes=[S.vprev])
    for b4 in range(4):
        pb = C.bank[1 + b4 % 2]
        for j in range(4):
            c = b4 * 4 + j
            k.op("pe", lambda e, c=c, j=j, pb=pb: e.transpose(pb[:, j * 128:j * 128 + T], S.o_tm[0:T, c * 128:(c + 1) * 128], C.ident[0:T, 0:T]), reads=[S.o_tm, C.ident], writes=[pb])
        k.op("act", lambda e, b4=b4, pb=pb: e.copy(out=S.oT[:, b4 * 4:(b4 + 1) * 4, 0:T], in_=pb[:, :].rearrange("p (a b) -> p a b", a=4)[:, :, 0:T]), reads=[pb], writes=[S.oT])
    for nb in range(4):
        slot = ws.block(Wd["w_o"], 0, nb * 512)
        pb = C.bank[3 + nb % 2]
        for c in range(NCH):
            k.op("pe", lambda e, c=c, pb=pb, slot=slot: e.matmul(pb[0:T, :], lhsT=S.oT[:, c, 0:T], rhs=slot[:, c, :], start=(c == 0), stop=(c == NCH - 1)), reads=[S.oT, slot], writes=[pb])
        k.op("act", lambda e, nb=nb, pb=pb: e.copy(out=S.out_tm[0:T, nb * 512:(nb + 1) * 512], in_=pb[0:T, :]), reads=[pb], writes=[S.out_tm])
    gated_residual_add(k, C, xT, S.out_tm, gate, T)


def attn_scratch(k):
    S = NS()
    S.hT32 = k.sb_once("p_hT32", [128, NCH, 128], F32)
    S.hTb = k.sb_once("p_hTb", [128, NCH, 128], BF16)
    S.out_tm = k.sb_once("p_acc", [128, D], F32)
    S.qT = k.sb("t_qT", [HD, NH, 128], BF16)
    S.kv_tm = k.sb("t_kv", [128, 512], F32)
    S.kTcur = k.sb("t_kTc", [HD, NKV, 128], BF16)
    S.kTprev = k.sb("t_kTp", [HD, NKV, 128], BF16)
    S.vcur = k.sb("t_vc", [128, 256], BF16)
    S.vprev = k.sb("t_vp", [128, 256], BF16)
    S.sc = k.sb("t_sc", [128, 256], F32)
    S.p = k.sb("t_p", [128, 256], F32)
    S.pT = k.sb("t_pT", [128, 2, 128], BF16)
    S.m = k.sb("t_m", [128, 4], F32)
    S.o_tm = k.sb("t_o", [128, D], F32)
    S.oT = k.sb("t_oT", [128, NCH, 128], BF16)
    return S


def ada_layer(k, C, ws, scT, adaw, adab_fm, modv, l, ng):
    for nb in range(24):
        slot = ws.block(adaw, 0, nb * 512, cache=False)
        pb = C.bank[3 + nb % 2]
        for j in range(4):
            for c in range(NCH):
                k.op("pe", lambda e, j=j, c=c, pb=pb, slot=slot: e.matmul(pb[:, j * 8:j * 8 + ng], lhsT=slot[:, c, j * 128:(j + 1) * 128], rhs=scT[:, c, 0:ng],
                                                                        start=(c == 0), stop=(c == NCH - 1)), reads=[slot, scT], writes=[pb])
        for j in range(4):
            ch = nb * 4 + j
            k.op("dve", lambda e, j=j, ch=ch, pb=pb: e.tensor_scalar(out=modv[:, l, 0:ng, ch], in0=pb[:, j * 8:j * 8 + ng], scalar1=adab_fm[:, ch:ch + 1], scalar2=None, op0=ALU.add),
                 reads=[pb, adab_fm], writes=[modv])


def final_norm_out(k, C, xT, gfin, zero16, S, out_rows, T=128):
    rms_modulate(k, C, xT, gfin, zero16, S.hT32, None, T)
    to_token_major(k, C, S.hT32, S.out_tm, T)
    k.dma("sp", lambda e: e.dma_start(out=out_rows, in_=S.out_tm[0:T, :]), S.out_tm, reads=[S.out_tm])


def peer_q_streamed(k, C, ws, wq_dram, S, T=128):
    for nb in range(4):
        slot = ws.block(wq_dram, 0, nb * 512)
        for j in range(4):
            hc = nb * 4 + j
            pb = C.bank[3 + (hc % 2)]
            for c in range(NCH):
                k.op("pe", lambda e, j=j, c=c, pb=pb, slot=slot: e.matmul(pb[:, 0:T], lhsT=slot[:, c, j * 128:(j + 1) * 128], rhs=S.hTb[:, c, 0:T],
                                                                        start=(c == 0), stop=(c == NCH - 1)), reads=[slot, S.hTb], writes=[pb])
            k.op("act", lambda e, hc=hc, pb=pb: e.copy(out=S.qT[:, hc, 0:T], in_=pb[:, 0:T]), reads=[pb], writes=[S.qT])


NCORES = 8
NTILE = 16
NHALO = 2
DEPTH = 4
KIND = [0, 1, 2, 0]


def _view(k, arena, p0, p1, a, b, name, shape3=None):
    ap = arena.ap[p0:p1, a:b]
    if shape3 is not None:
        ap = ap.rearrange("p (a b) -> p a b", a=shape3[0])
    return k.view(ap, name)


def build_nc(ntile=NTILE, nsamp=1, halo=True):
    nc = bass.Bass("TRN2", target_bir_lowering=False)

    def din(n, s, dt=F32):
        return nc.dram_tensor(n, list(s), dt, kind="ExternalInput").ap()

    def dout(n, s, dt=F32):
        return nc.dram_tensor(n, list(s), dt, kind="ExternalOutput").ap()

    NG = 1 + nsamp
    nh = NHALO if halo else 0
    xp_d = din("xp_fm", [nh + ntile, 128, 16, 128]); flag_d = din("flag", [128, 1]); xs_d = din("xs_fm", [nsamp, 128, 16, 128]); cT_d = din("cT", [128, 16, NG])
    ck_d = din("cache_k", [nsamp, 128, 256]); cv_d = din("cache_v", [nsamp, 128, 256]); st_d = din("state_conv", [nsamp, 30, 2048])
    gmix_d = din("g_mix_fm", [128, DEPTH, 16]); gch_d = din("g_ch_fm", [128, DEPTH, 16]); gfin_d = din("g_fin_fm", [128, 16])
    adaw_d = [din(f"ada_w{l}", [2048, 12288]) for l in range(DEPTH)]; adab_d = din("ada_b_fm", [128, DEPTH, 96])
    sgu_win_d = [din(f"sgu_w_in{i}", [2048, 12288]) for i in range(2)]; sgu_wout_d = [din(f"sgu_w_out{i}", [6144, 2048]) for i in range(2)]
    sgu_bin_d = din("sgu_b_in", [2, 12288]); sgu_lng_d = din("sgu_ln_g", [2, 6144]); sgu_lnb_d = din("sgu_ln_b", [2, 6144])
    wsT_d = din("sgu_wsT", [128, 2, 8, 128]); bs_d = din("sgu_bs_tm", [128, 2, 8])
    cwin_d = din("conv_w_in", [2048, 4096]); cwout_d = din("conv_w_out", [2048, 2048]); cbout_d = din("conv_b_out", [2048])
    cbin_d = din("conv_b_in_fm", [128, 32]); cdw_d = din("conv_dwT", [128, 16, 31]); cdwb_d = din("conv_dw_b_fm", [128, 16])
    clng_d = din("conv_ln_g_fm", [128, 16]); clnb_d = din("conv_ln_b_fm", [128, 16])
    wqkv_d = din("attn_w_qkv", [2048, 2560]); bqkv_d = din("attn_b_qkv", [2560]); bq_d = din("attn_bq_fm", [64, 32]); sink_d = din("attn_sinks", [32]); wo_d = din("attn_w_o", [2048, 2048])
    pwq_d = [din(f"peer_w_q{l}", [2048, 2048]) for l in range(DEPTH)]; skT_d = din("peer_skT", [128, DEPTH, 16, 128])
    pu_d = [din(f"peer_u{l}", [16384, 2048]) for l in range(DEPTH)]; pv_d = [din(f"peer_v{l}", [16384, 2048]) for l in range(DEPTH)]
    y_p = dout("y_p", [ntile * 128, 2048]); y_s = dout("y_s", [nsamp, 16, 2048]); cs_p = dout("cs_p", [30, 2048]); kw_p = dout("kw_p", [128, 256]); vw_p = dout("vw_p", [128, 256])
    cs_s = dout("cs_s", [nsamp, 30, 2048]); kw_s = dout("kw_s", [nsamp, 128, 256]); vw_s = dout("vw_s", [nsamp, 128, 256]); sv_s = dout("sv_s", [2, nsamp, 16, 6144])

    with ExitStack() as st:
        k = K(nc, st)
        C = Common(k)
        A = attn_consts(k)
        def load(name, shape, src, dt=F32, q="sp"):
            b = k.sb(name, shape, dt)
            k.dma(q, lambda e, b=b, src=src: e.dma_start(out=b[:], in_=src), b, writes=[b])
            return b
        cT = load("cT_sb", [128, 16, NG], cT_d)
        gmix = load("gmix_sb", [128, DEPTH, 16], gmix_d); gch = load("gch_sb", [128, DEPTH, 16], gch_d); gfin = load("gfin_sb", [128, 16], gfin_d)
        adab = load("adab_sb", [128, DEPTH, 96], adab_d)
        wsT = load("wsT_sb", [128, 2, 8, 128], wsT_d, BF16, q="pool")
        bs = load("bs_sb", [128, 2, 8], bs_d)
        cbin = load("cbin_sb", [128, 32], cbin_d); cdw = load("cdw_sb", [128, 16, 31], cdw_d); cdwb = load("cdwb_sb", [128, 16], cdwb_d)
        clng = load("clng_sb", [128, 16], clng_d); clnb = load("clnb_sb", [128, 16], clnb_d)
        bq = load("bq_sb", [64, 32], bq_d)
        sink = k.sb("sink_sb", [128, 32], F32)
        k.dma("sp", lambda e: e.dma_start(out=sink[:], in_=sink_d.unsqueeze(0).partition_broadcast(128)), sink, writes=[sink])
        zero16 = k.sb("zero16", [128, 16], F32)
        k.op("dve", lambda e: e.memset(zero16[:], 0.0), writes=[zero16])
        flag = load("flag_sb", [128, 1], flag_d)
        nf = k.sb("nf_sb", [128, 1], F32)
        k.op("dve", lambda e: e.tensor_scalar(out=nf[:], in0=flag[:], scalar1=-BIGD, scalar2=BIGD, op0=ALU.mult, op1=ALU.add), reads=[flag], writes=[nf])
        Dsel = k.sb("a_Dsel", [128, 256], F32)
        k.op("dve", lambda e: e.tensor_scalar(out=Dsel[:, 0:128], in0=A.Dmid[:, 0:128], scalar1=nf[:, 0:1], scalar2=None, op0=ALU.add), reads=[A.Dmid, nf], writes=[Dsel])
        k.op("dve", lambda e: e.tensor_copy(out=Dsel[:, 128:256], in_=A.Dmid[:, 128:256]), reads=[A.Dmid], writes=[Dsel])
        k.op("dve", lambda e: e.memset(wsT[64:128, :, :, 0:64], 0.0), writes=[wsT])
        xT = k.sb("xT", [128, 16, 128], F32)
        hT32 = k.sb_once("p_hT32", [128, NCH, 128], F32); hTb = k.sb_once("p_hTb", [128, NCH, 128], BF16); acc = k.sb_once("p_acc", [128, D], F32)
        NF, NB = 10240, 18432
        AFa = k.sb("arena_f32", [128, NF], F32); ABa = k.sb("arena_bf16", [128, NB], BF16)
        skslot = k.sb("skslot", [128, 16, 128], BF16)
        SG = NS(); SG.hT32, SG.hTb, SG.out_tm = hT32, hTb, acc
        SG.v = _view(k, AFa, 0, 128, 0, 6144, "g_v"); SG.prod = SG.v
        SG.u = _view(k, ABa, 0, 128, 0, 6144, "g_u"); SG.vb = _view(k, ABa, 0, 128, 6144, 12288, "g_vb")
        SG.prodT = _view(k, ABa, 0, 128, 12288, 18432, "g_prodT", (48, 128)); SG.st = k.sb("g_st", [128, 4], F32)
        SG.tmp = _view(k, AFa, 0, 128, 6144, 6656, "g_tmp")
        CV = NS(); CV.hT32, CV.hTb, CV.out_tm = hT32, hTb, acc
        CV.xp = k.sb("c_xp", [128, NCH, CW - 1 + 128], F32); CV.hist_tmp = k.sb("c_hist", [128, NCH, CW - 1], F32)
        CV.y = _view(k, AFa, 0, 128, 0, 2048, "c_y", (16, 128)); CV.tm30 = _view(k, AFa, 0, 32, 2048, 4096, "c_tm30")
        CV.sg = _view(k, AFa, 0, 128, 4096, 4224, "c_sg"); CV.mv = _view(k, AFa, 0, 128, 4224, 4608, "c_mv")
        CV.yb = _view(k, ABa, 0, 128, 0, 2048, "c_yb", (16, 128)); CV.y2b = _view(k, ABa, 0, 128, 2048, 4096, "c_y2b", (16, 128))
        AT = NS(); AT.hT32, AT.hTb, AT.out_tm = hT32, hTb, acc
        AT.kTprev = k.sb("t_kTp", [HD, NKV, 128], BF16); AT.vprev = k.sb("t_vp", [128, 256], BF16); AT.m = k.sb("t_m", [128, 4], F32)
        AT.o_tm = _view(k, AFa, 0, 128, 0, 2048, "t_o"); AT.kv_tm = _view(k, AFa, 0, 128, 2048, 2560, "t_kv")
        AT.sc = _view(k, AFa, 0, 128, 2560, 2816, "t_sc"); AT.p = _view(k, AFa, 0, 128, 2816, 3072, "t_p")
        AT.qT = _view(k, ABa, 0, HD, 0, 4096, "t_qT", (NH, 128)); AT.kTcur = _view(k, ABa, 0, HD, 4096, 4608, "t_kTc", (NKV, 128))
        AT.vcur = _view(k, ABa, 0, 128, 4608, 4864, "t_vc"); AT.pT = _view(k, ABa, 0, 128, 4864, 5120, "t_pT", (2, 128))
        AT.oT = _view(k, ABa, 0, 128, 5120, 7168, "t_oT", (16, 128))
        PE = NS(); PE.hT32, PE.hTb, PE.acc = hT32, hTb, acc
        PE.h_tm = _view(k, AFa, 0, 128, 0, 2048, "p_h_tm")
        PE.s = _view(k, AFa, 0, 128, 2048, 4096, "p_s", (NHC, PK)); PE.s2 = _view(k, AFa, 0, 128, 4096, 6144, "p_s2", (NHC, PK))
        PE.cand = k.view(AFa.ap[:, 6144:8192].rearrange("p (h a b) -> p h a b", h=PH, a=16), "p_cand")
        PE.cand2 = k.view(AFa.ap[:, 8192:10240].rearrange("p (h a b) -> p h a b", h=PH, a=16), "p_cand2")
        PE.gbuf = [_view(k, ABa, 0, 128, 2048 * (i + 1), 2048 * (i + 2), f"p_gb{i}") for i in range(6)]
        PE.junk = _view(k, AFa, 0, 128, 8192, 10240, "p_junk")
        PE.qT = _view(k, ABa, 0, 128, 0, 2048, "p_qT", (NHC, 128))
        for n_, shp, dt_ in (("sv", [128, NHC, 16], F32), ("si", [128, NHC, 16], U32), ("sif", [128, NHC, 16], F32), ("best", [128, PH, 16], F32), ("bi", [128, PH, 16], U32),
                             ("b1", [128, PH, 16], U32), ("b2", [128, PH, 16], U32), ("b1f", [128, PH, 16], F32), ("b2f", [128, PH, 16], F32), ("oh", [128, 16, 16], F32),
                             ("I1", [128, PH, 16], F32), ("I2", [128, PH, 16], F32), ("ef", [128, PH, 16], F32), ("ei", [128, PH, 16], I32), ("g", [128, PH, 16], F32),
                             ("gs", [128, PH], F32), ("a", [128, PH, 16], F32), ("w", [128, PH, 16], F32)):
            setattr(PE, n_, k.sb("p_" + n_, shp, dt_))
        tb_u = [nc.dram_tensor(f"ub{l}", [16384, 2048], BF16).ap() for l in range(DEPTH)]
        tb_v = [nc.dram_tensor(f"vb{l}", [16384, 2048], BF16).ap() for l in range(DEPTH)]
        cbufs = [k.view(ABa.ap[:, q * 4608:q * 4608 + 4096].rearrange("p (a b) -> p a b", a=2), f"cvt{q}") for q in range(4)]
        ci = 0
        for (src_l, dst_l) in ((pu_d, tb_u), (pv_d, tb_v)):
            for l in range(DEPTH):
                sv_ = src_l[l].rearrange("(p r) d -> p r d", p=128)
                dv_ = dst_l[l].rearrange("(p r) d -> p r d", p=128)
                dbuf = k.view(dst_l[l], f"tb{ci}")
                for r0 in range(0, 128, 2):
                    cb = cbufs[ci % 4]
                    ci += 1
                    k.dma("pool", lambda e, cb=cb, sv_=sv_, r0=r0: e.dma_start(out=cb[:, :, :], in_=sv_[:, r0:r0 + 2, :]), cb, writes=[cb])
                    k.dma("sp", lambda e, cb=cb, dv_=dv_, r0=r0: e.dma_start(out=dv_[:, r0:r0 + 2, :], in_=cb[:, :, :]), cb, reads=[cb], writes=[dbuf])
        k.barrier()
        scT = k.sb("scT", [128, 16, NG], BF16)
        modv = k.sb("modv", [128, DEPTH, NG, 96], F32)
        der = k.sb("der", [128, DEPTH, NG, 2, 16], F32)
        k.sb_once("rm_sq", [128, NCH, 128], BF16); k.sb_once("rm_rstd", [128, 128], F32)
        left = nc.sbuf_bytes_remaining - 4 * 2048 - 1024
        nsl = max(1, min(3, left // 16384))
        print("[kernel] sbuf left before weight slots:", nc.sbuf_bytes_remaining, "-> slots:", nsl, flush=True)
        assert left >= 16384, "no room for a weight slot"
        ws = WS(k, nslots=nsl, q="pool")
        ws.cache_ap = nc.dram_tensor("wcache", [128, 128, 16 * 512], BF16).ap()
        k.op("act", lambda e: e.activation(out=scT[:], in_=cT[:], func=AF.Silu), reads=[cT], writes=[scT])
        for l in range(DEPTH):
            adab_l = k.view(adab.ap[:, l, :], f"adab{l}")
            k.barrier()
            ada_layer(k, C, ws, scT, adaw_d[l], adab_l, modv, l, NG)
        for l in range(DEPTH):
            for g in range(NG):
                for (j, off, gn) in ((0, 16, gmix), (1, 64, gch)):
                    k.op("dve", lambda e, l=l, g=g, j=j, off=off: e.tensor_scalar(out=der[:, l, g, j, :], in0=modv[:, l, g, off:off + 16], scalar1=1.0, scalar2=None, op0=ALU.add),
                         reads=[modv], writes=[der])
                    k.op("dve", lambda e, l=l, g=g, j=j, gn=gn: e.tensor_tensor(out=der[:, l, g, j, :], in0=der[:, l, g, j, :], in1=gn[:, l, :], op=ALU.mult), reads=[der, gn], writes=[der])
        k.barrier()

        def vec(ap, name):
            return k.view(ap, name)

        def mods(l, g):
            return dict(gm1=vec(der.ap[:, l, g, 0, :], "gm1"), sh1=vec(modv.ap[:, l, g, 0:16], "sh1"), g1=vec(modv.ap[:, l, g, 32:48], "g1"),
                        gm2=vec(der.ap[:, l, g, 1, :], "gm2"), sh2=vec(modv.ap[:, l, g, 48:64], "sh2"), g2=vec(modv.ap[:, l, g, 80:96], "g2"))
        MODS = [[mods(l, g) for g in range(NG)] for l in range(DEPTH)]
        SGW = [dict(w_in=sgu_win_d[i], b_in=sgu_bin_d[i], ln_g=sgu_lng_d[i], ln_b=sgu_lnb_d[i], w_out=sgu_wout_d[i],
                    wsT=vec(wsT.ap[:, i], f"wsT{i}"), bs=vec(bs.ap[:, i], f"bs{i}")) for i in range(2)]
        CVW = dict(w_in=cwin_d, w_out=cwout_d, b_out=cbout_d, b_in=cbin, dw=cdw, dw_b=cdwb, ln_g=clng, ln_b=clnb)
        ATW = dict(w_qkv=wqkv_d, b_qkv=bqkv_d, w_o=wo_d, bq=bq, sink=sink)

        def run_tile(grp, T, x_src, first, last, si, mode="full", attn_first=None, Dm_first=None):
            k.dma("sp", lambda e: e.dma_start(out=xT[:], in_=x_src), xT, writes=[xT])
            isgu = 0
            afirst = first if attn_first is None else attn_first
            for l in range(DEPTH):
                M = MODS[l][grp]
                k.barrier()
                if KIND[l] == 0:
                    vout = sv_s[isgu, si] if si is not None else None
                    sgu(k, C, ws, xT, M["gm1"], M["sh1"], M["g1"], SGW[isgu], SG, T=T, v_out=vout)
                    isgu += 1
                elif KIND[l] == 1:
                    if si is not None:
                        conv_load_hist_state(k, C, CV, st_d[si])
                    elif first:
                        conv_load_hist_zero(k, CV)
                    so = cs_s[si] if si is not None else (cs_p if last else None)
                    conv(k, C, ws, xT, M["gm1"], M["sh1"], M["g1"], CVW, CV, T=T, state_out=so, carry=(si is None))
                    if mode == "h0":
                        k.barrier()
                        return
                else:
                    if si is not None:
                        attn_load_prev(k, C, AT, ck_d[si], cv_d[si])
                        cpy = k.view(kw_s[si], f"kws_cpy{si}")
                        k.dma("sp", lambda e, si=si: e.dma_start(out=kw_s[si, 0:112, :], in_=ck_d[si, 16:128, :]), cpy, writes=[cpy])
                        k.dma("sp", lambda e, si=si: e.dma_start(out=vw_s[si, 0:112, :], in_=cv_d[si, 16:128, :]), cpy, writes=[cpy])
                        attn(k, C, ws, xT, M["gm1"], M["sh1"], M["g1"], ATW, AT, A.Dsamp, T=T, kwin_out=kw_s[si], vwin_out=vw_s[si], win_rows=(112, 16), carry=False)
                    else:
                        if afirst:
                            k.op("dve", lambda e: e.memset(AT.kTprev[:, :, :], 0.0), writes=[AT.kTprev])
                            k.op("dve", lambda e: e.memset(AT.vprev[:, :], 0.0), writes=[AT.vprev])
                        Dm = A.Dfirst if afirst else (Dm_first if Dm_first is not None else A.Dmid)
                        attn(k, C, ws, xT, M["gm1"], M["sh1"], M["g1"], ATW, AT, Dm, T=T,
                             kwin_out=kw_p if last else None, vwin_out=vw_p if last else None, win_rows=(0, 128) if last else None)
                        if mode == "h1":
                            k.barrier()
                            return
                k.barrier()
                k.dma("pool", lambda e, l=l: e.dma_start(out=skslot[:], in_=skT_d[:, l]), skslot, writes=[skslot])
                peer(k, C, xT, M["gm2"], M["sh2"], M["g2"], PeerW(k, pwq_d[l], skslot, tb_u[l], tb_v[l]), PE, T=128, ws=ws, mid_barrier=True)
            k.barrier()
            return

        FN = NS(); FN.hT32, FN.out_tm = hT32, acc
        if halo:
            run_tile(0, 128, xp_d[0], True, False, None, mode="h0")
            run_tile(0, 128, xp_d[1], False, False, None, mode="h1", attn_first=True)
            k.op("dve", lambda e: e.tensor_scalar(out=CV.xp[:, :, 0:CW - 1], in0=CV.xp[:, :, 0:CW - 1], scalar1=flag[:, 0:1], scalar2=None, op0=ALU.mult),
                 reads=[CV.xp, flag], writes=[CV.xp])
        for t in range(ntile):
            if halo:
                run_tile(0, 128, xp_d[nh + t], False, t == ntile - 1, None, attn_first=False, Dm_first=(Dsel if t == 0 else None))
            else:
                run_tile(0, 128, xp_d[t], t == 0, t == ntile - 1, None)
            final_norm_out(k, C, xT, gfin, zero16, FN, y_p[t * 128:(t + 1) * 128, :], T=128)
        for si in range(nsamp):
            run_tile(1 + si, 16, xs_d[si], False, False, si)
            final_norm_out(k, C, xT, gfin, zero16, FN, y_s[si], T=16)
        k.finish()
        print("[kernel] instructions:", k.n, "sbuf bytes left:", nc.sbuf_bytes_remaining, "weight slots:", nsl, flush=True)
    return nc


def _fm(v):
    v = np.asarray(v)
    lead = v.shape[:-1]
    return np.ascontiguousarray(np.moveaxis(v.reshape(*lead, -1, 128), -1, 0))


def make_in_maps(I, ncores, ntile, nsamp, halo=True):
    f32 = lambda a: np.ascontiguousarray(np.asarray(a), dtype=np.float32)
    shared = {}
    shared["g_mix_fm"] = _fm(I["norm_mix_g"]); shared["g_ch_fm"] = _fm(I["norm_ch_g"]); shared["g_fin_fm"] = _fm(I["norm_final_g"])
    shared["ada_b_fm"] = _fm(I["ada_b"])
    for l in range(DEPTH):
        shared[f"ada_w{l}"] = f32(I["ada_w"][l]); shared[f"peer_w_q{l}"] = f32(I["peer_w_q"][l])
        shared[f"peer_u{l}"] = f32(I["peer_u"][l]); shared[f"peer_v{l}"] = f32(I["peer_v"][l])
    for i in range(2):
        shared[f"sgu_w_in{i}"] = f32(I["sgu_w_in"][i]); shared[f"sgu_w_out{i}"] = f32(I["sgu_w_out"][i])
    shared["sgu_b_in"] = f32(I["sgu_b_in"]); shared["sgu_ln_g"] = f32(I["sgu_ln_g"]); shared["sgu_ln_b"] = f32(I["sgu_ln_b"])
    shared["sgu_wsT"] = np.ascontiguousarray(np.asarray(I["sgu_w_s"]).transpose(3, 0, 1, 2))
    shared["sgu_bs_tm"] = np.ascontiguousarray(np.asarray(I["sgu_b_s"]).transpose(2, 0, 1))
    shared["conv_w_in"] = f32(I["conv_w_in"][0]); shared["conv_w_out"] = f32(I["conv_w_out"][0]); shared["conv_b_out"] = f32(I["conv_b_out"][0])
    shared["conv_b_in_fm"] = _fm(I["conv_b_in"][0]); shared["conv_dw_b_fm"] = _fm(I["conv_dw_b"][0])
    shared["conv_ln_g_fm"] = _fm(I["conv_ln_g"][0]); shared["conv_ln_b_fm"] = _fm(I["conv_ln_b"][0])
    shared["conv_dwT"] = np.ascontiguousarray(np.asarray(I["conv_dw"][0]).T.reshape(16, 128, 31).transpose(1, 0, 2))
    shared["attn_w_qkv"] = f32(I["attn_w_qkv"][0]); shared["attn_b_qkv"] = f32(I["attn_b_qkv"][0]); shared["attn_w_o"] = f32(I["attn_w_o"][0])
    shared["attn_bq_fm"] = np.ascontiguousarray(np.asarray(I["attn_b_qkv"][0][:2048]).reshape(32, 64).T); shared["attn_sinks"] = f32(I["attn_sinks"][0])
    shared["peer_skT"] = np.ascontiguousarray(np.asarray(I["peer_subkeys"]).reshape(DEPTH, 16, 128, 128).transpose(3, 0, 1, 2))
    xp = np.asarray(I["x_prompt"]); xs = np.asarray(I["x_sample"])
    in_maps = []
    for c in range(ncores):
        m = dict(shared)
        if halo:
            b, half = c // 2, c % 2
            rows = np.zeros(((NHALO + ntile) * 128, 2048), np.float32)
            if half:
                rows[:NHALO * 128] = xp[b, ntile * 128 - NHALO * 128:ntile * 128]
            rows[NHALO * 128:] = xp[b, half * ntile * 128:(half + 1) * ntile * 128]
            m["xp_fm"] = np.ascontiguousarray(rows.reshape(NHALO + ntile, 128, 16, 128).transpose(0, 3, 2, 1))
            m["flag"] = np.full((128, 1), float(half), np.float32)
        else:
            m["xp_fm"] = np.ascontiguousarray(xp[c].reshape(ntile, 128, 16, 128).transpose(0, 3, 2, 1))
            m["flag"] = np.ones((128, 1), np.float32)
        xsp = np.zeros((nsamp, 128, 2048), np.float32); xsp[:, :16] = xs[nsamp * c:nsamp * c + nsamp]
        m["xs_fm"] = np.ascontiguousarray(xsp.reshape(nsamp, 128, 16, 128).transpose(0, 3, 2, 1))
        cc = np.stack([np.asarray(I["c_prompt"])[c // 2 if halo else c]] + [np.asarray(I["c_sample"])[nsamp * c + i] for i in range(nsamp)])
        m["cT"] = np.ascontiguousarray(cc.reshape(1 + nsamp, 16, 128).transpose(2, 1, 0))
        m["cache_k"] = f32(np.asarray(I["cache_k_win"])[0, nsamp * c:nsamp * c + nsamp].reshape(nsamp, 128, 256))
        m["cache_v"] = f32(np.asarray(I["cache_v_win"])[0, nsamp * c:nsamp * c + nsamp].reshape(nsamp, 128, 256))
        m["state_conv"] = f32(np.asarray(I["state_conv"])[0, nsamp * c:nsamp * c + nsamp])
        in_maps.append(m)
    return in_maps


def kernel(**I):
    in_maps = make_in_maps(I, NCORES, NTILE, 1, halo=True)
    nc = build_nc(NTILE, 1, True)
    res = run_bass_kernel_spmd(nc, in_maps, core_ids=list(range(NCORES))).results
    R = lambda key: np.stack([np.asarray(r[key]) for r in res])
    odd = lambda key: np.stack([np.asarray(res[c][key]) for c in range(1, NCORES, 2)])
    y_prompt = R("y_p").reshape(4, 4096, 2048)
    y_sample = R("y_s").reshape(8, 16, 2048)
    conv_state_prompt = odd("cs_p").reshape(1, 4, 30, 2048)
    k_win_prompt = odd("kw_p").reshape(1, 4, 128, 4, 64); v_win_prompt = odd("vw_p").reshape(1, 4, 128, 4, 64)
    conv_state_sample = R("cs_s").reshape(1, 8, 30, 2048)
    k_win_sample = R("kw_s").reshape(1, 8, 128, 4, 64); v_win_sample = R("vw_s").reshape(1, 8, 128, 4, 64)
    sgu_v_sample = np.ascontiguousarray(R("sv_s").transpose(1, 0, 2, 3, 4)).reshape(2, 8, 16, 6144)
    return tuple(np.ascontiguousarray(a, dtype=np.float32) for a in (y_prompt, y_sample, conv_state_prompt, k_win_prompt, v_win_prompt,
                                                                     conv_state_sample, k_win_sample, v_win_sample, sgu_v_sample))
```
